# Optimizing a Trainium2 kernel written in Bass

```python
import math
import jax, jax.numpy as jnp
from jax import lax
import numpy as np

D_MODEL = 1024
BATCH = 8
SEQ = 2048
DEPTH = 4

GRID_W = 64
CTX_LEN = 256
HEAD_DIM = 64
A_HEADS = 4
A_KV_HEADS = 2
A_BLOCK = 128
A_WINDOW = 128
ROPE_BASE = 10000.0
SSD_HEADS = 8
SSD_HEAD_DIM = 64
SSD_INNER = SSD_HEADS * SSD_HEAD_DIM
SSD_GROUPS = 2
SSD_STATE = 64
SSD_CONV = 5
SSD_CHUNK = 128
SSD_CONV_DIM = SSD_INNER + 2 * SSD_GROUPS * SSD_STATE
NA_HEADS = 4
NA_WIN_ROWS = 8
NA_WIN_COLS = 16
A_WIDTH = A_HEADS * HEAD_DIM
NA_WIDTH = NA_HEADS * HEAD_DIM
D_MIX = A_WIDTH + SSD_INNER + NA_WIDTH
A_IN = (A_HEADS + 2 * A_KV_HEADS) * HEAD_DIM
SSD_IN = SSD_INNER + SSD_CONV_DIM + 2 * SSD_HEADS
NA_IN = 3 * NA_WIDTH
N_IN = A_IN + SSD_IN + NA_IN
MOE_GROUPS = 4
MOE_EXPERTS = 8
MOE_TOPK = 2
D_EXPERT = 256
N_MOD = 6
RMS_EPS = 1e-6
NEG_INF = -1e30

kernel_name = 'hybrid_parallel_heads_diffusion_trunk'

F32 = jnp.float32


def rmsnorm(x, g):
    xf = x.astype(F32)
    y = xf * lax.rsqrt(jnp.mean(xf * xf, axis=-1, keepdims=True) + RMS_EPS)
    return (y * g.astype(F32)).astype(x.dtype)


def modulate(h, shift, scale):
    return h * (1 + scale) + shift


def to_heads(t):
    return t.reshape(t.shape[:-1] + (-1, HEAD_DIM))


def rope_axis(xa, pos):
    d = xa.shape[-1]
    inv = 1.0 / (ROPE_BASE ** (jnp.arange(0, d, 2, dtype=F32) / d))
    ang = pos.astype(F32)[:, None] * inv[None, :]
    cos = jnp.cos(ang)[:, None, :]
    sin = jnp.sin(ang)[:, None, :]
    x1, x2 = xa[..., : d // 2], xa[..., d // 2:]
    return jnp.concatenate([x1 * cos - x2 * sin, x2 * cos + x1 * sin], axis=-1)


def rope_2d(x, rows, cols):
    xf = x.astype(F32)
    half = x.shape[-1] // 2
    out = jnp.concatenate([rope_axis(xf[..., :half], rows), rope_axis(xf[..., half:], cols)], axis=-1)
    return out.astype(x.dtype)


def dense_attn(q, k, v, sink):
    b, l, hq, hd = q.shape
    hkv = k.shape[2]
    rep = hq // hkv
    qg = q.reshape(b, l, hkv, rep, hd)
    s = jnp.einsum('blgrd,bmgd->bgrlm', qg, k).astype(F32) * hd ** -0.5
    if sink is not None:
        sk = jnp.broadcast_to(sink.astype(F32).reshape(hkv, rep, 1, 1), (b, hkv, rep, l, 1))
        s = jnp.concatenate([s, sk], axis=-1)
    p = jax.nn.softmax(s, axis=-1)[..., : k.shape[1]]
    o = jnp.einsum('bgrlm,bmgd->blgrd', p.astype(v.dtype), v)
    return o.reshape(b, l, hq * hd)


def window_gqa(q, k, v, kc, vc, sink):
    b, s, hq, hd = q.shape
    nb = s // A_BLOCK
    rep = hq // A_KV_HEADS
    qb = q.reshape(b, nb, A_BLOCK, A_KV_HEADS, rep, hd)
    pad = ((0, 0), (A_BLOCK, A_BLOCK), (0, 0), (0, 0))
    kp = jnp.pad(k, pad).reshape(b, nb + 2, A_BLOCK, A_KV_HEADS, hd)
    vp = jnp.pad(v, pad).reshape(b, nb + 2, A_BLOCK, A_KV_HEADS, hd)
    kb = jnp.concatenate([kp[:, :-2], kp[:, 1:-1], kp[:, 2:]], axis=2)
    vb = jnp.concatenate([vp[:, :-2], vp[:, 1:-1], vp[:, 2:]], axis=2)
    scale = hd ** -0.5
    s_loc = jnp.einsum('bnqgrd,bnkgd->bngrqk', qb, kb).astype(F32) * scale
    qpos = jnp.arange(nb)[:, None] * A_BLOCK + jnp.arange(A_BLOCK)[None, :]
    kpos = jnp.arange(nb)[:, None] * A_BLOCK - A_BLOCK + jnp.arange(3 * A_BLOCK)[None, :]
    valid = ((jnp.abs(qpos[:, :, None] - kpos[:, None, :]) <= A_WINDOW)
             & (kpos >= 0)[:, None, :] & (kpos < s)[:, None, :])
    s_loc = jnp.where(valid[None, :, None, None], s_loc, NEG_INF)
    s_ctx = jnp.einsum('bnqgrd,blgd->bngrql', qb, kc).astype(F32) * scale
    s_sink = jnp.broadcast_to(sink.astype(F32).reshape(1, 1, A_KV_HEADS, rep, 1, 1),
                              (b, nb, A_KV_HEADS, rep, A_BLOCK, 1))
    p = jax.nn.softmax(jnp.concatenate([s_loc, s_ctx, s_sink], axis=-1), axis=-1)
    n_loc = 3 * A_BLOCK
    p_loc = p[..., :n_loc].astype(v.dtype)
    p_ctx = p[..., n_loc:n_loc + kc.shape[1]].astype(v.dtype)
    o = (jnp.einsum('bngrqk,bnkgd->bnqgrd', p_loc, vb)
         + jnp.einsum('bngrql,blgd->bnqgrd', p_ctx, vc))
    return o.reshape(b, s, hq * hd)


def neighborhood_attn(q, k, v, kc, vc, rpb):
    b, s, h, hd = q.shape
    rows = s // GRID_W
    kh = min(NA_WIN_ROWS, rows)
    kw = NA_WIN_COLS
    r = jnp.arange(rows)
    rs = jnp.clip(r - kh // 2, 0, rows - kh)
    row_idx = rs[:, None] + jnp.arange(kh)[None, :]
    cq = jnp.arange(GRID_W)
    cs = jnp.clip(cq - kw // 2, 0, GRID_W - kw)
    kcol = jnp.arange(GRID_W)
    col_valid = (kcol[None, :] >= cs[:, None]) & (kcol[None, :] < cs[:, None] + kw)
    qg = q.reshape(b, rows, GRID_W, h, hd)
    kg = k.reshape(b, rows, GRID_W, h, hd)[:, row_idx]
    vg = v.reshape(b, rows, GRID_W, h, hd)[:, row_idx]
    scale = hd ** -0.5
    sc = jnp.einsum('brchd,brikhd->brhcik', qg, kg).astype(F32) * scale
    roff = row_idx - r[:, None] + (NA_WIN_ROWS - 1)
    coff = jnp.clip(kcol[None, :] - cq[:, None], -(kw - 1), kw - 1) + (kw - 1)
    bias = rpb.astype(F32)[:, roff[:, None, :, None], coff[None, :, None, :]]
    sc = sc + jnp.transpose(bias, (1, 0, 2, 3, 4))[None]
    sc = jnp.where(col_valid[None, None, None, :, None, :], sc, NEG_INF)
    sc = sc.reshape(b, rows, h, GRID_W, kh * GRID_W)
    s_ctx = jnp.einsum('brchd,blhd->brhcl', qg, kc).astype(F32) * scale
    p = jax.nn.softmax(jnp.concatenate([sc, s_ctx], axis=-1), axis=-1)
    n_loc = kh * GRID_W
    p_loc = p[..., :n_loc].reshape(b, rows, h, GRID_W, kh, GRID_W).astype(v.dtype)
    p_ctx = p[..., n_loc:].astype(v.dtype)
    o = (jnp.einsum('brhcik,brikhd->brchd', p_loc, vg)
         + jnp.einsum('brhcl,blhd->brchd', p_ctx, vc))
    return o.reshape(b, s, h * hd)


def conv_centred(x, w, bias):
    kk, ch = w.shape
    y = lax.conv_general_dilated(x, w.astype(x.dtype).reshape(kk, 1, ch), window_strides=(1,),
                                 padding=[(kk // 2, kk // 2)],
                                 dimension_numbers=('NWC', 'WIO', 'NWC'), feature_group_count=ch)
    return y + bias


def ssd_scan(x, dt, a, bm, cm, init_state, with_output):
    b, l, h, p = x.shape
    n = bm.shape[-1]
    nc = l // SSD_CHUNK
    q = SSD_CHUNK
    xdt = (x.astype(F32) * dt[..., None]).reshape(b, nc, q, h, p)
    bc = bm.astype(F32).reshape(b, nc, q, h, n)
    cc = cm.astype(F32).reshape(b, nc, q, h, n)
    acs = jnp.cumsum((dt * a).reshape(b, nc, q, h), axis=2)
    total = acs[:, :, -1]
    decay_to_end = jnp.exp(total[:, :, None, :] - acs)
    states = jnp.einsum('bcqhn,bcqh,bcqhp->bchpn', bc, decay_to_end, xdt)

    def step(carry, inp):
        st, tot = inp
        return carry * jnp.exp(tot)[:, :, None, None] + st, carry

    final, prev = lax.scan(step, init_state,
                           (jnp.transpose(states, (1, 0, 2, 3, 4)), jnp.transpose(total, (1, 0, 2))))
    if not with_output:
        return None, final
    prev = jnp.transpose(prev, (1, 0, 2, 3, 4))
    y_off = jnp.einsum('bcqhn,bchpn->bcqhp', cc, prev) * jnp.exp(acs)[..., None]
    seg = acs[:, :, :, None, :] - acs[:, :, None, :, :]
    lower = jnp.tril(jnp.ones((q, q), dtype=bool))
    lmat = jnp.exp(jnp.where(lower[None, None, :, :, None], seg, NEG_INF))
    scores = jnp.einsum('bcihn,bcjhn->bcijh', cc, bc) * lmat
    y_diag = jnp.einsum('bcijh,bcjhp->bcihp', scores, xdt)
    return (y_diag + y_off).reshape(b, l, h, p), final


def ssd_branch(u, conv_w, conv_b, dt_bias, a_log, d_skip, norm_g, init_f, init_b, with_output):
    b, l, _ = u.shape
    z, xbc, dt_raw = jnp.split(u, [SSD_INNER, SSD_INNER + SSD_CONV_DIM], axis=-1)
    xbc = jax.nn.silu(conv_centred(xbc, conv_w, conv_b))
    xs, bm, cm = jnp.split(xbc, [SSD_INNER, SSD_INNER + SSD_GROUPS * SSD_STATE], axis=-1)
    xs = xs.reshape(b, l, SSD_HEADS, SSD_HEAD_DIM)
    rep = SSD_HEADS // SSD_GROUPS
    bm = jnp.repeat(bm.reshape(b, l, SSD_GROUPS, SSD_STATE), rep, axis=2)
    cm = jnp.repeat(cm.reshape(b, l, SSD_GROUPS, SSD_STATE), rep, axis=2)
    dt = jax.nn.softplus(dt_raw.astype(F32).reshape(b, l, 2, SSD_HEADS) + dt_bias.astype(F32))
    a = -jnp.exp(a_log.astype(F32))
    if init_f is None:
        init_f = jnp.zeros((b, SSD_HEADS, SSD_HEAD_DIM, SSD_STATE), F32)
        init_b = jnp.zeros((b, SSD_HEADS, SSD_HEAD_DIM, SSD_STATE), F32)
    flip = lambda t: jnp.flip(t, axis=1)
    y_f, st_f = ssd_scan(xs, dt[:, :, 0], a[0], bm, cm, init_f, with_output)
    y_b, st_b = ssd_scan(flip(xs), flip(dt[:, :, 1]), a[1], flip(bm), flip(cm), init_b, with_output)
    if not with_output:
        return None, st_f, st_b
    y = y_f + flip(y_b) + d_skip.astype(F32)[:, None] * xs.astype(F32)
    y = y.reshape(b, l, SSD_INNER) * jax.nn.silu(z.astype(F32))
    return rmsnorm(y, norm_g).astype(u.dtype), st_f, st_b


def token_mixers(ux, uc, rows_pos, cols_pos, sink, conv_w, conv_b, dt_bias, a_log, d_skip,
                 norm_g, rpb, need_ctx):
    ax, bx, nx = jnp.split(ux, [A_IN, A_IN + SSD_IN], axis=-1)
    ac, bcx, nc = jnp.split(uc, [A_IN, A_IN + SSD_IN], axis=-1)
    a_split = [A_WIDTH, A_WIDTH + A_KV_HEADS * HEAD_DIM]
    qa, ka, va = [to_heads(t) for t in jnp.split(ax, a_split, axis=-1)]
    qa_c, ka_c, va_c = [to_heads(t) for t in jnp.split(ac, a_split, axis=-1)]
    qa = rope_2d(qa, rows_pos, cols_pos)
    ka = rope_2d(ka, rows_pos, cols_pos)
    oa = window_gqa(qa, ka, va, ka_c, va_c, sink)
    ob_c, st_f, st_b = ssd_branch(bcx, conv_w, conv_b, dt_bias, a_log, d_skip, norm_g, None, None, need_ctx)
    ob, _, _ = ssd_branch(bx, conv_w, conv_b, dt_bias, a_log, d_skip, norm_g, st_f, st_b, True)
    n_split = [NA_WIDTH, 2 * NA_WIDTH]
    qn, kn, vn = [to_heads(t) for t in jnp.split(nx, n_split, axis=-1)]
    qn_c, kn_c, vn_c = [to_heads(t) for t in jnp.split(nc, n_split, axis=-1)]
    on = neighborhood_attn(qn, kn, vn, kn_c, vn_c, rpb)
    o_x = jnp.concatenate([oa, ob, on], axis=-1)
    if not need_ctx:
        return o_x, None
    oa_c = dense_attn(qa_c, ka_c, va_c, sink)
    on_c = dense_attn(qn_c, kn_c, vn_c, None)
    o_c = jnp.concatenate([oa_c, ob_c, on_c], axis=-1)
    return o_x, o_c


def hier_moe(h, w_rg, b_rg, w_re, b_re, w_gate, w_up, w_down):
    n = h.shape[0]
    g_prob = jax.nn.softmax((h @ w_rg).astype(F32) + b_rg.astype(F32), axis=-1)
    g_w, g_idx = lax.top_k(g_prob, 1)
    e_logits = ((h @ w_re).astype(F32) + b_re.astype(F32)).reshape(n, MOE_GROUPS, MOE_EXPERTS)
    e_logits = jnp.take_along_axis(e_logits, g_idx[:, :, None], axis=1)[:, 0]
    top_l, top_i = lax.top_k(e_logits, MOE_TOPK)
    top_w = jax.nn.softmax(top_l, axis=-1) * g_w
    comb = jnp.einsum('nk,nke->ne', top_w, jax.nn.one_hot(top_i, MOE_EXPERTS, dtype=F32))
    out = jnp.zeros((n, h.shape[1]), F32)
    for gi in range(MOE_GROUPS):
        wsel = jnp.where(g_idx == gi, comb, 0.0)
        hid = (jax.nn.silu(jnp.einsum('nd,edf->nef', h, w_gate[gi]))
               * jnp.einsum('nd,edf->nef', h, w_up[gi]))
        out = out + jnp.einsum('nef,efd->nd', hid * wsel[..., None].astype(hid.dtype), w_down[gi])
    return out.astype(h.dtype)


def setup_inputs(seed: int = 0) -> dict:
    key = jax.random.key(seed)
    ks = jax.random.split(key, 28)

    def nrm(k, shape, scale):
        return jax.random.normal(k, shape, F32) * scale

    x = nrm(ks[0], (BATCH, SEQ, D_MODEL), 1.0)
    c = nrm(ks[1], (BATCH, D_MODEL), 1.0)
    ctx = nrm(ks[2], (BATCH, CTX_LEN, D_MODEL), 1.0)
    c_ctx = nrm(ks[3], (D_MODEL,), 1.0)
    w_mod = nrm(ks[4], (DEPTH, D_MODEL, N_MOD * D_MODEL), 0.5 * D_MODEL ** -0.5)
    b_mod = nrm(ks[5], (DEPTH, N_MOD * D_MODEL), 0.02)
    g_mix = 1.0 + nrm(ks[6], (DEPTH, D_MODEL), 0.02)
    w_in = nrm(ks[7], (DEPTH, D_MODEL, N_IN), D_MODEL ** -0.5)
    attn_sink = nrm(ks[8], (DEPTH, A_HEADS), 0.5)
    ssd_conv_w = nrm(ks[9], (DEPTH, SSD_CONV, SSD_CONV_DIM), SSD_CONV ** -0.5)
    ssd_conv_b = nrm(ks[10], (DEPTH, SSD_CONV_DIM), 0.02)
    dt0 = jnp.exp(jax.random.uniform(ks[11], (DEPTH, 2, SSD_HEADS), F32, math.log(1e-3), math.log(1e-1)))
    ssd_dt_bias = dt0 + jnp.log(-jnp.expm1(-dt0))
    ssd_a_log = jnp.log(jax.random.uniform(ks[12], (DEPTH, 2, SSD_HEADS), F32, 1.0, 16.0))
    ssd_d = 1.0 + nrm(ks[13], (DEPTH, SSD_HEADS), 0.02)
    ssd_norm_g = 1.0 + nrm(ks[14], (DEPTH, SSD_INNER), 0.02)
    na_rpb = nrm(ks[15], (DEPTH, NA_HEADS, 2 * NA_WIN_ROWS - 1, 2 * NA_WIN_COLS - 1), 0.1)
    w_out = nrm(ks[16], (DEPTH, D_MIX, D_MODEL), D_MIX ** -0.5)
    g_ffn = 1.0 + nrm(ks[17], (DEPTH, D_MODEL), 0.02)
    w_router_group = nrm(ks[18], (DEPTH, D_MODEL, MOE_GROUPS), D_MODEL ** -0.5)
    b_router_group = nrm(ks[19], (DEPTH, MOE_GROUPS), 0.01)
    w_router_expert = nrm(ks[20], (DEPTH, D_MODEL, MOE_GROUPS * MOE_EXPERTS), D_MODEL ** -0.5)
    b_router_expert = nrm(ks[21], (DEPTH, MOE_GROUPS * MOE_EXPERTS), 0.01)
    w_exp_gate = nrm(ks[22], (DEPTH, MOE_GROUPS, MOE_EXPERTS, D_MODEL, D_EXPERT), D_MODEL ** -0.5)
    w_exp_up = nrm(ks[23], (DEPTH, MOE_GROUPS, MOE_EXPERTS, D_MODEL, D_EXPERT), D_MODEL ** -0.5)
    w_exp_down = nrm(ks[24], (DEPTH, MOE_GROUPS, MOE_EXPERTS, D_EXPERT, D_MODEL), D_EXPERT ** -0.5)
    g_final = 1.0 + nrm(ks[25], (D_MODEL,), 0.02)
    return {'x': x, 'c': c, 'ctx': ctx, 'c_ctx': c_ctx, 'w_mod': w_mod, 'b_mod': b_mod,
            'g_mix': g_mix, 'w_in': w_in, 'attn_sink': attn_sink, 'ssd_conv_w': ssd_conv_w,
            'ssd_conv_b': ssd_conv_b, 'ssd_dt_bias': ssd_dt_bias, 'ssd_a_log': ssd_a_log,
            'ssd_d': ssd_d, 'ssd_norm_g': ssd_norm_g, 'na_rpb': na_rpb, 'w_out': w_out,
            'g_ffn': g_ffn, 'w_router_group': w_router_group, 'b_router_group': b_router_group,
            'w_router_expert': w_router_expert, 'b_router_expert': b_router_expert,
            'w_exp_gate': w_exp_gate, 'w_exp_up': w_exp_up, 'w_exp_down': w_exp_down,
            'g_final': g_final}


def reference(x, c, ctx, c_ctx, w_mod, b_mod, g_mix, w_in, attn_sink, ssd_conv_w, ssd_conv_b,
              ssd_dt_bias, ssd_a_log, ssd_d, ssd_norm_g, na_rpb, w_out, g_ffn, w_router_group,
              b_router_group, w_router_expert, b_router_expert, w_exp_gate, w_exp_up, w_exp_down,
              g_final):
    b, s, d = x.shape
    l_ctx = ctx.shape[1]
    t = jnp.arange(s)
    rows_pos = t // GRID_W
    cols_pos = t % GRID_W
    mod_x_in = jax.nn.silu(c)
    mod_c_in = jax.nn.silu(c_ctx)
    for layer in range(DEPTH):
        need_ctx = layer < DEPTH - 1
        mx = (mod_x_in @ w_mod[layer] + b_mod[layer]).reshape(b, N_MOD, 1, d)
        mc = (mod_c_in @ w_mod[layer] + b_mod[layer]).reshape(N_MOD, d)
        hx = modulate(rmsnorm(x, g_mix[layer]), mx[:, 0], mx[:, 1])
        hc = modulate(rmsnorm(ctx, g_mix[layer]), mc[0], mc[1])
        ux = hx @ w_in[layer]
        uc = hc @ w_in[layer]
        o_x, o_c = token_mixers(ux, uc, rows_pos, cols_pos, attn_sink[layer], ssd_conv_w[layer],
                                ssd_conv_b[layer], ssd_dt_bias[layer], ssd_a_log[layer], ssd_d[layer],
                                ssd_norm_g[layer], na_rpb[layer], need_ctx)
        x = x + mx[:, 2] * (o_x @ w_out[layer])
        tokens = modulate(rmsnorm(x, g_ffn[layer]), mx[:, 3], mx[:, 4]).reshape(b * s, d)
        if need_ctx:
            ctx = ctx + mc[2] * (o_c @ w_out[layer])
            h2c = modulate(rmsnorm(ctx, g_ffn[layer]), mc[3], mc[4]).reshape(b * l_ctx, d)
            tokens = jnp.concatenate([tokens, h2c], axis=0)
        f = hier_moe(tokens, w_router_group[layer], b_router_group[layer], w_router_expert[layer],
                     b_router_expert[layer], w_exp_gate[layer], w_exp_up[layer], w_exp_down[layer])
        x = x + mx[:, 5] * f[: b * s].reshape(b, s, d)
        if need_ctx:
            ctx = ctx + mc[5] * f[b * s:].reshape(b, l_ctx, d)
    return rmsnorm(x, g_final)
```

```python
import contextlib
import math
import numpy as np
import concourse.bass as bass
import concourse.mybir as mybir
from concourse.bass_utils import run_bass_kernel_spmd

F32 = mybir.dt.float32
BF16 = mybir.dt.bfloat16
AF = mybir.ActivationFunctionType
ALU = mybir.AluOpType

EPOCH = 24000
NEG = -30000.0
D = 1024
NLAT = 16
NT = 18
TOK = NT * 128
DEPTH = 4
REGN = 37632


class Res:
    __slots__ = ("name", "w", "r", "psum")

    def __init__(self, name="", psum=False):
        self.name = name
        self.w = None
        self.r = []
        self.psum = psum


class Sched:
    ENGS = ("pe", "act", "dve", "pool", "sp")

    def __init__(self, nc):
        self.nc = nc
        self.streams = {e: [] for e in self.ENGS}
        self.cnt = {e: 0 for e in self.ENGS}
        self.waited = {e: {} for e in self.ENGS}
        self.semkeys = []
        self.semset = set()
        self.dmacnt = {}
        self.last = {}

    def _need(self, key):
        if key not in self.semset:
            self.semset.add(key)
            self.semkeys.append(key)

    def _wait(self, eng, k, v):
        if k[0] == eng and eng == "pe":
            return
        wd = self.waited[eng]
        if wd.get(k, 0) < v:
            wd[k] = v
            self.streams[eng].append(("wait", k, v))

    def _deps(self, eng, reads, writes):
        for r in reads:
            if r.w is not None:
                self._wait(eng, *r.w)
        for w in writes:
            if w.w is not None:
                self._wait(eng, *w.w)
            for t in w.r:
                self._wait(eng, *t)

    def _post(self, tok, reads, writes):
        self.last[tok[0]] = tok[1]
        for r in reads:
            r.r.append(tok)
            if len(r.r) > 64:
                best = {}
                for (k, v) in r.r:
                    if best.get(k, 0) < v:
                        best[k] = v
                r.r = list(best.items())
        for w in writes:
            w.w = tok
            w.r = []

    def op(self, eng, fn, reads=(), writes=()):
        if eng != "pe":
            ex = [r for r in reads if r.psum]
            if ex:
                writes = list(writes) + ex
        self._deps(eng, reads, writes)
        c = self.cnt[eng]
        key = (eng, c // EPOCH)
        val = c % EPOCH + 1
        self._need(key)
        self.cnt[eng] = c + 1
        self.streams[eng].append(("op", fn, key, 1))
        tok = (key, val)
        self._post(tok, reads, writes)
        return tok

    def dma(self, eng, fns, semname, reads=(), writes=()):
        self._deps(eng, reads, writes)
        key = ("dma", semname)
        self._need(key)
        c = self.dmacnt.get(key, 0)
        for fn in fns:
            self.streams[eng].append(("op", fn, key, 16))
            c += 16
        self.dmacnt[key] = c
        tok = (key, c)
        self._post(tok, reads, writes)
        return tok

    def barrier(self):
        toks = list(self.last.items())
        for eng in self.ENGS:
            for (k, v) in toks:
                self._wait(eng, k, v)

    def emit(self):
        nc = self.nc
        with contextlib.ExitStack() as es:
            sems = {}
            for i, k in enumerate(self.semkeys):
                sems[k] = es.enter_context(nc.semaphore("s%d" % i))
            block = es.enter_context(nc.Block())

            def runner(stream):
                def f(e):
                    for it in stream:
                        if it[0] == "wait":
                            e.wait_ge(sems[it[1]], it[2])
                        else:
                            it[1](e).then_inc(sems[it[2]], it[3])
                return f

            block.tensor(runner(self.streams["pe"]))
            block.scalar(runner(self.streams["act"]))
            block.vector(runner(self.streams["dve"]))
            block.gpsimd(runner(self.streams["pool"]))
            block.sync(runner(self.streams["sp"]))


def _rope_tables():
    t = np.arange(2048)
    rows = (t // 64).astype(np.float64)
    cols = (t % 64).astype(np.float64)
    inv = 1.0 / (10000.0 ** (np.arange(0, 32, 2, dtype=np.float64) / 32.0))
    C = np.zeros((64, 2048), np.float64)
    S = np.zeros((64, 2048), np.float64)
    ar = rows[None, :] * inv[:, None]
    ac = cols[None, :] * inv[:, None]
    C[0:16] = np.cos(ar); C[16:32] = np.cos(ar); C[32:48] = np.cos(ac); C[48:64] = np.cos(ac)
    S[0:16] = -np.sin(ar); S[16:32] = np.sin(ar); S[32:48] = -np.sin(ac); S[48:64] = np.sin(ac)
    C = np.concatenate([C, C], 0).astype(np.float32)
    S = np.concatenate([S, S], 0).astype(np.float32)
    return C, S


def _swap_idx():
    i = np.arange(64)
    return np.where(i < 16, i + 16, np.where(i < 32, i - 16, np.where(i < 48, i + 16, i - 16)))


def _const_tables():
    k = np.arange(128)[:, None]
    i = np.arange(128)[None, :]
    c = {}
    c["ident"] = np.eye(128, dtype=np.float32)
    c["triU"] = (k <= i).astype(np.float32)
    c["triL"] = (k >= i).astype(np.float32)
    c["maskF"] = np.where(k <= i, 0.0, NEG).astype(np.float32)
    c["maskB"] = np.where(k >= i, 0.0, NEG).astype(np.float32)
    mA = np.zeros((128, 3, 128), np.float32)
    mA[:, 0, :] = np.where(i <= k, 0.0, NEG)
    mA[:, 2, :] = np.where(k <= i, 0.0, NEG)
    c["maskA"] = mA
    C, S = _rope_tables()
    c["ropeC"] = C
    c["ropeS"] = S
    return c


def _na_table(rpb):
    kr = (np.arange(128) // 64)[:, None]
    kc = (np.arange(128) % 64)[:, None]
    qr = (np.arange(128) // 64)[None, :]
    qc = (np.arange(128) % 64)[None, :]
    cs = np.clip(qc - 8, 0, 48)
    colv = (kc >= cs) & (kc < cs + 16)
    coff = np.clip(kc - qc, -15, 15) + 15
    out = np.full((128, 4, 12, 128), NEG, np.float32)
    for v in range(12):
        if v < 5:
            dj = v - 2
            dr = 2 * dj + kr - qr
            rowv = (dr >= -4) & (dr <= 3)
        else:
            dj = v - 5 - 3
            dr = 2 * dj + kr - qr
            rowv = np.abs(dr) <= 7
        valid = rowv & colv
        drc = np.clip(dr + 7, 0, 14)
        for h in range(4):
            g = rpb[h][drc, coff]
            out[:, h, v, :] = np.where(valid, g, np.float32(NEG))
    return out


def _na_keys(t):
    if t < 2:
        return [(j, 5 + (j - t) + 3) for j in range(0, 4)]
    if t >= 14:
        return [(j, 5 + (j - t) + 3) for j in range(12, 16)]
    return [(j, (j - t) + 2) for j in range(t - 2, t + 3)]


def build_program(cfg):
    nlayers = cfg.get("nlayers", DEPTH)
    phases = cfg.get("phases", "ANSM")
    final_norm = cfg.get("final_norm", True)

    nc = bass.Bass("TRN2", target_bir_lowering=False)

    def din(name, shape):
        return nc.dram_tensor(name, list(shape), F32, kind="ExternalInput").ap()

    x_d = din("x", [2048, D])
    ctx_d = din("ctx", [256, D])
    scT_d = din("scT", [128, 16])
    w_mod_d = din("w_mod", [DEPTH, D, 6 * D])
    b_modT_d = din("b_modT", [DEPTH, 128, 48])
    b_mod_d = din("b_mod", [DEPTH, 6 * D])
    g_mixT_d = din("g_mixT", [DEPTH, 128, 8])
    g_ffnT_d = din("g_ffnT", [DEPTH, 128, 8])
    g_final_d = din("g_final", [1, D])
    w_inA_d = din("w_inA", [DEPTH, D, 896])
    w_in_d = din("w_in", [DEPTH, D, 2576])
    w_outP_d = din("w_outP", [DEPTH, D, D])
    sinkE_d = din("sinkE", [DEPTH, 128, 2])
    convw_d = din("convw", [DEPTH, 128, 30])
    convb_d = din("convb", [DEPTH, 128, 6])
    dtb_d = din("dtb", [DEPTH, 16])
    alog_d = din("alog", [DEPTH, 16])
    ssdd_d = din("ssdd", [DEPTH, 8])
    normgT_d = din("normgT", [DEPTH, 128, 4])
    naTab_d = din("naTab", [DEPTH, 128, 6144])
    w_rt_d = din("w_rt", [DEPTH, D, 36])
    b_rt_d = din("b_rt", [DEPTH, 36])
    wg_d = din("w_exp_gate", [DEPTH, 32, D, 256])
    wu_d = din("w_exp_up", [DEPTH, 32, D, 256])
    wd_d = din("w_exp_down", [DEPTH, 32, 256, D])
    ident_d = din("ident", [128, 128])
    triU_d = din("triU", [128, 128])
    triL_d = din("triL", [128, 128])
    maskF_d = din("maskF", [128, 128])
    maskB_d = din("maskB", [128, 128])
    maskA_d = din("maskA", [128, 384])
    ropeC_d = din("ropeC", [128, 2048])
    ropeS_d = din("ropeS", [128, 2048])
    out_d = nc.dram_tensor("out", [2048, D], F32, kind="ExternalOutput").ap()
    dbg_d = None
    if cfg.get("dump_ctx"):
        dbg_d = nc.dram_tensor("out_ctx", [256, D], F32, kind="ExternalOutput").ap()

    es = contextlib.ExitStack()
    with es:
        def sb(name, shape, dt):
            return es.enter_context(nc.sbuf_tensor("sb_" + name, list(shape), dt))

        S = Sched(nc)

        X = sb("X", [128, NT, D], F32)
        HT = sb("HT", [128, 8, TOK], BF16)
        REG = sb("REG", [128, REGN], BF16)
        GATE = sb("GATE", [128, 4, D], F32)
        ident_f = sb("ident_f", [128, 128], F32)
        ident_b = sb("ident_b", [128, 128], BF16)
        triU = sb("triU", [128, 128], F32)
        triL = sb("triL", [128, 128], F32)
        maskF = sb("maskF", [128, 128], F32)
        maskB = sb("maskB", [128, 128], F32)
        maskA = sb("maskA", [128, 3, 128], BF16)
        ones_f = sb("ones_f", [128, 128], F32)
        ones_b = sb("ones_b", [128, 128], BF16)
        scT = sb("scT", [128, 16], F32)
        sc_b = sb("sc_b", [128, 8, 2], BF16)
        sc_rep = sb("sc_rep", [128, 2, 8, 128], BF16)
        modT = sb("modT", [128, 48, 2], F32)
        bmT = sb("bmT", [128, 48], F32)
        gT = sb("gT", [128, 16], F32)
        AV = sb("AV", [128, 2, 2, 8], F32)
        SV = sb("SV", [128, 2, 2, 8], F32)
        small = sb("small", [128, 64], F32)

        banks = [es.enter_context(nc.psum_tensor("bank%d" % i, [128, 512], F32)) for i in range(8)]
        RB = [Res("bank%d" % i, psum=True) for i in range(8)]

        RX = [Res("X%d" % t) for t in range(NT)]
        RH = [Res("H%d" % t) for t in range(NT)]
        Rc = {}

        def R(name):
            if name not in Rc:
                Rc[name] = Res(name)
            return Rc[name]

        class Carver:
            def __init__(self, base, nbytes):
                self.base = base
                self.off = 0
                self.nbytes = nbytes

            def take(self, shape, dt):
                esz = 2 if dt == BF16 else 4
                n = int(np.prod(shape[1:]))
                nb = n * esz
                nb_al = (nb + 63) // 64 * 64
                assert self.off + nb_al <= self.nbytes, ("region overflow", self.off, nb_al, self.nbytes)
                a = self.base[:, self.off // 2:(self.off + nb) // 2]
                self.off += nb_al
                if dt != BF16:
                    a = a.bitcast(dt)
                if len(shape) > 2:
                    names = " ".join("d%d" % i for i in range(1, len(shape)))
                    kw = {"d%d" % i: shape[i] for i in range(1, len(shape))}
                    a = a.rearrange("p (%s) -> p %s" % (names, names), **kw)
                return a

        def region():
            return Carver(REG, REGN * 2)

        def ht_region():
            return Carver(HT[:].rearrange("p k t -> p (k t)"), 8 * TOK * 2)

        def mm(out, lhsT, rhs, start, stop, reads, writes, sgc=False):
            if sgc:
                S.op("pe", lambda e: e.matmul(out, lhsT=lhsT, rhs=rhs, start=start, stop=stop, skip_group_check=True), reads, writes)
            else:
                S.op("pe", lambda e: e.matmul(out, lhsT=lhsT, rhs=rhs, start=start, stop=stop), reads, writes)

        def tr(out, in_, ident, reads, writes):
            S.op("pe", lambda e: e.transpose(out=out, in_=in_, identity=ident), reads, writes)

        def act(out, in_, func, reads, writes, bias=None, scale=None, accum_out=None):
            kw = {}
            if bias is not None:
                kw["bias"] = bias
            if scale is not None:
                kw["scale"] = scale
            if accum_out is not None:
                kw["accum_out"] = accum_out
            S.op("act", lambda e: e.activation(out=out, in_=in_, func=func, **kw), reads, writes)

        def tt(eng, out, in0, in1, op, reads, writes):
            S.op(eng, lambda e: e.tensor_tensor(out=out, in0=in0, in1=in1, op=op), reads, writes)

        def ts(eng, out, in0, s1, op0, reads, writes, s2=None, op1=None):
            if op1 is None:
                S.op(eng, lambda e: e.tensor_scalar(out=out, in0=in0, scalar1=s1, scalar2=None, op0=op0), reads, writes)
            else:
                S.op(eng, lambda e: e.tensor_scalar(out=out, in0=in0, scalar1=s1, scalar2=s2, op0=op0, op1=op1), reads, writes)

        def stt(out, in0, scalar, in1, op0, op1, reads, writes):
            S.op("dve", lambda e: e.scalar_tensor_tensor(out=out, in0=in0, scalar=scalar, in1=in1, op0=op0, op1=op1), reads, writes)

        def cp(eng, out, in_, reads, writes):
            if eng == "act_copy":
                S.op("act", lambda e: e.activation(out=out, in_=in_, func=AF.Copy), reads, writes)
            else:
                S.op(eng, lambda e: e.tensor_copy(out=out, in_=in_), reads, writes)

        def memset(eng, ap, val, writes):
            S.op(eng, lambda e: e.memset(ap, val), (), writes)

        dma_ctr = [0]

        def load(q, out, in_, writes, reads=(), sem=None):
            if sem is None:
                sem = "u%d" % (dma_ctr[0] % 40)
                dma_ctr[0] += 1
            S.dma(q, [lambda e: e.dma_start(out=out, in_=in_)], sem, reads, writes)

        def wview(w2d, ncols_lo, ncols_hi):
            return w2d[:, ncols_lo:ncols_hi].rearrange("(k p) n -> p k n", p=128)

        xv = x_d.rearrange("(t p) d -> p t d", p=128)
        cv = ctx_d.rearrange("(t p) d -> p t d", p=128)
        for t in range(NLAT):
            load("sp", X[:, t, :], xv[:, t, :], [RX[t]])
        for t in range(2):
            load("sp", X[:, NLAT + t, :], cv[:, t, :], [RX[NLAT + t]])
        load("sp", ident_f[:], ident_d, [R("ident_f")])
        load("pool", ident_b[:], ident_d, [R("ident_b")])
        load("sp", triU[:], triU_d, [R("triU")])
        load("sp", triL[:], triL_d, [R("triL")])
        load("sp", maskF[:], maskF_d, [R("maskF")])
        load("sp", maskB[:], maskB_d, [R("maskB")])
        load("pool", maskA[:].rearrange("p a b -> p (a b)"), maskA_d, [R("maskA")])
        load("sp", scT[:], scT_d, [R("scT")])
        memset("dve", ones_f[:], 1.0, [R("ones_f")])
        memset("dve", ones_b[:], 1.0, [R("ones_b")])
        act(scT[:], scT[:], AF.Silu, [R("scT")], [R("scT")])
        cp("dve", sc_b[:].rearrange("p k j -> p (k j)"), scT[:], [R("scT")], [R("sc_b")])
        for j in range(2):
            for k in range(8):
                ts("dve", sc_rep[:, j, k, :], ones_f[:], scT[:, 2 * k + j:2 * k + j + 1], ALU.mult,
                   [R("scT"), R("ones_f")], [R("sc_rep")])

        def mod_phase(l):
            S.barrier()
            rg = region()
            wb = [rg.take([128, 8, 512], BF16) for _ in range(3)]
            Rw = [Res("modw%d" % i) for i in range(3)]
            brow = rg.take([128, 4, 512], F32)
            load("sp", bmT[:], b_modT_d[l], [R("bmT")])
            load("sp", gT[:, 0:8], g_mixT_d[l], [R("gT")])
            load("sp", gT[:, 8:16], g_ffnT_d[l], [R("gT")])
            gate_chunks = {4: (0, 0), 5: (0, 1), 10: (1, 0), 11: (1, 1)}
            gi = 0
            for ch in range(12):
                i = ch % 3
                load("pool", wb[i], wview(w_mod_d[l], ch * 512, ch * 512 + 512), [Rw[i]], sem="modw%d" % i)
                if ch in gate_chunks:
                    g, half = gate_chunks[ch]
                    load("sp", brow[:, gi, :], b_mod_d[l:l + 1, ch * 512:ch * 512 + 512].partition_broadcast(128),
                         [R("brow")])
                    for j in range(2):
                        bk = banks[j]
                        for k in range(8):
                            mm(bk[:, :], sc_rep[:, j, k, :], wb[i][:, k, :], k == 0, k == 7,
                               [R("sc_rep"), Rw[i]], [RB[j]])
                        tt("dve", GATE[:, 2 * g + j, half * 512:half * 512 + 512], bk[:, :], brow[:, gi, :], ALU.add,
                           [RB[j], R("brow")], [R("GATE")])
                    gi += 1
                else:
                    for sub in range(4):
                        jn = ch * 4 + sub
                        for k in range(8):
                            mm(banks[2][:, 2 * jn:2 * jn + 2], wb[i][:, k, sub * 128:sub * 128 + 128], sc_b[:, k, :],
                               k == 0, k == 7, [R("sc_b"), Rw[i]], [RB[2]])
            mp = banks[2][:, 0:96].rearrange("p (j c) -> p j c", c=2)
            for j in range(2):
                tt("dve", modT[:, :, j], mp[:, :, j], bmT[:], ALU.add, [RB[2], R("bmT")], [R("modT")])
            for n in range(2):
                base = 0 if n == 0 else 24
                for j in range(2):
                    stt(AV[:, n, j, :], modT[:, base + 8:base + 16, j], 1.0, gT[:, 8 * n:8 * n + 8], ALU.add, ALU.mult,
                        [R("modT"), R("gT")], [R("AV")])
                    cp("dve", SV[:, n, j, :], modT[:, base:base + 8, j], [R("modT")], [R("SV")])

        def norm_phase(n, tiles, router=None, rg=None):
            if rg is None:
                rg = region()
            junk = rg.take([128, D], BF16)
            XN = rg.take([128, 2, D], F32)
            HF = rg.take([128, 2, 8, 128], F32)
            for idx, t in enumerate(tiles):
                j = 0 if t < NLAT else 1
                b = idx % 2
                rxn, rhf = R("XN%d" % b), R("HF%d" % b)
                act(junk[:], X[:, t, :], AF.Square, [RX[t]], [R("junk"), R("ss%d" % t)], accum_out=small[:, t:t + 1])
                act(small[:, 32 + t:33 + t], small[:, t:t + 1], AF.Ln, [R("ss%d" % t)], [R("rs%d" % t)], bias=1e-6, scale=1.0 / D)
                act(small[:, 32 + t:33 + t], small[:, 32 + t:33 + t], AF.Exp, [R("rs%d" % t)], [R("rs%d" % t)], scale=-0.5)
                ts("dve", XN[:, b, :], X[:, t, :], small[:, 32 + t:33 + t], ALU.mult, [RX[t], R("rs%d" % t)], [rxn])
                pb = [banks[2 * b], banks[2 * b + 1]]
                for k in range(8):
                    bk = pb[k // 4]
                    mm(bk[:, (k % 4) * 128:(k % 4) * 128 + 128], XN[:, b, k * 128:k * 128 + 128], ident_f[:], True, True,
                       [rxn, R("ident_f")], [RB[2 * b + k // 4]])
                for k in range(8):
                    bk = pb[k // 4]
                    act(HF[:, b, k, :], bk[:, (k % 4) * 128:(k % 4) * 128 + 128], AF.Identity,
                        [RB[2 * b + k // 4], R("AV"), R("SV")], [rhf],
                        bias=SV[:, n, j, k:k + 1], scale=AV[:, n, j, k:k + 1])
                cp("pool", HT[:, :, t * 128:t * 128 + 128], HF[:, b, :, :], [rhf], [RH[t]])
                if router is not None:
                    router(t, HF[:, b, :, :], rhf)

        def x_update(t, ps_lo, ps_hi, rb_lo, rb_hi, g, tmp, rtmp):
            for half, (ps, rb) in enumerate(((ps_lo, rb_lo), (ps_hi, rb_hi))):
                sl = slice(half * 512, half * 512 + 512)
                tt("dve", tmp[:, sl], ps, GATE[:, g, sl], ALU.mult, [rb, R("GATE")], [rtmp])
                tt("dve", X[:, t, sl], X[:, t, sl], tmp[:, sl], ALU.add, [rtmp, RX[t]], [RX[t]])

        def proj_fm(dst, rdst, wt, rw, col0, tiles_blocks, evac):
            for bi, (t0, ntile) in enumerate(tiles_blocks):
                bk = banks[bi % 2]
                n = ntile * 128
                for k in range(8):
                    mm(bk[:, 0:n], wt[:, k, col0:col0 + 128], HT[:, k, t0 * 128:t0 * 128 + n], k == 0, k == 7,
                       [rw] + RH[t0:t0 + ntile], [RB[bi % 2]])
                evac(bk[:, 0:n], RB[bi % 2], t0, n)

        BLOCKS = [(0, 4), (4, 4), (8, 4), (12, 4), (16, 2)]

        def attn_phase(kind, l, need_ctx):
            S.barrier()
            rg = region()
            if kind == "A":
                ncolw = 896
                wt = rg.take([128, 8, ncolw], BF16)
                load("pool", wt, wview(w_inA_d[l], 0, 896), [R("wt")], sem="wt")
                ropeC = rg.take([128, 2048], F32)
                ropeS = rg.take([128, 2048], F32)
                load("sp", ropeC, ropeC_d, [R("ropeC")])
                load("sp", ropeS, ropeS_d, [R("ropeS")])
                nq = 2
                Qs = [rg.take([128, TOK], BF16) for _ in range(2)]
                Ks = [rg.take([128, TOK], BF16)]
                Ks = [Ks[0], Ks[0]]
                vcols = 128
                Vtm = rg.take([128, NT, vcols], BF16)
                t1 = rg.take([128, 512], F32)
                t2 = rg.take([128, 512], F32)
                wo = rg.take([128, 2, D], BF16)
                load("pool", wo, w_outP_d[l][0:256, :].rearrange("(k p) n -> p k n", p=128), [R("wo")], sem="wo")
                esink = rg.take([128, 2], F32)
                load("sp", esink, sinkE_d[l], [R("esink")])
                act(esink, esink, AF.Exp, [R("esink")], [R("esink")])
                scale = 0.125
                nkmax = 5
                for ci, (dst, rn) in enumerate(((Qs[0], "Q0"), (Qs[1], "Q1"), (Ks[0], "K0"))):
                    for bi, (t0, ntile) in enumerate(BLOCKS):
                        n = ntile * 128
                        b0, b1 = banks[2 * (bi % 2)], banks[2 * (bi % 2) + 1]
                        r0, r1 = RB[2 * (bi % 2)], RB[2 * (bi % 2) + 1]
                        for k in range(8):
                            mm(b0[:, 0:n], wt[:, k, ci * 128:ci * 128 + 128], HT[:, k, t0 * 128:t0 * 128 + n], k == 0, k == 7,
                               [R("wt")] + RH[t0:t0 + ntile], [r0])
                        if t0 < NLAT:
                            for k in range(8):
                                mm(b1[:, 0:n], wt[:, k, (ci + 3) * 128:(ci + 3) * 128 + 128], HT[:, k, t0 * 128:t0 * 128 + n],
                                   k == 0, k == 7, [R("wt")] + RH[t0:t0 + ntile], [r1])
                            tt("dve", t1[:, 0:n], b0[:, 0:n], ropeC[:, t0 * 128:t0 * 128 + n], ALU.mult, [r0, R("ropeC")], [R("t1")])
                            tt("dve", t2[:, 0:n], b1[:, 0:n], ropeS[:, t0 * 128:t0 * 128 + n], ALU.mult, [r1, R("ropeS")], [R("t2")])
                            tt("pool", dst[:, t0 * 128:t0 * 128 + n], t1[:, 0:n], t2[:, 0:n], ALU.add, [R("t1"), R("t2")], [R(rn)])
                        else:
                            act(dst[:, t0 * 128:t0 * 128 + n], b0[:, 0:n], AF.Copy, [r0], [R(rn)])
                Rc["K1"] = Rc["K0"]
                vcol0 = 768
            else:
                wt = rg.take([128, 8, 768], BF16)
                load("pool", wt, wview(w_in_d[l], 1808, 2576), [R("wt")], sem="wt")
                tab = rg.take([128, 4, 12, 128], BF16)
                load("pool", tab[:].rearrange("p a b c -> p (a b c)"), naTab_d[l], [R("biasT")], sem="tab")
                Qs = [rg.take([128, TOK], BF16) for _ in range(2)]
                Ks = [rg.take([128, TOK], BF16) for _ in range(2)]
                vcols = 256
                Vtm = rg.take([128, NT, vcols], BF16)
                wo = rg.take([128, 2, D], BF16)
                load("pool", wo, w_outP_d[l][768:1024, :].rearrange("(k p) n -> p k n", p=128), [R("wo")], sem="wo")
                scale = 1.0
                nkmax = 7
                for ci, (dst, rn, sc_) in enumerate(((Qs[0], "Q0", 0.125), (Qs[1], "Q1", 0.125), (Ks[0], "K0", 1.0), (Ks[1], "K1", 1.0))):
                    for bi, (t0, ntile) in enumerate(BLOCKS):
                        n = ntile * 128
                        b0, r0 = banks[bi % 4], RB[bi % 4]
                        for k in range(8):
                            mm(b0[:, 0:n], wt[:, k, ci * 128:ci * 128 + 128], HT[:, k, t0 * 128:t0 * 128 + n], k == 0, k == 7,
                               [R("wt")] + RH[t0:t0 + ntile], [r0])
                        ts("dve", dst[:, t0 * 128:t0 * 128 + n], b0[:, 0:n], sc_, ALU.mult, [r0], [R(rn)])
                vcol0 = 512
            for t in range(NT):
                bk, rb = banks[4 + t % 4], RB[4 + t % 4]
                for k in range(8):
                    mm(bk[:, 0:vcols], HT[:, k, t * 128:t * 128 + 128], wt[:, k, vcol0:vcol0 + vcols], k == 0, k == 7,
                       [R("wt"), RH[t]], [rb])
                cp("dve", Vtm[:, t, :], bk[:, 0:vcols], [rb], [R("V")])

            PT = [rg.take([128, nkmax * 128], BF16) for _ in range(2)]
            RPT = [Res("PT%d" % i) for i in range(2)]
            OT = [rg.take([128, 128], BF16) for _ in range(4)]
            ROT = [Res("OT%d" % i) for i in range(4)]
            RDn = [rg.take([128, 128], F32) for _ in range(2)]
            RRD = [Res("RD%d" % i) for i in range(2)]
            tmp = rg.take([128, D], F32)
            rtmp = Res("updtmp")

            def keys_of(t):
                if t >= NLAT:
                    return [(16, None), (17, None)]
                if kind == "A":
                    ks = [(j, j - t + 1) for j in (t - 1, t, t + 1) if 0 <= j < NLAT]
                    ks = [(j, (b if b != 1 else None)) for (j, b) in ks]
                else:
                    ks = _na_keys(t)
                return ks + [(16, None), (17, None)]

            def bias_ap(pr, half, bid):
                if kind == "A":
                    return maskA[:, bid, :]
                return tab[:, 2 * pr + half, bid, :]

            qtiles = list(range(NLAT)) + ([16, 17] if need_ctx else [])
            work = [(t, pr, half) for t in qtiles for pr in range(2) for half in range(2)]

            def scores(i):
                t, pr, half = work[i]
                keys = keys_of(t)
                ps_ = slice(64 * half, 64 * half + 64)
                for kk, (j, bid) in enumerate(keys):
                    col = kk * 128
                    bi_ = 2 * (i % 2) + col // 512
                    c0 = col % 512
                    bk, rb = banks[bi_], RB[bi_]
                    has_b = bid is not None
                    mm(bk[:, c0:c0 + 128], Ks[pr][ps_, j * 128:j * 128 + 128], Qs[pr][ps_, t * 128:t * 128 + 128],
                       True, not has_b, [R("K%d" % pr), R("Q%d" % pr)], [rb])
                    if has_b:
                        mm(bk[:, c0:c0 + 128], ident_b[:], bias_ap(pr, half, bid), False, True,
                           [R("ident_b"), R("biasT"), R("maskA")], [rb])

            def expo(i):
                t, pr, half = work[i]
                ncol = len(keys_of(t)) * 128
                for q in range((ncol + 511) // 512):
                    n = min(512, ncol - q * 512)
                    bi_ = 2 * (i % 2) + q
                    act(PT[i % 2][:, q * 512:q * 512 + n], banks[bi_][:, 0:n], AF.Exp, [RB[bi_]], [RPT[i % 2]], scale=scale)

            def pv(i):
                t, pr, half = work[i]
                ip = i // 2
                keys = keys_of(t)
                nk = len(keys)
                ob, rob = banks[4 + (ip % 2)], RB[4 + (ip % 2)]
                ps_ = slice(64 * half, 64 * half + 64)
                if kind == "A":
                    vsl = slice(64 * half, 64 * half + 64)
                else:
                    h = 2 * pr + half
                    vsl = slice(64 * h, 64 * h + 64)
                for kk, (j, bid) in enumerate(keys):
                    p_ = PT[i % 2][:, kk * 128:kk * 128 + 128]
                    mm(ob[ps_, 0:128], Vtm[:, j, vsl], p_, kk == 0, kk == nk - 1, [R("V"), RPT[i % 2]], [rob])
                for kk, (j, bid) in enumerate(keys):
                    p_ = PT[i % 2][:, kk * 128:kk * 128 + 128]
                    mm(ob[ps_, 128:256], ones_b[:, 0:64], p_, kk == 0, kk == nk - 1, [R("ones_b"), RPT[i % 2]], [rob])
                if half == 0:
                    return
                rd, rrd = RDn[ip % 2], RRD[ip % 2]
                if kind == "A":
                    ts("dve", rd, ob[:, 128:256], esink[:, pr:pr + 1], ALU.add, [rob, R("esink")], [rrd])
                    S.op("dve", lambda e: e.reciprocal(out=rd, in_=rd), [rrd], [rrd])
                else:
                    S.op("dve", lambda e: e.reciprocal(out=rd, in_=ob[:, 128:256]), [rob], [rrd])
                tt("dve", OT[ip % 4], ob[:, 0:128], rd, ALU.mult, [rob, rrd], [ROT[ip % 4]])
                if pr == 0:
                    return
                j = 0 if t < NLAT else 1
                o0, o1 = OT[(ip - 1) % 4], OT[ip % 4]
                ro0, ro1 = ROT[(ip - 1) % 4], ROT[ip % 4]
                for hf in range(2):
                    bk, rb = banks[6 + hf], RB[6 + hf]
                    sl = slice(hf * 512, hf * 512 + 512)
                    mm(bk[:, :], o0, wo[:, 0, sl], True, False, [ro0, R("wo")], [rb])
                    mm(bk[:, :], o1, wo[:, 1, sl], False, True, [ro1, R("wo")], [rb])
                x_update(t, banks[6][:, :], banks[7][:, :], RB[6], RB[7], 0 + j, tmp, rtmp)

            n = len(work)
            scores(0)
            for i in range(n):
                expo(i)
                if i + 1 < n:
                    scores(i + 1)
                pv(i)

        def bc_mid(ap2d, n):
            return ap2d.unsqueeze(1).to_broadcast([128, n, ap2d.shape[1]])

        def bc_last(ap2d, n):
            return ap2d.unsqueeze(2).to_broadcast([128, ap2d.shape[1], n])

        def ssd_phase(l, need_ctx):
            S.barrier()
            rg = region()
            xs_tm = rg.take([128, NT, 512], BF16)
            B_tm = rg.take([128, NT, 128], BF16)
            BT = rg.take([128, TOK], BF16)
            CT = rg.take([128, TOK], BF16)
            dt = rg.take([128, NT, 16], F32)
            da = rg.take([128, NT, 16], F32)
            acs = rg.take([128, NT, 16], F32)
            scw = rg.take([128, NT, 16], F32)
            etg = rg.take([128, NT, 8], F32)
            convw = rg.take([128, 30], F32)
            convb = rg.take([128, 6], F32)
            dtb_b = rg.take([128, 16], F32)
            a_b = rg.take([128, 16], F32)
            Db = rg.take([128, 8], F32)
            normg = rg.take([128, 4], F32)
            mark = rg.off
            load("sp", convw, convw_d[l], [R("convw")])
            load("sp", convb, convb_d[l], [R("convb")])
            load("sp", dtb_b, dtb_d[l:l + 1, :].partition_broadcast(128), [R("dtb_b")])
            load("sp", a_b, alog_d[l:l + 1, :].partition_broadcast(128), [R("a_b")])
            load("sp", Db, ssdd_d[l:l + 1, :].partition_broadcast(128), [R("Db")])
            load("sp", normg, normgT_d[l], [R("normg")])
            act(a_b, a_b, AF.Exp, [R("a_b")], [R("a_b")])
            ts("dve", a_b, a_b, -1.0, ALU.mult, [R("a_b")], [R("a_b")])

            wx = [rg.take([128, 8, 128], BF16) for _ in range(2)]
            Rwx = [Res("wx%d" % i) for i in range(2)]
            pre = rg.take([128, TOK], F32)
            acc = rg.take([128, TOK], F32)
            post = rg.take([128, TOK], BF16)
            SEGS = [(0, 2048), (2048, 2304)]
            for ci in range(6):
                i = ci % 2
                load("pool", wx[i], wview(w_in_d[l], 1024 + ci * 128, 1024 + ci * 128 + 128), [Rwx[i]], sem="wx%d" % i)
                for bi, (t0, ntile) in enumerate(BLOCKS):
                    n = ntile * 128
                    bk, rb = banks[bi % 4], RB[bi % 4]
                    for k in range(8):
                        mm(bk[:, 0:n], wx[i][:, k, :], HT[:, k, t0 * 128:t0 * 128 + n], k == 0, k == 7,
                           [Rwx[i]] + RH[t0:t0 + ntile], [rb])
                    act(pre[:, t0 * 128:t0 * 128 + n], bk[:, 0:n], AF.Copy, [rb], [R("pre")])
                for (a, b) in SEGS:
                    ts("dve", acc[:, a:b], pre[:, a:b], convw[:, ci * 5 + 2:ci * 5 + 3], ALU.mult, [R("pre"), R("convw"), R("convb")], [R("acc")],
                       s2=convb[:, ci:ci + 1], op1=ALU.add)
                    for kk in (0, 1, 3, 4):
                        s = kk - 2
                        lo = max(a, a - s)
                        hi = min(b, b - s)
                        stt(acc[:, lo:hi], pre[:, lo + s:hi + s], convw[:, ci * 5 + kk:ci * 5 + kk + 1], acc[:, lo:hi], ALU.mult, ALU.add,
                            [R("pre"), R("convw"), R("acc")], [R("acc")])
                if ci < 4 or ci == 4:
                    dst_fm = post if ci < 4 else BT
                    rdst = R("post") if ci < 4 else R("BT")
                else:
                    dst_fm, rdst = CT, R("CT")
                act(dst_fm, acc, AF.Silu, [R("acc")], [rdst])
                if ci <= 4:
                    for g0 in range(0, NT, 8):
                        ng = min(8, NT - g0)
                        bi_ = 4 + (g0 // 8) % 2 + 2 * (ci % 2)
                        bk, rb = banks[bi_], RB[bi_]
                        bv = bk.bitcast(BF16)
                        for q in range(ng):
                            t = g0 + q
                            tr(bv[:, q * 128:q * 128 + 128], dst_fm[:, t * 128:t * 128 + 128], ident_b[:], [rdst, R("ident_b")], [rb])
                        src = bv[:, 0:ng * 128].rearrange("p (q c) -> p q c", c=128)
                        if ci < 4:
                            cp("act_copy", xs_tm[:, g0:g0 + ng, ci * 128:ci * 128 + 128], src, [rb], [R("xs_tm")])
                        else:
                            cp("act_copy", B_tm[:, g0:g0 + ng, :], src, [rb], [R("B_tm")])

            if cfg.get("ssd_stop", 9) <= 1:
                return
            S.barrier()
            rg.off = mark
            zs = rg.take([128, NT, 512], BF16)
            mark2 = rg.off
            wz = rg.take([128, 8, 512], BF16)
            wdt = rg.take([128, 8, 16], BF16)
            tot = rg.take([128, NT, 16], F32)
            etot = rg.take([128, NT, 16], F32)
            load("pool", wz, wview(w_in_d[l], 512, 1024), [R("wz")], sem="wz")
            load("pool", wdt, wview(w_in_d[l], 1792, 1808), [R("wdt")], sem="wdt")
            for t in range(NT):
                for k in range(8):
                    mm(banks[0][:, t * 16:t * 16 + 16], HT[:, k, t * 128:t * 128 + 128], wdt[:, k, :], k == 0, k == 7,
                       [R("wdt"), RH[t]], [RB[0]])
            p0 = banks[0][:, 0:NT * 16].rearrange("p (t c) -> p t c", c=16)
            tt("dve", dt, p0, bc_mid(dtb_b, NT), ALU.add, [RB[0], R("dtb_b")], [R("dt")])
            act(dt, dt, AF.Exp, [R("dt")], [R("dt")])
            act(dt, dt, AF.Ln, [R("dt")], [R("dt")], bias=1.0, scale=1.0)
            tt("dve", da, dt, bc_mid(a_b, NT), ALU.mult, [R("dt"), R("a_b")], [R("da")])
            for t in range(NT):
                mm(banks[1][:, t * 16:t * 16 + 8], triU[:], da[:, t, 0:8], True, True, [R("triU"), R("da")], [RB[1]])
                mm(banks[1][:, t * 16 + 8:t * 16 + 16], triL[:], da[:, t, 8:16], True, True, [R("triL"), R("da")], [RB[1]])
                mm(banks[2][:, t * 16:t * 16 + 16], ones_f[:], da[:, t, :], True, True, [R("ones_f"), R("da")], [RB[2]])
            p1 = banks[1][:, 0:NT * 16].rearrange("p (t c) -> p t c", c=16)
            p2 = banks[2][:, 0:NT * 16].rearrange("p (t c) -> p t c", c=16)
            cp("dve", acs, p1, [RB[1]], [R("acs")])
            cp("dve", tot, p2, [RB[2]], [R("tot")])
            tt("dve", scw, tot, acs, ALU.subtract, [R("tot"), R("acs")], [R("scw")])
            act(scw, scw, AF.Exp, [R("scw")], [R("scw")])
            tt("dve", scw, scw, dt, ALU.mult, [R("scw"), R("dt")], [R("scw")])
            act(etot, tot, AF.Exp, [R("tot")], [R("etot")])
            for d_ in range(2):
                for g in range(2):
                    cp("dve", etg[64 * g:64 * g + 64, :, 4 * d_:4 * d_ + 4], etot[64 * g:64 * g + 64, :, 8 * d_ + 4 * g:8 * d_ + 4 * g + 4],
                       [R("etot")], [R("etg")])
            for t in range(NT):
                if t >= NLAT and not need_ctx:
                    continue
                bk, rb = banks[4 + t % 4], RB[4 + t % 4]
                for k in range(8):
                    mm(bk[:, :], HT[:, k, t * 128:t * 128 + 128], wz[:, k, :], k == 0, k == 7, [R("wz"), RH[t]], [rb])
                act(zs[:, t, :], bk[:, :], AF.Silu, [rb], [R("zs")])

            if cfg.get("ssd_stop", 9) <= 2:
                return
            S.barrier()
            rg.off = mark2
            prevB = rg.take([128, NT, 256], BF16)
            woS = rg.take([128, 4, D], BF16)
            load("pool", woS, w_outP_d[l][256:768, :].rearrange("(k p) n -> p k n", p=128), [R("woS")], sem="woS")
            hr = ht_region()
            xw = [hr.take([128, 512], BF16) for _ in range(2)]
            Rxw = [Res("xw%d" % i) for i in range(2)]
            xdt = [hr.take([128, 512], BF16) for _ in range(2)]
            Rxdt = [Res("xdt%d" % i) for i in range(2)]
            rhsU = [hr.take([128, 8, 128], F32) for _ in range(2)]
            RrhsU = [Res("rhsU%d" % i) for i in range(2)]
            E = [hr.take([128, 8, 128], BF16) for _ in range(2)]
            RE = [Res("E%d" % i) for i in range(2)]
            Eb = [hr.take([128, 8, 128], BF16) for _ in range(2)]
            REb = [Res("Eb%d" % i) for i in range(2)]
            Gm = [hr.take([128, 2, 128], BF16) for _ in range(2)]
            RGm = [Res("Gm%d" % i) for i in range(2)]
            nacs = hr.take([128, NT, 16], F32)
            Sf = hr.take([128, 256], F32)
            Sf_bf = hr.take([128, 256], BF16)
            Sb = hr.take([128, 256], F32)
            tmpD = hr.take([128, 512], F32)
            ytot = hr.take([128, 512], F32)
            yn = hr.take([128, 512], BF16)
            oT = hr.take([128, 4, 128], BF16)
            upd = hr.take([128, 512], F32)
            sjunk = hr.take([128, 512], BF16)
            ssq = hr.take([128, 64], F32)
            rupd = Res("updS")
            for t in range(NT):
                RH[t] = Res("H%d" % t)

            memset("dve", Sb, 0.0, [R("Sb")])
            memset("dve", Sf, 0.0, [R("Sf")])
            memset("dve", Sf_bf, 0.0, [R("Sf_bf")])
            ts("dve", nacs, acs, -1.0, ALU.mult, [R("acs")], [R("nacs")])

            def xs3(c):
                return xs_tm[:, c, :].rearrange("p (h q) -> p h q", q=64)

            STB = 6

            def states(c, d_, i, Sacc, rS):
                tt("pool", xw[i][:].rearrange("p (h q) -> p h q", q=64), xs3(c), bc_last(scw[:, c, 8 * d_:8 * d_ + 8], 64), ALU.mult,
                   [R("xs_tm"), R("scw")], [Rxw[i]])
                for g in range(2):
                    mm(banks[STB][64 * g:64 * g + 64, 128:384], B_tm[:, c, 64 * g:64 * g + 64], xw[i][:, 256 * g:256 * g + 256], True, True,
                       [R("B_tm"), Rxw[i]], [RB[STB]])
                for hl in range(4):
                    sl = slice(64 * hl, 64 * hl + 64)
                    stt(Sacc[:, sl], Sacc[:, sl], etg[:, c, 4 * d_ + hl:4 * d_ + hl + 1], banks[STB][:, 128 + 64 * hl:128 + 64 * hl + 64], ALU.mult, ALU.add,
                        [rS, R("etg"), RB[STB]], [rS])

            for n_, c in enumerate([17, 16] + list(range(15, -1, -1))):
                cp("act_copy", prevB[:, c, :], Sb, [R("Sb")], [R("prevB")])
                if c != 0:
                    states(c, 1, n_ % 2, Sb, R("Sb"))

            order2 = [16, 17] + list(range(NLAT))

            def front(n_, c):
                emit_out = need_ctx or c < NLAT
                csl = slice(c * 128, c * 128 + 128)
                if emit_out:
                    gbanks = ((banks[5][:, 0:128], RB[5]), (banks[6][:, 0:128], RB[6]))
                    for g in range(2):
                        ps_ = slice(64 * g, 64 * g + 64)
                        mm(gbanks[g][0], BT[ps_, csl], CT[ps_, csl], True, True, [R("BT"), R("CT")], [gbanks[g][1]])
                    for d_ in range(2):
                        tri = triU if d_ == 0 else triL
                        for g in range(2):
                            tt("dve", Gm[d_][:, g, :], gbanks[g][0], tri[:], ALU.mult, [gbanks[g][1], R("triU"), R("triL")], [RGm[d_]])
                    for d_ in range(2):
                        tri = triU if d_ == 0 else triL
                        tt("pool", rhsU[d_], bc_mid(tri[:], 8), bc_last(da[:, c, 8 * d_:8 * d_ + 8], 128), ALU.mult,
                           [R("triU"), R("triL"), R("da")], [RrhsU[d_]])
                        for h in range(8):
                            bi_ = 2 * d_ + h // 4
                            mm(banks[bi_][:, (h % 4) * 128:(h % 4) * 128 + 128], ones_f[:], rhsU[d_][:, h, :], True, True,
                               [R("ones_f"), RrhsU[d_]], [RB[bi_]])
                        for h in range(8):
                            bi_ = 2 * d_ + h // 4
                            act(E[d_][:, h, :], banks[bi_][:, (h % 4) * 128:(h % 4) * 128 + 128], AF.Exp, [RB[bi_], R("nacs")], [RE[d_]],
                                bias=nacs[:, c, 8 * d_ + h:8 * d_ + h + 1], scale=1.0)
                        for q in range(2):
                            bi_ = 2 * d_ + q
                            act(Eb[d_][:, 4 * q:4 * q + 4, :].rearrange("p h i -> p (h i)"), banks[bi_][:, :], AF.Exp, [RB[bi_]], [REb[d_]])
                        for g in range(2):
                            stt(E[d_][:, 4 * g:4 * g + 4, :], E[d_][:, 4 * g:4 * g + 4, :], 1.0, bc_mid(Gm[d_][:, g, :], 4), ALU.min, ALU.mult,
                                [RE[d_], RGm[d_]], [RE[d_]])
                        tt("pool", Eb[d_], Eb[d_], bc_mid(CT[:, csl], 8), ALU.mult, [REb[d_], R("CT")], [REb[d_]])
                        tt("dve", xdt[d_][:].rearrange("p (h q) -> p h q", q=64), xs3(c), bc_last(dt[:, c, 8 * d_:8 * d_ + 8], 64), ALU.mult,
                           [R("xs_tm"), R("dt")], [Rxdt[d_]])

            def front_y(n_, c):
                emit_out = need_ctx or c < NLAT
                if emit_out:
                    for d_ in range(2):
                        for h in range(8):
                            g, hl = h // 4, h % 4
                            ysl = slice(64 * h, 64 * h + 64)
                            mm(banks[4][:, ysl], E[d_][:, h, :], xdt[d_][:, ysl], d_ == 0 and h == 0, False, [RE[d_], Rxdt[d_]], [RB[4]], sgc=True)
                            st_ = Sf_bf if d_ == 0 else prevB[:, c, :]
                            rst = R("Sf_bf") if d_ == 0 else R("prevB")
                            mm(banks[4][:, ysl], Eb[d_][64 * g:64 * g + 64, h, :], st_[64 * g:64 * g + 64, 64 * hl:64 * hl + 64], False, d_ == 1,
                               [REb[d_], rst], [RB[4]], sgc=True)
                if c != NLAT - 1:
                    states(c, 0, n_ % 2, Sf, R("Sf"))
                    cp("act_copy", Sf_bf, Sf, [R("Sf")], [R("Sf_bf")])

            def back1(n_, c):
                emit_out = need_ctx or c < NLAT
                if not emit_out:
                    return
                tt("dve", tmpD[:].rearrange("p (h q) -> p h q", q=64), xs3(c), bc_last(Db, 64), ALU.mult, [R("xs_tm"), R("Db")], [R("tmpD")])
                tt("dve", ytot, banks[4][:, :], tmpD, ALU.add, [RB[4], R("tmpD")], [R("ytot")])

            def back(n_, c):
                emit_out = need_ctx or c < NLAT
                if not emit_out:
                    return
                j = 0 if c < NLAT else 1
                tt("dve", ytot, ytot, zs[:, c, :], ALU.mult, [R("ytot"), R("zs")], [R("ytot")])
                act(sjunk, ytot, AF.Square, [R("ytot")], [R("sjunk"), R("ssq")], accum_out=ssq[:, c:c + 1])
                act(ssq[:, 32 + c:33 + c], ssq[:, c:c + 1], AF.Ln, [R("ssq")], [R("ssq")], bias=1e-6, scale=1.0 / 512)
                act(ssq[:, 32 + c:33 + c], ssq[:, 32 + c:33 + c], AF.Exp, [R("ssq")], [R("ssq")], scale=-0.5)
                ts("dve", yn, ytot, ssq[:, 32 + c:33 + c], ALU.mult, [R("ytot"), R("ssq")], [R("yn")])
                b5 = banks[5].bitcast(BF16)
                for kc in range(4):
                    tr(b5[:, 512 + kc * 128:512 + kc * 128 + 128], yn[:, kc * 128:kc * 128 + 128], ident_b[:], [R("yn"), R("ident_b")], [RB[5]])
                for kc in range(4):
                    ts("dve", oT[:, kc, :], b5[:, 512 + kc * 128:512 + kc * 128 + 128], normg[:, kc:kc + 1], ALU.mult, [RB[5], R("normg")], [R("oT")])
                for hf in range(2):
                    sl = slice(hf * 512, hf * 512 + 512)
                    for kc in range(4):
                        mm(banks[7][:, :], oT[:, kc, :], woS[:, kc, sl], kc == 0, kc == 3, [R("oT"), R("woS")], [RB[7]])
                    tt("dve", upd, banks[7][:, :], GATE[:, j, sl], ALU.mult, [RB[7], R("GATE")], [rupd])
                    tt("dve", X[:, c, sl], X[:, c, sl], upd, ALU.add, [rupd, RX[c]], [RX[c]])

            front(0, order2[0])
            front_y(0, order2[0])
            for n_, c in enumerate(order2):
                if n_ + 1 < len(order2):
                    front(n_ + 1, order2[n_ + 1])
                back1(n_, c)
                if n_ + 1 < len(order2):
                    front_y(n_ + 1, order2[n_ + 1])
                back(n_, c)


        def tree(op, dst, src, width, n1, tmpbuf):
            cur = src
            w = width
            while w > 1:
                h = w // 2
                out = dst.unsqueeze(2) if h == 1 else tmpbuf[:, :, 0:h]
                tt("dve", out, cur[:, :, 0:h], cur[:, :, h:w], op, [R("rt")], [R("rt")])
                cur = out
                w = h

        def moe_phase(l, need_ctx):
            tiles = list(range(NT)) if need_ctx else list(range(NLAT))
            ntl = len(tiles)
            S.barrier()
            rg = region()
            comb = rg.take([128, NT, 32], F32)
            lg = rg.take([128, NT, 36], F32)
            mark = rg.off
            w_rt = rg.take([128, 8, 36], F32)
            brt = rg.take([128, 36], F32)
            load("sp", w_rt, w_rt_d[l].rearrange("(k p) n -> p k n", p=128), [R("w_rt")])
            load("sp", brt, b_rt_d[l:l + 1, :].partition_broadcast(128), [R("brt")])

            def router(t, hf, rhf):
                bi_ = 4 + t // 9
                c0 = (t % 9) * 36
                for k in range(8):
                    mm(banks[bi_][:, c0:c0 + 36], hf[:, k, :], w_rt[:, k, :], k == 0, k == 7, [rhf, R("w_rt")], [RB[bi_]])

            norm_phase(1, tiles, rg=rg, router=router)
            for q in range(2):
                t0, t1 = 9 * q, min(9 * q + 9, ntl)
                if t1 <= t0:
                    continue
                n_ = t1 - t0
                src = banks[4 + q][:, 0:n_ * 36].rearrange("p (t c) -> p t c", c=36)
                tt("dve", lg[:, t0:t1, :], src, bc_mid(brt, n_), ALU.add, [RB[4 + q], R("brt")], [R("rt")])
            S.barrier()
            rg.off = mark
            gl = lg[:, 0:ntl, 0:4]
            el = lg[:, 0:ntl, 4:36]
            t4 = rg.take([128, NT, 4], F32)
            t32 = rg.take([128, NT, 32], F32)
            elm = rg.take([128, NT, 32], F32)
            m1b = rg.take([128, NT, 32], F32)
            m2b = rg.take([128, NT, 32], F32)
            gmax = rg.take([128, NT], F32)
            gw = rg.take([128, NT], F32)
            m1 = rg.take([128, NT], F32)
            m2 = rg.take([128, NT], F32)
            w1 = rg.take([128, NT], F32)
            w2 = rg.take([128, NT], F32)
            RT = [R("rt")]
            n = ntl
            tree(ALU.max, gmax[:, 0:n], gl, 4, n, t4[:, 0:n, :])
            tt("dve", t4[:, 0:n, :], gl, bc_last(gmax[:, 0:n], 4), ALU.subtract, RT, RT)
            act(t4[:, 0:n, :], t4[:, 0:n, :], AF.Exp, RT, RT)
            tree(ALU.add, gw[:, 0:n], t4[:, 0:n, :], 4, n, t32[:, 0:n, 0:4])
            S.op("dve", lambda e: e.reciprocal(out=gw[:, 0:n], in_=gw[:, 0:n]), RT, RT)
            tt("dve", t4[:, 0:n, :], gl, bc_last(gmax[:, 0:n], 4), ALU.is_equal, RT, RT)
            ts("dve", t4[:, 0:n, :], t4[:, 0:n, :], -1.0, ALU.add, RT, RT, s2=-NEG, op1=ALU.mult)
            for g in range(4):
                tt("dve", elm[:, 0:n, 8 * g:8 * g + 8], el[:, :, 8 * g:8 * g + 8], t4[:, 0:n, g:g + 1].to_broadcast([128, n, 8]), ALU.add, RT, RT)
            tree(ALU.max, m1[:, 0:n], elm[:, 0:n, :], 32, n, t32[:, 0:n, :])
            tt("dve", m1b[:, 0:n, :], elm[:, 0:n, :], bc_last(m1[:, 0:n], 32), ALU.is_equal, RT, RT)
            stt(elm[:, 0:n, :], m1b[:, 0:n, :], 2 * NEG, elm[:, 0:n, :], ALU.mult, ALU.add, RT, RT)
            tree(ALU.max, m2[:, 0:n], elm[:, 0:n, :], 32, n, t32[:, 0:n, :])
            tt("dve", m2b[:, 0:n, :], elm[:, 0:n, :], bc_last(m2[:, 0:n], 32), ALU.is_equal, RT, RT)
            tt("dve", w2[:, 0:n], m2[:, 0:n], m1[:, 0:n], ALU.subtract, RT, RT)
            act(w2[:, 0:n], w2[:, 0:n], AF.Exp, RT, RT)
            ts("dve", w1[:, 0:n], w2[:, 0:n], 1.0, ALU.add, RT, RT)
            S.op("dve", lambda e: e.reciprocal(out=w1[:, 0:n], in_=w1[:, 0:n]), RT, RT)
            tt("dve", w1[:, 0:n], w1[:, 0:n], gw[:, 0:n], ALU.mult, RT, RT)
            tt("dve", w2[:, 0:n], w2[:, 0:n], w1[:, 0:n], ALU.mult, RT, RT)
            tt("dve", m1b[:, 0:n, :], m1b[:, 0:n, :], bc_last(w1[:, 0:n], 32), ALU.mult, RT, RT)
            tt("dve", m2b[:, 0:n, :], m2b[:, 0:n, :], bc_last(w2[:, 0:n], 32), ALU.mult, RT, RT)
            tt("dve", comb[:, 0:n, :], m1b[:, 0:n, :], m2b[:, 0:n, :], ALU.add, RT, [R("comb")])
            S.barrier()
            rg.off = mark
            NS = 3
            WGU = [rg.take([128, 8, 512], BF16) for _ in range(NS)]
            WD = [rg.take([128, 2, D], BF16) for _ in range(NS)]
            WDx = [rg.take([128, 2, D], BF16) for _ in range(NS)]
            WDc = [rg.take([128, 2, D], BF16) for _ in range(NS)]
            Rgu = [Res("wgu%d" % i) for i in range(NS)]
            Rwd = [Res("wd%d" % i) for i in range(NS)]
            Rwdx = [Res("wdx%d" % i) for i in range(NS)]
            s_sb = [rg.take([128, 256], F32) for _ in range(2)]
            Rs = [Res("s%d" % i) for i in range(2)]
            hid = [rg.take([128, 256], BF16) for _ in range(2)]
            Rhid = [Res("hid%d" % i) for i in range(2)]
            hidT = [rg.take([128, 2, 128], BF16) for _ in range(2)]
            RhT = [Res("hidT%d" % i) for i in range(2)]

            def load_expert(e):
                sl = e % NS
                S.dma("pool", [lambda en: en.dma_start(out=WGU[sl][:, :, 0:256], in_=wg_d[l, e].rearrange("(k p) n -> p k n", p=128)),
                               lambda en: en.dma_start(out=WGU[sl][:, :, 256:512], in_=wu_d[l, e].rearrange("(k p) n -> p k n", p=128))],
                      "wgu%d" % sl, [], [Rgu[sl]])
                S.dma("pool", [lambda en: en.dma_start(out=WD[sl], in_=wd_d[l, e].rearrange("(k p) n -> p k n", p=128))],
                      "wd%d" % sl, [], [Rwd[sl]])
                for fc in range(2):
                    tt("pool", WDx[sl][:, fc, :], WD[sl][:, fc, :], GATE[:, 2, :], ALU.mult, [Rwd[sl], R("GATE")], [Rwdx[sl]])
                    if need_ctx:
                        tt("pool", WDc[sl][:, fc, :], WD[sl][:, fc, :], GATE[:, 3, :], ALU.mult, [Rwd[sl], R("GATE")], [Rwdx[sl]])

            items = [(e, t) for e in range(32) for t in tiles]
            nit = len(items)
            b2 = banks[2].bitcast(BF16)

            def stageG(i):
                e, t = items[i]
                sl = e % NS
                bk, rb = banks[i % 2], RB[i % 2]
                for k in range(8):
                    mm(bk[:, :], HT[:, k, t * 128:t * 128 + 128], WGU[sl][:, k, :], k == 0, k == 7, [RH[t], Rgu[sl]], [rb])
                act(s_sb[i % 2], bk[:, 0:256], AF.Silu, [rb], [Rs[i % 2]])
                stt(hid[i % 2], bk[:, 256:512], comb[:, t, e:e + 1], s_sb[i % 2], ALU.mult, ALU.mult, [rb, R("comb"), Rs[i % 2]], [Rhid[i % 2]])

            def stageT(i):
                for fc in range(2):
                    tr(b2[:, (i % 2) * 256 + fc * 128:(i % 2) * 256 + fc * 128 + 128], hid[i % 2][:, fc * 128:fc * 128 + 128], ident_b[:],
                       [Rhid[i % 2], R("ident_b")], [RB[2]])
                cp("act_copy", hidT[i % 2][:].rearrange("p a b -> p (a b)"), b2[:, (i % 2) * 256:(i % 2) * 256 + 256], [RB[2]], [RhT[i % 2]])

            def stageD(i):
                e, t = items[i]
                sl = e % NS
                wdd = WDx[sl] if t < NLAT else WDc[sl]
                for hf in range(2):
                    bi_ = 4 + 2 * (i % 2) + hf
                    for fc in range(2):
                        mm(banks[bi_][:, :], hidT[i % 2][:, fc, :], wdd[:, fc, hf * 512:hf * 512 + 512], fc == 0, fc == 1,
                           [RhT[i % 2], Rwdx[sl]], [RB[bi_]])
                for hf in range(2):
                    bi_ = 4 + 2 * (i % 2) + hf
                    sl_ = slice(hf * 512, hf * 512 + 512)
                    tt("dve", X[:, t, sl_], X[:, t, sl_], banks[bi_][:, :], ALU.add, [RX[t], RB[bi_]], [RX[t]])

            for e in range(NS):
                load_expert(e)
            for i in range(nit + 2):
                if i < nit:
                    stageG(i)
                if 0 <= i - 1 < nit:
                    stageT(i - 1)
                if 0 <= i - 2 < nit:
                    stageD(i - 2)
                    e, t = items[i - 2]
                    if t == tiles[-1] and e + NS < 32:
                        load_expert(e + NS)


        def final_phase():
            S.barrier()
            rg = region()
            gfb = rg.take([128, D], F32)
            junk = rg.take([128, D], BF16)
            load("sp", gfb, g_final_d.partition_broadcast(128), [R("gfb")])
            ot = [rg.take([128, D], F32) for _ in range(2)]
            rot = [Res("fo%d" % i) for i in range(2)]
            ov = out_d.rearrange("(t p) d -> p t d", p=128)
            for t in range(NLAT):
                b = t % 2
                if final_norm:
                    act(junk[:], X[:, t, :], AF.Square, [RX[t]], [R("junk"), R("ss%d" % t)], accum_out=small[:, t:t + 1])
                    act(small[:, 32 + t:33 + t], small[:, t:t + 1], AF.Ln, [R("ss%d" % t)], [R("rs%d" % t)], bias=1e-6, scale=1.0 / D)
                    act(small[:, 32 + t:33 + t], small[:, 32 + t:33 + t], AF.Exp, [R("rs%d" % t)], [R("rs%d" % t)], scale=-0.5)
                    stt(ot[b], X[:, t, :], small[:, 32 + t:33 + t], gfb, ALU.mult, ALU.mult, [RX[t], R("rs%d" % t), R("gfb")], [rot[b]])
                else:
                    cp("dve", ot[b], X[:, t, :], [RX[t]], [rot[b]])
                S.dma("sp", [lambda e, b=b, t=t: e.dma_start(out=ov[:, t, :], in_=ot[b])], "outst", [rot[b]], [])
            if dbg_d is not None:
                dv = dbg_d.rearrange("(t p) d -> p t d", p=128)
                for t in range(2):
                    S.dma("sp", [lambda e, t=t: e.dma_start(out=dv[:, t, :], in_=X[:, NLAT + t, :])], "outst", [RX[NLAT + t]], [])
            S.streams["sp"].append(("wait", ("dma", "outst"), S.dmacnt[("dma", "outst")]))

        for l in range(nlayers):
            need_ctx = l < DEPTH - 1
            mod_phase(l)
            S.barrier()
            norm_phase(0, list(range(NT)))
            if "A" in phases:
                attn_phase("A", l, need_ctx)
            if "N" in phases:
                attn_phase("N", l, need_ctx)
            if "S" in phases:
                ssd_phase(l, need_ctx)
            if "M" in phases:
                moe_phase(l, need_ctx)
        final_phase()
        S.emit()
    return nc


def _prep_shared(inp):
    f = np.float32
    sh = {}
    sh["w_mod"] = np.ascontiguousarray(inp["w_mod"], f)
    sh["b_mod"] = np.ascontiguousarray(inp["b_mod"], f)
    sh["b_modT"] = np.ascontiguousarray(inp["b_mod"].reshape(DEPTH, 48, 128).transpose(0, 2, 1), f)
    sh["g_mixT"] = np.ascontiguousarray(inp["g_mix"].reshape(DEPTH, 8, 128).transpose(0, 2, 1), f)
    sh["g_ffnT"] = np.ascontiguousarray(inp["g_ffn"].reshape(DEPTH, 8, 128).transpose(0, 2, 1), f)
    sh["g_final"] = np.ascontiguousarray(inp["g_final"].reshape(1, D), f)
    w_in = np.asarray(inp["w_in"], f)
    sw = _swap_idx()
    qcol = lambda h: np.arange(64 * h, 64 * h + 64)
    kcol = lambda g: 256 + np.arange(64 * g, 64 * g + 64)
    Q02 = np.concatenate([qcol(0), qcol(2)])
    Q13 = np.concatenate([qcol(1), qcol(3)])
    K01 = np.concatenate([kcol(0), kcol(1)])
    Q02s = np.concatenate([qcol(0)[sw], qcol(2)[sw]])
    Q13s = np.concatenate([qcol(1)[sw], qcol(3)[sw]])
    K01s = np.concatenate([kcol(0)[sw], kcol(1)[sw]])
    Vc = 384 + np.arange(128)
    colsA = np.concatenate([Q02, Q13, K01, Q02s, Q13s, K01s, Vc])
    sh["w_inA"] = np.ascontiguousarray(w_in[:, :, colsA])
    sh["w_in"] = np.ascontiguousarray(w_in)
    rows = np.concatenate([np.arange(0, 64), np.arange(128, 192), np.arange(64, 128), np.arange(192, 256), np.arange(256, 1024)])
    sh["w_outP"] = np.ascontiguousarray(np.asarray(inp["w_out"], f)[:, rows, :])
    sk = np.asarray(inp["attn_sink"], f)
    sinkE = np.zeros((DEPTH, 128, 2), f)
    sinkE[:, 0:64, 0] = sk[:, 0:1]; sinkE[:, 64:128, 0] = sk[:, 2:3]
    sinkE[:, 0:64, 1] = sk[:, 1:2]; sinkE[:, 64:128, 1] = sk[:, 3:4]
    sh["sinkE"] = sinkE
    cw = np.asarray(inp["ssd_conv_w"], f)
    sh["convw"] = np.ascontiguousarray(cw.reshape(DEPTH, 5, 6, 128).transpose(0, 3, 2, 1).reshape(DEPTH, 128, 30))
    sh["convb"] = np.ascontiguousarray(np.asarray(inp["ssd_conv_b"], f).reshape(DEPTH, 6, 128).transpose(0, 2, 1))
    sh["dtb"] = np.ascontiguousarray(np.asarray(inp["ssd_dt_bias"], f).reshape(DEPTH, 16))
    sh["alog"] = np.ascontiguousarray(np.asarray(inp["ssd_a_log"], f).reshape(DEPTH, 16))
    sh["ssdd"] = np.ascontiguousarray(np.asarray(inp["ssd_d"], f))
    sh["normgT"] = np.ascontiguousarray(np.asarray(inp["ssd_norm_g"], f).reshape(DEPTH, 4, 128).transpose(0, 2, 1))
    rpb = np.asarray(inp["na_rpb"], f)
    sh["naTab"] = np.stack([_na_table(rpb[l]).reshape(128, 6144) for l in range(DEPTH)], 0)
    sh["w_rt"] = np.ascontiguousarray(np.concatenate([np.asarray(inp["w_router_group"], f), np.asarray(inp["w_router_expert"], f)], -1))
    sh["b_rt"] = np.ascontiguousarray(np.concatenate([np.asarray(inp["b_router_group"], f), np.asarray(inp["b_router_expert"], f)], -1))
    sh["w_exp_gate"] = np.ascontiguousarray(np.asarray(inp["w_exp_gate"], f).reshape(DEPTH, 32, D, 256))
    sh["w_exp_up"] = np.ascontiguousarray(np.asarray(inp["w_exp_up"], f).reshape(DEPTH, 32, D, 256))
    sh["w_exp_down"] = np.ascontiguousarray(np.asarray(inp["w_exp_down"], f).reshape(DEPTH, 32, 256, D))
    ct = _const_tables()
    ct["maskA"] = ct["maskA"].reshape(128, 384)
    sh.update(ct)
    return sh


def _run(inp, cfg, cores=None):
    sh = _prep_shared(inp)
    x = np.asarray(inp["x"], np.float32)
    c = np.asarray(inp["c"], np.float32)
    ctx = np.asarray(inp["ctx"], np.float32)
    c_ctx = np.asarray(inp["c_ctx"], np.float32)
    cores = list(range(8)) if cores is None else cores
    in_maps = []
    for b in cores:
        m = dict(sh)
        m["x"] = np.ascontiguousarray(x[b])
        m["ctx"] = np.ascontiguousarray(ctx[b])
        scT = np.zeros((128, 8, 2), np.float32)
        scT[:, :, 0] = c[b].reshape(8, 128).T
        scT[:, :, 1] = c_ctx.reshape(8, 128).T
        m["scT"] = scT.reshape(128, 16)
        in_maps.append(m)
    nc = build_program(cfg)
    res = run_bass_kernel_spmd(nc, in_maps, core_ids=list(range(len(cores))))
    return res


def kernel(**inputs):
    res = _run(inputs, {})
    return np.stack([r["out"] for r in res.results], 0).astype(np.float32)
```

```python
import contextlib
import math
import numpy as np
import concourse.bass as bass
import concourse.mybir as mybir
from concourse.bass_utils import run_bass_kernel_spmd

F32 = mybir.dt.float32
BF16 = mybir.dt.bfloat16
AF = mybir.ActivationFunctionType
ALU = mybir.AluOpType

EPOCH = 24000
NEG = -30000.0
D = 1024
NLAT = 16
NT = 18
TOK = NT * 128
DEPTH = 4
REGN = 37632


class Res:
    __slots__ = ("name", "w", "r", "psum")

    def __init__(self, name="", psum=False):
        self.name = name
        self.w = None
        self.r = []
        self.psum = psum


class Sched:
    ENGS = ("pe", "act", "dve", "pool", "sp")

    def __init__(self, nc):
        self.nc = nc
        self.streams = {e: [] for e in self.ENGS}
        self.cnt = {e: 0 for e in self.ENGS}
        self.waited = {e: {} for e in self.ENGS}
        self.semkeys = []
        self.semset = set()
        self.dmacnt = {}
        self.last = {}

    def _need(self, key):
        if key not in self.semset:
            self.semset.add(key)
            self.semkeys.append(key)

    def _wait(self, eng, k, v):
        if k[0] == eng and eng == "pe":
            return
        wd = self.waited[eng]
        if wd.get(k, 0) < v:
            wd[k] = v
            self.streams[eng].append(("wait", k, v))

    def _deps(self, eng, reads, writes):
        for r in reads:
            if r.w is not None:
                self._wait(eng, *r.w)
        for w in writes:
            if w.w is not None:
                self._wait(eng, *w.w)
            for t in w.r:
                self._wait(eng, *t)

    def _post(self, tok, reads, writes):
        self.last[tok[0]] = tok[1]
        for r in reads:
            r.r.append(tok)
            if len(r.r) > 64:
                best = {}
                for (k, v) in r.r:
                    if best.get(k, 0) < v:
                        best[k] = v
                r.r = list(best.items())
        for w in writes:
            w.w = tok
            w.r = []

    def op(self, eng, fn, reads=(), writes=()):
        if eng != "pe":
            ex = [r for r in reads if r.psum]
            if ex:
                writes = list(writes) + ex
        self._deps(eng, reads, writes)
        c = self.cnt[eng]
        key = (eng, c // EPOCH)
        val = c % EPOCH + 1
        self._need(key)
        self.cnt[eng] = c + 1
        self.streams[eng].append(("op", fn, key, 1))
        tok = (key, val)
        self._post(tok, reads, writes)
        return tok

    def dma(self, eng, fns, semname, reads=(), writes=()):
        self._deps(eng, reads, writes)
        key = ("dma", semname)
        self._need(key)
        c = self.dmacnt.get(key, 0)
        for fn in fns:
            self.streams[eng].append(("op", fn, key, 16))
            c += 16
        self.dmacnt[key] = c
        tok = (key, c)
        self._post(tok, reads, writes)
        return tok

    def barrier(self):
        toks = list(self.last.items())
        for eng in self.ENGS:
            for (k, v) in toks:
                self._wait(eng, k, v)

    def emit(self):
        nc = self.nc
        with contextlib.ExitStack() as es:
            sems = {}
            for i, k in enumerate(self.semkeys):
                sems[k] = es.enter_context(nc.semaphore("s%d" % i))
            block = es.enter_context(nc.Block())

            def runner(stream):
                def f(e):
                    for it in stream:
                        if it[0] == "wait":
                            e.wait_ge(sems[it[1]], it[2])
                        else:
                            it[1](e).then_inc(sems[it[2]], it[3])
                return f

            block.tensor(runner(self.streams["pe"]))
            block.scalar(runner(self.streams["act"]))
            block.vector(runner(self.streams["dve"]))
            block.gpsimd(runner(self.streams["pool"]))
            block.sync(runner(self.streams["sp"]))


def _rope_tables():
    t = np.arange(2048)
    rows = (t // 64).astype(np.float64)
    cols = (t % 64).astype(np.float64)
    inv = 1.0 / (10000.0 ** (np.arange(0, 32, 2, dtype=np.float64) / 32.0))
    C = np.zeros((64, 2048), np.float64)
    S = np.zeros((64, 2048), np.float64)
    ar = rows[None, :] * inv[:, None]
    ac = cols[None, :] * inv[:, None]
    C[0:16] = np.cos(ar); C[16:32] = np.cos(ar); C[32:48] = np.cos(ac); C[48:64] = np.cos(ac)
    S[0:16] = -np.sin(ar); S[16:32] = np.sin(ar); S[32:48] = -np.sin(ac); S[48:64] = np.sin(ac)
    C = np.concatenate([C, C], 0).astype(np.float32)
    S = np.concatenate([S, S], 0).astype(np.float32)
    return C, S


def _swap_idx():
    i = np.arange(64)
    return np.where(i < 16, i + 16, np.where(i < 32, i - 16, np.where(i < 48, i + 16, i - 16)))


def _const_tables():
    k = np.arange(128)[:, None]
    i = np.arange(128)[None, :]
    c = {}
    c["ident"] = np.eye(128, dtype=np.float32)
    c["triU"] = (k <= i).astype(np.float32)
    c["triL"] = (k >= i).astype(np.float32)
    c["maskF"] = np.where(k <= i, 0.0, NEG).astype(np.float32)
    c["maskB"] = np.where(k >= i, 0.0, NEG).astype(np.float32)
    mA = np.zeros((128, 3, 128), np.float32)
    mA[:, 0, :] = np.where(i <= k, 0.0, NEG)
    mA[:, 2, :] = np.where(k <= i, 0.0, NEG)
    c["maskA"] = mA
    C, S = _rope_tables()
    c["ropeC"] = C
    c["ropeS"] = S
    return c


def _na_table(rpb):
    kr = (np.arange(128) // 64)[:, None]
    kc = (np.arange(128) % 64)[:, None]
    qr = (np.arange(128) // 64)[None, :]
    qc = (np.arange(128) % 64)[None, :]
    cs = np.clip(qc - 8, 0, 48)
    colv = (kc >= cs) & (kc < cs + 16)
    coff = np.clip(kc - qc, -15, 15) + 15
    out = np.full((128, 4, 12, 128), NEG, np.float32)
    for v in range(12):
        if v < 5:
            dj = v - 2
            dr = 2 * dj + kr - qr
            rowv = (dr >= -4) & (dr <= 3)
        else:
            dj = v - 5 - 3
            dr = 2 * dj + kr - qr
            rowv = np.abs(dr) <= 7
        valid = rowv & colv
        drc = np.clip(dr + 7, 0, 14)
        for h in range(4):
            g = rpb[h][drc, coff]
            out[:, h, v, :] = np.where(valid, g, np.float32(NEG))
    return out


def _na_keys(t):
    if t < 2:
        return [(j, 5 + (j - t) + 3) for j in range(0, 4)]
    if t >= 14:
        return [(j, 5 + (j - t) + 3) for j in range(12, 16)]
    return [(j, (j - t) + 2) for j in range(t - 2, t + 3)]


def build_program(cfg):
    nlayers = cfg.get("nlayers", DEPTH)
    phases = cfg.get("phases", "ANSM")
    final_norm = cfg.get("final_norm", True)

    nc = bass.Bass("TRN2", target_bir_lowering=False)

    def din(name, shape):
        return nc.dram_tensor(name, list(shape), F32, kind="ExternalInput").ap()

    x_d = din("x", [2048, D])
    ctx_d = din("ctx", [256, D])
    scT_d = din("scT", [128, 16])
    w_mod_d = din("w_mod", [DEPTH, D, 6 * D])
    b_modT_d = din("b_modT", [DEPTH, 128, 48])
    b_mod_d = din("b_mod", [DEPTH, 6 * D])
    g_mixT_d = din("g_mixT", [DEPTH, 128, 8])
    g_ffnT_d = din("g_ffnT", [DEPTH, 128, 8])
    g_final_d = din("g_final", [1, D])
    w_inA_d = din("w_inA", [DEPTH, D, 896])
    w_in_d = din("w_in", [DEPTH, D, 2576])
    w_outP_d = din("w_outP", [DEPTH, D, D])
    sinkE_d = din("sinkE", [DEPTH, 128, 2])
    convw_d = din("convw", [DEPTH, 128, 30])
    convb_d = din("convb", [DEPTH, 128, 6])
    dtb_d = din("dtb", [DEPTH, 16])
    alog_d = din("alog", [DEPTH, 16])
    ssdd_d = din("ssdd", [DEPTH, 8])
    normgT_d = din("normgT", [DEPTH, 128, 4])
    naTab_d = din("naTab", [DEPTH, 128, 6144])
    w_rt_d = din("w_rt", [DEPTH, D, 36])
    b_rt_d = din("b_rt", [DEPTH, 36])
    wg_d = din("w_exp_gate", [DEPTH, 32, D, 256])
    wu_d = din("w_exp_up", [DEPTH, 32, D, 256])
    wd_d = din("w_exp_down", [DEPTH, 32, 256, D])
    ident_d = din("ident", [128, 128])
    triU_d = din("triU", [128, 128])
    triL_d = din("triL", [128, 128])
    maskF_d = din("maskF", [128, 128])
    maskB_d = din("maskB", [128, 128])
    maskA_d = din("maskA", [128, 384])
    ropeC_d = din("ropeC", [128, 2048])
    ropeS_d = din("ropeS", [128, 2048])
    out_d = nc.dram_tensor("out", [2048, D], F32, kind="ExternalOutput").ap()
    dbg_d = None
    if cfg.get("dump_ctx"):
        dbg_d = nc.dram_tensor("out_ctx", [256, D], F32, kind="ExternalOutput").ap()

    es = contextlib.ExitStack()
    with es:
        def sb(name, shape, dt):
            return es.enter_context(nc.sbuf_tensor("sb_" + name, list(shape), dt))

        S = Sched(nc)

        X = sb("X", [128, NT, D], F32)
        HT = sb("HT", [128, 8, TOK], BF16)
        REG = sb("REG", [128, REGN], BF16)
        GATE = sb("GATE", [128, 4, D], F32)
        ident_f = sb("ident_f", [128, 128], F32)
        ident_b = sb("ident_b", [128, 128], BF16)
        triU = sb("triU", [128, 128], F32)
        triL = sb("triL", [128, 128], F32)
        maskF = sb("maskF", [128, 128], F32)
        maskB = sb("maskB", [128, 128], F32)
        maskA = sb("maskA", [128, 3, 128], BF16)
        ones_f = sb("ones_f", [128, 128], F32)
        ones_b = sb("ones_b", [128, 128], BF16)
        scT = sb("scT", [128, 16], F32)
        sc_b = sb("sc_b", [128, 8, 2], BF16)
        sc_rep = sb("sc_rep", [128, 2, 8, 128], BF16)
        modT = sb("modT", [128, 48, 2], F32)
        bmT = sb("bmT", [128, 48], F32)
        gT = sb("gT", [128, 16], F32)
        AV = sb("AV", [128, 2, 2, 8], F32)
        SV = sb("SV", [128, 2, 2, 8], F32)
        small = sb("small", [128, 64], F32)

        banks = [es.enter_context(nc.psum_tensor("bank%d" % i, [128, 512], F32)) for i in range(8)]
        RB = [Res("bank%d" % i, psum=True) for i in range(8)]

        RX = [Res("X%d" % t) for t in range(NT)]
        RH = [Res("H%d" % t) for t in range(NT)]
        Rc = {}

        def R(name):
            if name not in Rc:
                Rc[name] = Res(name)
            return Rc[name]

        class Carver:
            def __init__(self, base, nbytes):
                self.base = base
                self.off = 0
                self.nbytes = nbytes

            def take(self, shape, dt):
                esz = 2 if dt == BF16 else 4
                n = int(np.prod(shape[1:]))
                nb = n * esz
                nb_al = (nb + 63) // 64 * 64
                assert self.off + nb_al <= self.nbytes, ("region overflow", self.off, nb_al, self.nbytes)
                a = self.base[:, self.off // 2:(self.off + nb) // 2]
                self.off += nb_al
                if dt != BF16:
                    a = a.bitcast(dt)
                if len(shape) > 2:
                    names = " ".join("d%d" % i for i in range(1, len(shape)))
                    kw = {"d%d" % i: shape[i] for i in range(1, len(shape))}
                    a = a.rearrange("p (%s) -> p %s" % (names, names), **kw)
                return a

        def region():
            return Carver(REG, REGN * 2)

        def ht_region():
            return Carver(HT[:].rearrange("p k t -> p (k t)"), 8 * TOK * 2)

        def mm(out, lhsT, rhs, start, stop, reads, writes, sgc=False):
            if sgc:
                S.op("pe", lambda e: e.matmul(out, lhsT=lhsT, rhs=rhs, start=start, stop=stop, skip_group_check=True), reads, writes)
            else:
                S.op("pe", lambda e: e.matmul(out, lhsT=lhsT, rhs=rhs, start=start, stop=stop), reads, writes)

        def tr(out, in_, ident, reads, writes):
            S.op("pe", lambda e: e.transpose(out=out, in_=in_, identity=ident), reads, writes)

        def act(out, in_, func, reads, writes, bias=None, scale=None, accum_out=None):
            kw = {}
            if bias is not None:
                kw["bias"] = bias
            if scale is not None:
                kw["scale"] = scale
            if accum_out is not None:
                kw["accum_out"] = accum_out
            S.op("act", lambda e: e.activation(out=out, in_=in_, func=func, **kw), reads, writes)

        def tt(eng, out, in0, in1, op, reads, writes):
            S.op(eng, lambda e: e.tensor_tensor(out=out, in0=in0, in1=in1, op=op), reads, writes)

        def ts(eng, out, in0, s1, op0, reads, writes, s2=None, op1=None):
            if op1 is None:
                S.op(eng, lambda e: e.tensor_scalar(out=out, in0=in0, scalar1=s1, scalar2=None, op0=op0), reads, writes)
            else:
                S.op(eng, lambda e: e.tensor_scalar(out=out, in0=in0, scalar1=s1, scalar2=s2, op0=op0, op1=op1), reads, writes)

        def stt(out, in0, scalar, in1, op0, op1, reads, writes):
            S.op("dve", lambda e: e.scalar_tensor_tensor(out=out, in0=in0, scalar=scalar, in1=in1, op0=op0, op1=op1), reads, writes)

        def cp(eng, out, in_, reads, writes):
            if eng == "act_copy":
                S.op("act", lambda e: e.activation(out=out, in_=in_, func=AF.Copy), reads, writes)
            else:
                S.op(eng, lambda e: e.tensor_copy(out=out, in_=in_), reads, writes)

        def memset(eng, ap, val, writes):
            S.op(eng, lambda e: e.memset(ap, val), (), writes)

        dma_ctr = [0]

        def load(q, out, in_, writes, reads=(), sem=None):
            if sem is None:
                sem = "u%d" % (dma_ctr[0] % 40)
                dma_ctr[0] += 1
            S.dma(q, [lambda e: e.dma_start(out=out, in_=in_)], sem, reads, writes)

        def wview(w2d, ncols_lo, ncols_hi):
            return w2d[:, ncols_lo:ncols_hi].rearrange("(k p) n -> p k n", p=128)

        xv = x_d.rearrange("(t p) d -> p t d", p=128)
        cv = ctx_d.rearrange("(t p) d -> p t d", p=128)
        for t in range(NLAT):
            load("sp", X[:, t, :], xv[:, t, :], [RX[t]])
        for t in range(2):
            load("sp", X[:, NLAT + t, :], cv[:, t, :], [RX[NLAT + t]])
        load("sp", ident_f[:], ident_d, [R("ident_f")])
        load("pool", ident_b[:], ident_d, [R("ident_b")])
        load("sp", triU[:], triU_d, [R("triU")])
        load("sp", triL[:], triL_d, [R("triL")])
        load("sp", maskF[:], maskF_d, [R("maskF")])
        load("sp", maskB[:], maskB_d, [R("maskB")])
        load("pool", maskA[:].rearrange("p a b -> p (a b)"), maskA_d, [R("maskA")])
        load("sp", scT[:], scT_d, [R("scT")])
        memset("dve", ones_f[:], 1.0, [R("ones_f")])
        memset("dve", ones_b[:], 1.0, [R("ones_b")])
        act(scT[:], scT[:], AF.Silu, [R("scT")], [R("scT")])
        cp("dve", sc_b[:].rearrange("p k j -> p (k j)"), scT[:], [R("scT")], [R("sc_b")])
        for j in range(2):
            for k in range(8):
                ts("dve", sc_rep[:, j, k, :], ones_f[:], scT[:, 2 * k + j:2 * k + j + 1], ALU.mult,
                   [R("scT"), R("ones_f")], [R("sc_rep")])

        def mod_phase(l):
            S.barrier()
            rg = region()
            wb = [rg.take([128, 8, 512], BF16) for _ in range(3)]
            Rw = [Res("modw%d" % i) for i in range(3)]
            brow = rg.take([128, 4, 512], F32)
            load("sp", bmT[:], b_modT_d[l], [R("bmT")])
            load("sp", gT[:, 0:8], g_mixT_d[l], [R("gT")])
            load("sp", gT[:, 8:16], g_ffnT_d[l], [R("gT")])
            gate_chunks = {4: (0, 0), 5: (0, 1), 10: (1, 0), 11: (1, 1)}
            gi = 0
            for ch in range(12):
                i = ch % 3
                load("pool", wb[i], wview(w_mod_d[l], ch * 512, ch * 512 + 512), [Rw[i]], sem="modw%d" % i)
                if ch in gate_chunks:
                    g, half = gate_chunks[ch]
                    load("sp", brow[:, gi, :], b_mod_d[l:l + 1, ch * 512:ch * 512 + 512].partition_broadcast(128),
                         [R("brow")])
                    for j in range(2):
                        bk = banks[j]
                        for k in range(8):
                            mm(bk[:, :], sc_rep[:, j, k, :], wb[i][:, k, :], k == 0, k == 7,
                               [R("sc_rep"), Rw[i]], [RB[j]])
                        tt("dve", GATE[:, 2 * g + j, half * 512:half * 512 + 512], bk[:, :], brow[:, gi, :], ALU.add,
                           [RB[j], R("brow")], [R("GATE")])
                    gi += 1
                else:
                    for sub in range(4):
                        jn = ch * 4 + sub
                        for k in range(8):
                            mm(banks[2][:, 2 * jn:2 * jn + 2], wb[i][:, k, sub * 128:sub * 128 + 128], sc_b[:, k, :],
                               k == 0, k == 7, [R("sc_b"), Rw[i]], [RB[2]])
            mp = banks[2][:, 0:96].rearrange("p (j c) -> p j c", c=2)
            for j in range(2):
                tt("dve", modT[:, :, j], mp[:, :, j], bmT[:], ALU.add, [RB[2], R("bmT")], [R("modT")])
            for n in range(2):
                base = 0 if n == 0 else 24
                for j in range(2):
                    stt(AV[:, n, j, :], modT[:, base + 8:base + 16, j], 1.0, gT[:, 8 * n:8 * n + 8], ALU.add, ALU.mult,
                        [R("modT"), R("gT")], [R("AV")])
                    cp("dve", SV[:, n, j, :], modT[:, base:base + 8, j], [R("modT")], [R("SV")])

        def norm_phase(n, tiles, router=None, rg=None):
            if rg is None:
                rg = region()
            junk = rg.take([128, D], BF16)
            XN = rg.take([128, 2, D], F32)
            HF = rg.take([128, 2, 8, 128], F32)
            for idx, t in enumerate(tiles):
                j = 0 if t < NLAT else 1
                b = idx % 2
                rxn, rhf = R("XN%d" % b), R("HF%d" % b)
                act(junk[:], X[:, t, :], AF.Square, [RX[t]], [R("junk"), R("ss%d" % t)], accum_out=small[:, t:t + 1])
                act(small[:, 32 + t:33 + t], small[:, t:t + 1], AF.Ln, [R("ss%d" % t)], [R("rs%d" % t)], bias=1e-6, scale=1.0 / D)
                act(small[:, 32 + t:33 + t], small[:, 32 + t:33 + t], AF.Exp, [R("rs%d" % t)], [R("rs%d" % t)], scale=-0.5)
                ts("dve", XN[:, b, :], X[:, t, :], small[:, 32 + t:33 + t], ALU.mult, [RX[t], R("rs%d" % t)], [rxn])
                pb = [banks[2 * b], banks[2 * b + 1]]
                for k in range(8):
                    bk = pb[k // 4]
                    mm(bk[:, (k % 4) * 128:(k % 4) * 128 + 128], XN[:, b, k * 128:k * 128 + 128], ident_f[:], True, True,
                       [rxn, R("ident_f")], [RB[2 * b + k // 4]])
                for k in range(8):
                    bk = pb[k // 4]
                    if k % 4 < 2:
                        act(HF[:, b, k, :], bk[:, (k % 4) * 128:(k % 4) * 128 + 128], AF.Identity,
                            [RB[2 * b + k // 4], R("AV"), R("SV")], [rhf],
                            bias=SV[:, n, j, k:k + 1], scale=AV[:, n, j, k:k + 1])
                    else:
                        ts("dve", HF[:, b, k, :], bk[:, (k % 4) * 128:(k % 4) * 128 + 128], AV[:, n, j, k:k + 1], ALU.mult,
                           [RB[2 * b + k // 4], R("AV"), R("SV")], [rhf], s2=SV[:, n, j, k:k + 1], op1=ALU.add)
                cp("pool", HT[:, :, t * 128:t * 128 + 128], HF[:, b, :, :], [rhf], [RH[t]])
                if router is not None:
                    router(t, HF[:, b, :, :], rhf)

        def x_update(t, ps_lo, ps_hi, rb_lo, rb_hi, g, tmp, rtmp):
            for half, (ps, rb) in enumerate(((ps_lo, rb_lo), (ps_hi, rb_hi))):
                sl = slice(half * 512, half * 512 + 512)
                tt("dve", tmp[:, sl], ps, GATE[:, g, sl], ALU.mult, [rb, R("GATE")], [rtmp])
                tt("dve", X[:, t, sl], X[:, t, sl], tmp[:, sl], ALU.add, [rtmp, RX[t]], [RX[t]])

        def proj_fm(dst, rdst, wt, rw, col0, tiles_blocks, evac):
            for bi, (t0, ntile) in enumerate(tiles_blocks):
                bk = banks[bi % 2]
                n = ntile * 128
                for k in range(8):
                    mm(bk[:, 0:n], wt[:, k, col0:col0 + 128], HT[:, k, t0 * 128:t0 * 128 + n], k == 0, k == 7,
                       [rw] + RH[t0:t0 + ntile], [RB[bi % 2]])
                evac(bk[:, 0:n], RB[bi % 2], t0, n)

        BLOCKS = [(0, 4), (4, 4), (8, 4), (12, 4), (16, 2)]

        def attn_phase(kind, l, need_ctx):
            S.barrier()
            rg = region()
            if kind == "A":
                ncolw = 896
                wt = rg.take([128, 8, ncolw], BF16)
                load("pool", wt, wview(w_inA_d[l], 0, 896), [R("wt")], sem="wt")
                ropeC = rg.take([128, 2048], F32)
                ropeS = rg.take([128, 2048], F32)
                load("sp", ropeC, ropeC_d, [R("ropeC")])
                load("sp", ropeS, ropeS_d, [R("ropeS")])
                nq = 2
                Qs = [rg.take([128, TOK], BF16) for _ in range(2)]
                Ks = [rg.take([128, TOK], BF16)]
                Ks = [Ks[0], Ks[0]]
                vcols = 128
                Vtm = rg.take([128, NT, vcols], BF16)
                t1 = rg.take([128, 512], F32)
                t2 = rg.take([128, 512], F32)
                wo = rg.take([128, 2, D], BF16)
                load("pool", wo, w_outP_d[l][0:256, :].rearrange("(k p) n -> p k n", p=128), [R("wo")], sem="wo")
                esink = rg.take([128, 2], F32)
                load("sp", esink, sinkE_d[l], [R("esink")])
                act(esink, esink, AF.Exp, [R("esink")], [R("esink")])
                scale = 0.125
                nkmax = 5
                for ci, (dst, rn) in enumerate(((Qs[0], "Q0"), (Qs[1], "Q1"), (Ks[0], "K0"))):
                    for bi, (t0, ntile) in enumerate(BLOCKS):
                        n = ntile * 128
                        b0, b1 = banks[2 * (bi % 2)], banks[2 * (bi % 2) + 1]
                        r0, r1 = RB[2 * (bi % 2)], RB[2 * (bi % 2) + 1]
                        for k in range(8):
                            mm(b0[:, 0:n], wt[:, k, ci * 128:ci * 128 + 128], HT[:, k, t0 * 128:t0 * 128 + n], k == 0, k == 7,
                               [R("wt")] + RH[t0:t0 + ntile], [r0])
                        if t0 < NLAT:
                            for k in range(8):
                                mm(b1[:, 0:n], wt[:, k, (ci + 3) * 128:(ci + 3) * 128 + 128], HT[:, k, t0 * 128:t0 * 128 + n],
                                   k == 0, k == 7, [R("wt")] + RH[t0:t0 + ntile], [r1])
                            tt("dve", t1[:, 0:n], b0[:, 0:n], ropeC[:, t0 * 128:t0 * 128 + n], ALU.mult, [r0, R("ropeC")], [R("t1")])
                            tt("dve", t2[:, 0:n], b1[:, 0:n], ropeS[:, t0 * 128:t0 * 128 + n], ALU.mult, [r1, R("ropeS")], [R("t2")])
                            tt("pool", dst[:, t0 * 128:t0 * 128 + n], t1[:, 0:n], t2[:, 0:n], ALU.add, [R("t1"), R("t2")], [R(rn)])
                        else:
                            act(dst[:, t0 * 128:t0 * 128 + n], b0[:, 0:n], AF.Copy, [r0], [R(rn)])
                Rc["K1"] = Rc["K0"]
                vcol0 = 768
            else:
                wt = rg.take([128, 8, 768], BF16)
                load("pool", wt, wview(w_in_d[l], 1808, 2576), [R("wt")], sem="wt")
                tab = rg.take([128, 4, 12, 128], BF16)
                load("pool", tab[:].rearrange("p a b c -> p (a b c)"), naTab_d[l], [R("biasT")], sem="tab")
                Qs = [rg.take([128, TOK], BF16) for _ in range(2)]
                Ks = [rg.take([128, TOK], BF16) for _ in range(2)]
                vcols = 256
                Vtm = rg.take([128, NT, vcols], BF16)
                wo = rg.take([128, 2, D], BF16)
                load("pool", wo, w_outP_d[l][768:1024, :].rearrange("(k p) n -> p k n", p=128), [R("wo")], sem="wo")
                scale = 1.0
                nkmax = 7
                for ci, (dst, rn, sc_) in enumerate(((Qs[0], "Q0", 0.125), (Qs[1], "Q1", 0.125), (Ks[0], "K0", 1.0), (Ks[1], "K1", 1.0))):
                    for bi, (t0, ntile) in enumerate(BLOCKS):
                        n = ntile * 128
                        b0, r0 = banks[bi % 4], RB[bi % 4]
                        for k in range(8):
                            mm(b0[:, 0:n], wt[:, k, ci * 128:ci * 128 + 128], HT[:, k, t0 * 128:t0 * 128 + n], k == 0, k == 7,
                               [R("wt")] + RH[t0:t0 + ntile], [r0])
                        ts("dve", dst[:, t0 * 128:t0 * 128 + n], b0[:, 0:n], sc_, ALU.mult, [r0], [R(rn)])
                vcol0 = 512
            for t in range(NT):
                bk, rb = banks[4 + t % 4], RB[4 + t % 4]
                for k in range(8):
                    mm(bk[:, 0:vcols], HT[:, k, t * 128:t * 128 + 128], wt[:, k, vcol0:vcol0 + vcols], k == 0, k == 7,
                       [R("wt"), RH[t]], [rb])
                cp("dve", Vtm[:, t, :], bk[:, 0:vcols], [rb], [R("V")])

            PT = [rg.take([128, nkmax * 128], BF16) for _ in range(2)]
            RPT = [Res("PT%d" % i) for i in range(2)]
            OT = [rg.take([128, 128], BF16) for _ in range(4)]
            ROT = [Res("OT%d" % i) for i in range(4)]
            RDn = [rg.take([128, 128], F32) for _ in range(2)]
            RRD = [Res("RD%d" % i) for i in range(2)]
            tmp = rg.take([128, D], F32)
            rtmp = Res("updtmp")

            def keys_of(t):
                if t >= NLAT:
                    return [(16, None), (17, None)]
                if kind == "A":
                    ks = [(j, j - t + 1) for j in (t - 1, t, t + 1) if 0 <= j < NLAT]
                    ks = [(j, (b if b != 1 else None)) for (j, b) in ks]
                else:
                    ks = _na_keys(t)
                return ks + [(16, None), (17, None)]

            def bias_ap(pr, half, bid):
                if kind == "A":
                    return maskA[:, bid, :]
                return tab[:, 2 * pr + half, bid, :]

            qtiles = list(range(NLAT)) + ([16, 17] if need_ctx else [])
            work = [(t, pr, half) for t in qtiles for pr in range(2) for half in range(2)]

            def scores(i):
                t, pr, half = work[i]
                keys = keys_of(t)
                ps_ = slice(64 * half, 64 * half + 64)
                for kk, (j, bid) in enumerate(keys):
                    col = kk * 128
                    bi_ = 2 * (i % 2) + col // 512
                    c0 = col % 512
                    bk, rb = banks[bi_], RB[bi_]
                    has_b = bid is not None
                    mm(bk[:, c0:c0 + 128], Ks[pr][ps_, j * 128:j * 128 + 128], Qs[pr][ps_, t * 128:t * 128 + 128],
                       True, not has_b, [R("K%d" % pr), R("Q%d" % pr)], [rb])
                    if has_b:
                        mm(bk[:, c0:c0 + 128], ident_b[:], bias_ap(pr, half, bid), False, True,
                           [R("ident_b"), R("biasT"), R("maskA")], [rb])

            def expo(i):
                t, pr, half = work[i]
                ncol = len(keys_of(t)) * 128
                for q in range((ncol + 511) // 512):
                    n = min(512, ncol - q * 512)
                    bi_ = 2 * (i % 2) + q
                    act(PT[i % 2][:, q * 512:q * 512 + n], banks[bi_][:, 0:n], AF.Exp, [RB[bi_]], [RPT[i % 2]], scale=scale)

            def pv(i):
                t, pr, half = work[i]
                ip = i // 2
                keys = keys_of(t)
                nk = len(keys)
                ob, rob = banks[4 + (ip % 2)], RB[4 + (ip % 2)]
                ps_ = slice(64 * half, 64 * half + 64)
                if kind == "A":
                    vsl = slice(64 * half, 64 * half + 64)
                else:
                    h = 2 * pr + half
                    vsl = slice(64 * h, 64 * h + 64)
                for kk, (j, bid) in enumerate(keys):
                    p_ = PT[i % 2][:, kk * 128:kk * 128 + 128]
                    mm(ob[ps_, 0:128], Vtm[:, j, vsl], p_, kk == 0, kk == nk - 1, [R("V"), RPT[i % 2]], [rob])
                for kk, (j, bid) in enumerate(keys):
                    p_ = PT[i % 2][:, kk * 128:kk * 128 + 128]
                    mm(ob[ps_, 128:256], ones_b[:, 0:64], p_, kk == 0, kk == nk - 1, [R("ones_b"), RPT[i % 2]], [rob])
                if half == 0:
                    return
                rd, rrd = RDn[ip % 2], RRD[ip % 2]
                if kind == "A":
                    ts("dve", rd, ob[:, 128:256], esink[:, pr:pr + 1], ALU.add, [rob, R("esink")], [rrd])
                    S.op("dve", lambda e: e.reciprocal(out=rd, in_=rd), [rrd], [rrd])
                else:
                    S.op("dve", lambda e: e.reciprocal(out=rd, in_=ob[:, 128:256]), [rob], [rrd])
                tt("dve", OT[ip % 4], ob[:, 0:128], rd, ALU.mult, [rob, rrd], [ROT[ip % 4]])
                if pr == 0:
                    return
                pending.append((t, ip))

            pending = []

            def proj_tile(t, ip):
                j = 0 if t < NLAT else 1
                o0, o1 = OT[(ip - 1) % 4], OT[ip % 4]
                ro0, ro1 = ROT[(ip - 1) % 4], ROT[ip % 4]
                for hf in range(2):
                    bk, rb = banks[6 + hf], RB[6 + hf]
                    sl = slice(hf * 512, hf * 512 + 512)
                    mm(bk[:, :], o0, wo[:, 0, sl], True, False, [ro0, R("wo")], [rb])
                    mm(bk[:, :], o1, wo[:, 1, sl], False, True, [ro1, R("wo")], [rb])
                x_update(t, banks[6][:, :], banks[7][:, :], RB[6], RB[7], 0 + j, tmp, rtmp)

            n = len(work)
            scores(0)
            for i in range(n):
                expo(i)
                if i + 1 < n:
                    scores(i + 1)
                if len(pending) and work[i][1:] == (0, 1):
                    proj_tile(*pending.pop(0))
                pv(i)
            while pending:
                proj_tile(*pending.pop(0))

        def bc_mid(ap2d, n):
            return ap2d.unsqueeze(1).to_broadcast([128, n, ap2d.shape[1]])

        def bc_last(ap2d, n):
            return ap2d.unsqueeze(2).to_broadcast([128, ap2d.shape[1], n])

        def ssd_phase(l, need_ctx):
            S.barrier()
            rg = region()
            xs_tm = rg.take([128, NT, 512], BF16)
            B_tm = rg.take([128, NT, 128], BF16)
            BT = rg.take([128, TOK], BF16)
            CT = rg.take([128, TOK], BF16)
            dt = rg.take([128, NT, 16], F32)
            da = rg.take([128, NT, 16], F32)
            acs = rg.take([128, NT, 16], F32)
            scw = rg.take([128, NT, 16], F32)
            etg = rg.take([128, NT, 8], F32)
            convw = rg.take([128, 30], F32)
            convb = rg.take([128, 6], F32)
            dtb_b = rg.take([128, 16], F32)
            a_b = rg.take([128, 16], F32)
            Db = rg.take([128, 8], F32)
            normg = rg.take([128, 4], F32)
            mark = rg.off
            load("sp", convw, convw_d[l], [R("convw")])
            load("sp", convb, convb_d[l], [R("convb")])
            load("sp", dtb_b, dtb_d[l:l + 1, :].partition_broadcast(128), [R("dtb_b")])
            load("sp", a_b, alog_d[l:l + 1, :].partition_broadcast(128), [R("a_b")])
            load("sp", Db, ssdd_d[l:l + 1, :].partition_broadcast(128), [R("Db")])
            load("sp", normg, normgT_d[l], [R("normg")])
            act(a_b, a_b, AF.Exp, [R("a_b")], [R("a_b")])
            ts("dve", a_b, a_b, -1.0, ALU.mult, [R("a_b")], [R("a_b")])

            wx = [rg.take([128, 8, 128], BF16) for _ in range(2)]
            Rwx = [Res("wx%d" % i) for i in range(2)]
            pre = rg.take([128, TOK], F32)
            acc = rg.take([128, TOK], F32)
            post = rg.take([128, TOK], BF16)
            SEGS = [(0, 2048), (2048, 2304)]
            for ci in range(6):
                i = ci % 2
                load("pool", wx[i], wview(w_in_d[l], 1024 + ci * 128, 1024 + ci * 128 + 128), [Rwx[i]], sem="wx%d" % i)
                for bi, (t0, ntile) in enumerate(BLOCKS):
                    n = ntile * 128
                    bk, rb = banks[bi % 4], RB[bi % 4]
                    for k in range(8):
                        mm(bk[:, 0:n], wx[i][:, k, :], HT[:, k, t0 * 128:t0 * 128 + n], k == 0, k == 7,
                           [Rwx[i]] + RH[t0:t0 + ntile], [rb])
                    act(pre[:, t0 * 128:t0 * 128 + n], bk[:, 0:n], AF.Copy, [rb], [R("pre")])
                for (a, b) in SEGS:
                    ts("dve", acc[:, a:b], pre[:, a:b], convw[:, ci * 5 + 2:ci * 5 + 3], ALU.mult, [R("pre"), R("convw"), R("convb")], [R("acc")],
                       s2=convb[:, ci:ci + 1], op1=ALU.add)
                    for kk in (0, 1, 3, 4):
                        s = kk - 2
                        lo = max(a, a - s)
                        hi = min(b, b - s)
                        stt(acc[:, lo:hi], pre[:, lo + s:hi + s], convw[:, ci * 5 + kk:ci * 5 + kk + 1], acc[:, lo:hi], ALU.mult, ALU.add,
                            [R("pre"), R("convw"), R("acc")], [R("acc")])
                if ci < 4 or ci == 4:
                    dst_fm = post if ci < 4 else BT
                    rdst = R("post") if ci < 4 else R("BT")
                else:
                    dst_fm, rdst = CT, R("CT")
                act(dst_fm, acc, AF.Silu, [R("acc")], [rdst])
                if ci <= 4:
                    for g0 in range(0, NT, 8):
                        ng = min(8, NT - g0)
                        bi_ = 4 + (g0 // 8) % 2 + 2 * (ci % 2)
                        bk, rb = banks[bi_], RB[bi_]
                        bv = bk.bitcast(BF16)
                        for q in range(ng):
                            t = g0 + q
                            tr(bv[:, q * 128:q * 128 + 128], dst_fm[:, t * 128:t * 128 + 128], ident_b[:], [rdst, R("ident_b")], [rb])
                        src = bv[:, 0:ng * 128].rearrange("p (q c) -> p q c", c=128)
                        if ci < 4:
                            cp("act_copy", xs_tm[:, g0:g0 + ng, ci * 128:ci * 128 + 128], src, [rb], [R("xs_tm")])
                        else:
                            cp("act_copy", B_tm[:, g0:g0 + ng, :], src, [rb], [R("B_tm")])

            if cfg.get("ssd_stop", 9) <= 1:
                return
            S.barrier()
            rg.off = mark
            zs = rg.take([128, NT, 512], BF16)
            mark2 = rg.off
            wz = rg.take([128, 8, 512], BF16)
            wdt = rg.take([128, 8, 16], BF16)
            tot = rg.take([128, NT, 16], F32)
            etot = rg.take([128, NT, 16], F32)
            load("pool", wz, wview(w_in_d[l], 512, 1024), [R("wz")], sem="wz")
            load("pool", wdt, wview(w_in_d[l], 1792, 1808), [R("wdt")], sem="wdt")
            for t in range(NT):
                for k in range(8):
                    mm(banks[0][:, t * 16:t * 16 + 16], HT[:, k, t * 128:t * 128 + 128], wdt[:, k, :], k == 0, k == 7,
                       [R("wdt"), RH[t]], [RB[0]])
            p0 = banks[0][:, 0:NT * 16].rearrange("p (t c) -> p t c", c=16)
            tt("dve", dt, p0, bc_mid(dtb_b, NT), ALU.add, [RB[0], R("dtb_b")], [R("dt")])
            act(dt, dt, AF.Exp, [R("dt")], [R("dt")])
            act(dt, dt, AF.Ln, [R("dt")], [R("dt")], bias=1.0, scale=1.0)
            tt("dve", da, dt, bc_mid(a_b, NT), ALU.mult, [R("dt"), R("a_b")], [R("da")])
            for t in range(NT):
                mm(banks[1][:, t * 16:t * 16 + 8], triU[:], da[:, t, 0:8], True, True, [R("triU"), R("da")], [RB[1]])
                mm(banks[1][:, t * 16 + 8:t * 16 + 16], triL[:], da[:, t, 8:16], True, True, [R("triL"), R("da")], [RB[1]])
                mm(banks[2][:, t * 16:t * 16 + 16], ones_f[:], da[:, t, :], True, True, [R("ones_f"), R("da")], [RB[2]])
            p1 = banks[1][:, 0:NT * 16].rearrange("p (t c) -> p t c", c=16)
            p2 = banks[2][:, 0:NT * 16].rearrange("p (t c) -> p t c", c=16)
            cp("dve", acs, p1, [RB[1]], [R("acs")])
            cp("dve", tot, p2, [RB[2]], [R("tot")])
            tt("dve", scw, tot, acs, ALU.subtract, [R("tot"), R("acs")], [R("scw")])
            act(scw, scw, AF.Exp, [R("scw")], [R("scw")])
            tt("dve", scw, scw, dt, ALU.mult, [R("scw"), R("dt")], [R("scw")])
            act(etot, tot, AF.Exp, [R("tot")], [R("etot")])
            for d_ in range(2):
                for g in range(2):
                    cp("dve", etg[64 * g:64 * g + 64, :, 4 * d_:4 * d_ + 4], etot[64 * g:64 * g + 64, :, 8 * d_ + 4 * g:8 * d_ + 4 * g + 4],
                       [R("etot")], [R("etg")])
            for t in range(NT):
                if t >= NLAT and not need_ctx:
                    continue
                bk, rb = banks[4 + t % 4], RB[4 + t % 4]
                for k in range(8):
                    mm(bk[:, :], HT[:, k, t * 128:t * 128 + 128], wz[:, k, :], k == 0, k == 7, [R("wz"), RH[t]], [rb])
                act(zs[:, t, :], bk[:, :], AF.Silu, [rb], [R("zs")])

            if cfg.get("ssd_stop", 9) <= 2:
                return
            S.barrier()
            rg.off = mark2
            prevB = rg.take([128, NT, 256], BF16)
            woS = rg.take([128, 4, D], BF16)
            load("pool", woS, w_outP_d[l][256:768, :].rearrange("(k p) n -> p k n", p=128), [R("woS")], sem="woS")
            hr = ht_region()
            xw = [hr.take([128, 512], BF16) for _ in range(2)]
            Rxw = [Res("xw%d" % i) for i in range(2)]
            xdt = [hr.take([128, 512], BF16) for _ in range(2)]
            Rxdt = [Res("xdt%d" % i) for i in range(2)]
            rhsU = [hr.take([128, 8, 128], F32) for _ in range(2)]
            RrhsU = [Res("rhsU%d" % i) for i in range(2)]
            E = [hr.take([128, 8, 128], BF16) for _ in range(2)]
            RE = [Res("E%d" % i) for i in range(2)]
            Eb = [hr.take([128, 8, 128], BF16) for _ in range(2)]
            REb = [Res("Eb%d" % i) for i in range(2)]
            Gm = [hr.take([128, 2, 128], BF16) for _ in range(2)]
            RGm = [Res("Gm%d" % i) for i in range(2)]
            nacs = hr.take([128, NT, 16], F32)
            Sf = hr.take([128, 256], F32)
            Sf_bf = hr.take([128, 256], BF16)
            Sb = hr.take([128, 256], F32)
            tmpD = hr.take([128, 512], F32)
            ytot = hr.take([128, 512], F32)
            yn = hr.take([128, 512], BF16)
            oT = hr.take([128, 4, 128], BF16)
            upd = hr.take([128, 512], F32)
            sjunk = hr.take([128, 512], BF16)
            ssq = hr.take([128, 64], F32)
            rupd = Res("updS")
            for t in range(NT):
                RH[t] = Res("H%d" % t)

            memset("dve", Sb, 0.0, [R("Sb")])
            memset("dve", Sf, 0.0, [R("Sf")])
            memset("dve", Sf_bf, 0.0, [R("Sf_bf")])
            ts("dve", nacs, acs, -1.0, ALU.mult, [R("acs")], [R("nacs")])

            def xs3(c):
                return xs_tm[:, c, :].rearrange("p (h q) -> p h q", q=64)

            STB = 6

            def states(c, d_, i, Sacc, rS):
                tt("pool", xw[i][:].rearrange("p (h q) -> p h q", q=64), xs3(c), bc_last(scw[:, c, 8 * d_:8 * d_ + 8], 64), ALU.mult,
                   [R("xs_tm"), R("scw")], [Rxw[i]])
                for g in range(2):
                    mm(banks[STB][64 * g:64 * g + 64, 128:384], B_tm[:, c, 64 * g:64 * g + 64], xw[i][:, 256 * g:256 * g + 256], True, True,
                       [R("B_tm"), Rxw[i]], [RB[STB]])
                for hl in range(4):
                    sl = slice(64 * hl, 64 * hl + 64)
                    stt(Sacc[:, sl], Sacc[:, sl], etg[:, c, 4 * d_ + hl:4 * d_ + hl + 1], banks[STB][:, 128 + 64 * hl:128 + 64 * hl + 64], ALU.mult, ALU.add,
                        [rS, R("etg"), RB[STB]], [rS])

            for n_, c in enumerate([17, 16] + list(range(15, -1, -1))):
                cp("act_copy", prevB[:, c, :], Sb, [R("Sb")], [R("prevB")])
                if c != 0:
                    states(c, 1, n_ % 2, Sb, R("Sb"))

            order2 = [16, 17] + list(range(NLAT))

            def front(n_, c):
                emit_out = need_ctx or c < NLAT
                csl = slice(c * 128, c * 128 + 128)
                if emit_out:
                    gbanks = ((banks[5][:, 0:128], RB[5]), (banks[6][:, 0:128], RB[6]))
                    for g in range(2):
                        ps_ = slice(64 * g, 64 * g + 64)
                        mm(gbanks[g][0], BT[ps_, csl], CT[ps_, csl], True, True, [R("BT"), R("CT")], [gbanks[g][1]])
                    for d_ in range(2):
                        tri = triU if d_ == 0 else triL
                        for g in range(2):
                            tt("dve", Gm[d_][:, g, :], gbanks[g][0], tri[:], ALU.mult, [gbanks[g][1], R("triU"), R("triL")], [RGm[d_]])
                    for d_ in range(2):
                        tri = triU if d_ == 0 else triL
                        tt("pool", rhsU[d_], bc_mid(tri[:], 8), bc_last(da[:, c, 8 * d_:8 * d_ + 8], 128), ALU.mult,
                           [R("triU"), R("triL"), R("da")], [RrhsU[d_]])
                    for d_ in range(2):
                        for h in range(8):
                            bi_ = 2 * d_ + h // 4
                            mm(banks[bi_][:, (h % 4) * 128:(h % 4) * 128 + 128], ones_f[:], rhsU[d_][:, h, :], True, True,
                               [R("ones_f"), RrhsU[d_]], [RB[bi_]])
                    for d_ in range(2):
                        tt("dve", xdt[d_][:].rearrange("p (h q) -> p h q", q=64), xs3(c), bc_last(dt[:, c, 8 * d_:8 * d_ + 8], 64), ALU.mult,
                           [R("xs_tm"), R("dt")], [Rxdt[d_]])
                    for d_ in range(2):
                        for q in range(2):
                            bi_ = 2 * d_ + q
                            for h in range(4 * q, 4 * q + 4):
                                act(E[d_][:, h, :], banks[bi_][:, (h % 4) * 128:(h % 4) * 128 + 128], AF.Exp, [RB[bi_], R("nacs")], [RE[d_]],
                                    bias=nacs[:, c, 8 * d_ + h:8 * d_ + h + 1], scale=1.0)
                            act(Eb[d_][:, 4 * q:4 * q + 4, :].rearrange("p h i -> p (h i)"), banks[bi_][:, :], AF.Exp, [RB[bi_]], [REb[d_]])
                    for d_ in range(2):
                        for g in range(2):
                            stt(E[d_][:, 4 * g:4 * g + 4, :], E[d_][:, 4 * g:4 * g + 4, :], 1.0, bc_mid(Gm[d_][:, g, :], 4), ALU.min, ALU.mult,
                                [RE[d_], RGm[d_]], [RE[d_]])
                        tt("pool", Eb[d_], Eb[d_], bc_mid(CT[:, csl], 8), ALU.mult, [REb[d_], R("CT")], [REb[d_]])

            def front_y(n_, c):
                emit_out = need_ctx or c < NLAT
                if emit_out:
                    for d_ in range(2):
                        for h in range(8):
                            g, hl = h // 4, h % 4
                            ysl = slice(64 * h, 64 * h + 64)
                            mm(banks[4][:, ysl], E[d_][:, h, :], xdt[d_][:, ysl], d_ == 0 and h == 0, False, [RE[d_], Rxdt[d_]], [RB[4]], sgc=True)
                            st_ = Sf_bf if d_ == 0 else prevB[:, c, :]
                            rst = R("Sf_bf") if d_ == 0 else R("prevB")
                            mm(banks[4][:, ysl], Eb[d_][64 * g:64 * g + 64, h, :], st_[64 * g:64 * g + 64, 64 * hl:64 * hl + 64], False, d_ == 1,
                               [REb[d_], rst], [RB[4]], sgc=True)
                if c != NLAT - 1:
                    states(c, 0, n_ % 2, Sf, R("Sf"))
                    cp("act_copy", Sf_bf, Sf, [R("Sf")], [R("Sf_bf")])

            def back1(n_, c):
                emit_out = need_ctx or c < NLAT
                if not emit_out:
                    return
                tt("dve", tmpD[:].rearrange("p (h q) -> p h q", q=64), xs3(c), bc_last(Db, 64), ALU.mult, [R("xs_tm"), R("Db")], [R("tmpD")])
                tt("dve", ytot, banks[4][:, :], tmpD, ALU.add, [RB[4], R("tmpD")], [R("ytot")])

            def back(n_, c):
                emit_out = need_ctx or c < NLAT
                if not emit_out:
                    return
                j = 0 if c < NLAT else 1
                tt("dve", ytot, ytot, zs[:, c, :], ALU.mult, [R("ytot"), R("zs")], [R("ytot")])
                act(sjunk, ytot, AF.Square, [R("ytot")], [R("sjunk"), R("ssq")], accum_out=ssq[:, c:c + 1])
                act(ssq[:, 32 + c:33 + c], ssq[:, c:c + 1], AF.Ln, [R("ssq")], [R("ssq")], bias=1e-6, scale=1.0 / 512)
                act(ssq[:, 32 + c:33 + c], ssq[:, 32 + c:33 + c], AF.Exp, [R("ssq")], [R("ssq")], scale=-0.5)
                ts("dve", yn, ytot, ssq[:, 32 + c:33 + c], ALU.mult, [R("ytot"), R("ssq")], [R("yn")])
                b5 = banks[5].bitcast(BF16)
                for kc in range(4):
                    tr(b5[:, 512 + kc * 128:512 + kc * 128 + 128], yn[:, kc * 128:kc * 128 + 128], ident_b[:], [R("yn"), R("ident_b")], [RB[5]])
                for kc in range(4):
                    ts("dve", oT[:, kc, :], b5[:, 512 + kc * 128:512 + kc * 128 + 128], normg[:, kc:kc + 1], ALU.mult, [RB[5], R("normg")], [R("oT")])
                for hf in range(2):
                    sl = slice(hf * 512, hf * 512 + 512)
                    for kc in range(4):
                        mm(banks[7][:, :], oT[:, kc, :], woS[:, kc, sl], kc == 0, kc == 3, [R("oT"), R("woS")], [RB[7]])
                    tt("dve", upd, banks[7][:, :], GATE[:, j, sl], ALU.mult, [RB[7], R("GATE")], [rupd])
                    tt("dve", X[:, c, sl], X[:, c, sl], upd, ALU.add, [rupd, RX[c]], [RX[c]])

            front(0, order2[0])
            front_y(0, order2[0])
            for n_, c in enumerate(order2):
                if n_ + 1 < len(order2):
                    front(n_ + 1, order2[n_ + 1])
                back1(n_, c)
                if n_ + 1 < len(order2):
                    front_y(n_ + 1, order2[n_ + 1])
                back(n_, c)


        def tree(op, dst, src, width, n1, tmpbuf):
            cur = src
            w = width
            while w > 1:
                h = w // 2
                out = dst.unsqueeze(2) if h == 1 else tmpbuf[:, :, 0:h]
                tt("dve", out, cur[:, :, 0:h], cur[:, :, h:w], op, [R("rt")], [R("rt")])
                cur = out
                w = h

        def moe_phase(l, need_ctx):
            tiles = list(range(NT)) if need_ctx else list(range(NLAT))
            ntl = len(tiles)
            S.barrier()
            rg = region()
            comb = rg.take([128, NT, 32], F32)
            lg = rg.take([128, NT, 36], F32)
            mark = rg.off
            w_rt = rg.take([128, 8, 36], F32)
            brt = rg.take([128, 36], F32)
            load("sp", w_rt, w_rt_d[l].rearrange("(k p) n -> p k n", p=128), [R("w_rt")])
            load("sp", brt, b_rt_d[l:l + 1, :].partition_broadcast(128), [R("brt")])

            def router(t, hf, rhf):
                bi_ = 4 + t // 9
                c0 = (t % 9) * 36
                for k in range(8):
                    mm(banks[bi_][:, c0:c0 + 36], hf[:, k, :], w_rt[:, k, :], k == 0, k == 7, [rhf, R("w_rt")], [RB[bi_]])

            norm_phase(1, tiles, rg=rg, router=router)
            for q in range(2):
                t0, t1 = 9 * q, min(9 * q + 9, ntl)
                if t1 <= t0:
                    continue
                n_ = t1 - t0
                src = banks[4 + q][:, 0:n_ * 36].rearrange("p (t c) -> p t c", c=36)
                tt("dve", lg[:, t0:t1, :], src, bc_mid(brt, n_), ALU.add, [RB[4 + q], R("brt")], [R("rt")])
            S.barrier()
            rg.off = mark
            gl = lg[:, 0:ntl, 0:4]
            el = lg[:, 0:ntl, 4:36]
            t4 = rg.take([128, NT, 4], F32)
            t32 = rg.take([128, NT, 32], F32)
            elm = rg.take([128, NT, 32], F32)
            m1b = rg.take([128, NT, 32], F32)
            m2b = rg.take([128, NT, 32], F32)
            gmax = rg.take([128, NT], F32)
            gw = rg.take([128, NT], F32)
            m1 = rg.take([128, NT], F32)
            m2 = rg.take([128, NT], F32)
            w1 = rg.take([128, NT], F32)
            w2 = rg.take([128, NT], F32)
            RT = [R("rt")]
            n = ntl
            tree(ALU.max, gmax[:, 0:n], gl, 4, n, t4[:, 0:n, :])
            tt("dve", t4[:, 0:n, :], gl, bc_last(gmax[:, 0:n], 4), ALU.subtract, RT, RT)
            act(t4[:, 0:n, :], t4[:, 0:n, :], AF.Exp, RT, RT)
            tree(ALU.add, gw[:, 0:n], t4[:, 0:n, :], 4, n, t32[:, 0:n, 0:4])
            S.op("dve", lambda e: e.reciprocal(out=gw[:, 0:n], in_=gw[:, 0:n]), RT, RT)
            tt("dve", t4[:, 0:n, :], gl, bc_last(gmax[:, 0:n], 4), ALU.is_equal, RT, RT)
            ts("dve", t4[:, 0:n, :], t4[:, 0:n, :], -1.0, ALU.add, RT, RT, s2=-NEG, op1=ALU.mult)
            for g in range(4):
                tt("dve", elm[:, 0:n, 8 * g:8 * g + 8], el[:, :, 8 * g:8 * g + 8], t4[:, 0:n, g:g + 1].to_broadcast([128, n, 8]), ALU.add, RT, RT)
            tree(ALU.max, m1[:, 0:n], elm[:, 0:n, :], 32, n, t32[:, 0:n, :])
            tt("dve", m1b[:, 0:n, :], elm[:, 0:n, :], bc_last(m1[:, 0:n], 32), ALU.is_equal, RT, RT)
            stt(elm[:, 0:n, :], m1b[:, 0:n, :], 2 * NEG, elm[:, 0:n, :], ALU.mult, ALU.add, RT, RT)
            tree(ALU.max, m2[:, 0:n], elm[:, 0:n, :], 32, n, t32[:, 0:n, :])
            tt("dve", m2b[:, 0:n, :], elm[:, 0:n, :], bc_last(m2[:, 0:n], 32), ALU.is_equal, RT, RT)
            tt("dve", w2[:, 0:n], m2[:, 0:n], m1[:, 0:n], ALU.subtract, RT, RT)
            act(w2[:, 0:n], w2[:, 0:n], AF.Exp, RT, RT)
            ts("dve", w1[:, 0:n], w2[:, 0:n], 1.0, ALU.add, RT, RT)
            S.op("dve", lambda e: e.reciprocal(out=w1[:, 0:n], in_=w1[:, 0:n]), RT, RT)
            tt("dve", w1[:, 0:n], w1[:, 0:n], gw[:, 0:n], ALU.mult, RT, RT)
            tt("dve", w2[:, 0:n], w2[:, 0:n], w1[:, 0:n], ALU.mult, RT, RT)
            tt("dve", m1b[:, 0:n, :], m1b[:, 0:n, :], bc_last(w1[:, 0:n], 32), ALU.mult, RT, RT)
            tt("dve", m2b[:, 0:n, :], m2b[:, 0:n, :], bc_last(w2[:, 0:n], 32), ALU.mult, RT, RT)
            tt("dve", comb[:, 0:n, :], m1b[:, 0:n, :], m2b[:, 0:n, :], ALU.add, RT, [R("comb")])
            S.barrier()
            rg.off = mark
            NS = 3
            WGU = [rg.take([128, 8, 512], BF16) for _ in range(NS)]
            WD = [rg.take([128, 2, D], BF16) for _ in range(NS)]
            WDx = [rg.take([128, 2, D], BF16) for _ in range(NS)]
            WDc = [rg.take([128, 2, D], BF16) for _ in range(NS)]
            Rgu = [Res("wgu%d" % i) for i in range(NS)]
            Rwd = [Res("wd%d" % i) for i in range(NS)]
            Rwdx = [Res("wdx%d" % i) for i in range(NS)]
            s_sb = [rg.take([128, 256], F32) for _ in range(2)]
            Rs = [Res("s%d" % i) for i in range(2)]
            hid = [rg.take([128, 256], BF16) for _ in range(2)]
            Rhid = [Res("hid%d" % i) for i in range(2)]
            hidT = [rg.take([128, 2, 128], BF16) for _ in range(2)]
            RhT = [Res("hidT%d" % i) for i in range(2)]

            def load_expert(e):
                sl = e % NS
                S.dma("pool", [lambda en: en.dma_start(out=WGU[sl][:, :, 0:256], in_=wg_d[l, e].rearrange("(k p) n -> p k n", p=128)),
                               lambda en: en.dma_start(out=WGU[sl][:, :, 256:512], in_=wu_d[l, e].rearrange("(k p) n -> p k n", p=128))],
                      "wgu%d" % sl, [], [Rgu[sl]])
                S.dma("pool", [lambda en: en.dma_start(out=WD[sl], in_=wd_d[l, e].rearrange("(k p) n -> p k n", p=128))],
                      "wd%d" % sl, [], [Rwd[sl]])
                for fc in range(2):
                    tt("pool", WDx[sl][:, fc, :], WD[sl][:, fc, :], GATE[:, 2, :], ALU.mult, [Rwd[sl], R("GATE")], [Rwdx[sl]])
                    if need_ctx:
                        tt("pool", WDc[sl][:, fc, :], WD[sl][:, fc, :], GATE[:, 3, :], ALU.mult, [Rwd[sl], R("GATE")], [Rwdx[sl]])

            items = [(e, t) for e in range(32) for t in tiles]
            nit = len(items)
            b2 = banks[2].bitcast(BF16)

            def stageG(i):
                e, t = items[i]
                sl = e % NS
                bk, rb = banks[i % 2], RB[i % 2]
                for k in range(8):
                    mm(bk[:, :], HT[:, k, t * 128:t * 128 + 128], WGU[sl][:, k, :], k == 0, k == 7, [RH[t], Rgu[sl]], [rb])
                act(s_sb[i % 2], bk[:, 0:256], AF.Silu, [rb], [Rs[i % 2]])
                stt(hid[i % 2], bk[:, 256:512], comb[:, t, e:e + 1], s_sb[i % 2], ALU.mult, ALU.mult, [rb, R("comb"), Rs[i % 2]], [Rhid[i % 2]])

            def stageT(i):
                for fc in range(2):
                    tr(b2[:, (i % 2) * 256 + fc * 128:(i % 2) * 256 + fc * 128 + 128], hid[i % 2][:, fc * 128:fc * 128 + 128], ident_b[:],
                       [Rhid[i % 2], R("ident_b")], [RB[2]])
                cp("act_copy", hidT[i % 2][:].rearrange("p a b -> p (a b)"), b2[:, (i % 2) * 256:(i % 2) * 256 + 256], [RB[2]], [RhT[i % 2]])

            def stageD(i):
                e, t = items[i]
                sl = e % NS
                wdd = WDx[sl] if t < NLAT else WDc[sl]
                for fc in range(2):
                    for hf in range(2):
                        bi_ = 4 + 2 * (i % 2) + hf
                        mm(banks[bi_][:, :], hidT[i % 2][:, fc, :], wdd[:, fc, hf * 512:hf * 512 + 512], fc == 0, fc == 1,
                           [RhT[i % 2], Rwdx[sl]], [RB[bi_]])
                for hf in range(2):
                    bi_ = 4 + 2 * (i % 2) + hf
                    sl_ = slice(hf * 512, hf * 512 + 512)
                    tt("dve", X[:, t, sl_], X[:, t, sl_], banks[bi_][:, :], ALU.add, [RX[t], RB[bi_]], [RX[t]])

            for e in range(NS):
                load_expert(e)
            for i in range(nit + 2):
                if i < nit:
                    stageG(i)
                if 0 <= i - 1 < nit:
                    stageT(i - 1)
                if 0 <= i - 2 < nit:
                    stageD(i - 2)
                    e, t = items[i - 2]
                    if t == tiles[-1] and e + NS < 32:
                        load_expert(e + NS)


        def final_phase():
            S.barrier()
            rg = region()
            gfb = rg.take([128, D], F32)
            junk = rg.take([128, D], BF16)
            load("sp", gfb, g_final_d.partition_broadcast(128), [R("gfb")])
            ot = [rg.take([128, D], F32) for _ in range(2)]
            rot = [Res("fo%d" % i) for i in range(2)]
            ov = out_d.rearrange("(t p) d -> p t d", p=128)
            for t in range(NLAT):
                b = t % 2
                if final_norm:
                    act(junk[:], X[:, t, :], AF.Square, [RX[t]], [R("junk"), R("ss%d" % t)], accum_out=small[:, t:t + 1])
                    act(small[:, 32 + t:33 + t], small[:, t:t + 1], AF.Ln, [R("ss%d" % t)], [R("rs%d" % t)], bias=1e-6, scale=1.0 / D)
                    act(small[:, 32 + t:33 + t], small[:, 32 + t:33 + t], AF.Exp, [R("rs%d" % t)], [R("rs%d" % t)], scale=-0.5)
                    stt(ot[b], X[:, t, :], small[:, 32 + t:33 + t], gfb, ALU.mult, ALU.mult, [RX[t], R("rs%d" % t), R("gfb")], [rot[b]])
                else:
                    cp("dve", ot[b], X[:, t, :], [RX[t]], [rot[b]])
                S.dma("sp", [lambda e, b=b, t=t: e.dma_start(out=ov[:, t, :], in_=ot[b])], "outst", [rot[b]], [])
            if dbg_d is not None:
                dv = dbg_d.rearrange("(t p) d -> p t d", p=128)
                for t in range(2):
                    S.dma("sp", [lambda e, t=t: e.dma_start(out=dv[:, t, :], in_=X[:, NLAT + t, :])], "outst", [RX[NLAT + t]], [])
            S.streams["sp"].append(("wait", ("dma", "outst"), S.dmacnt[("dma", "outst")]))

        for l in range(nlayers):
            need_ctx = l < DEPTH - 1
            mod_phase(l)
            S.barrier()
            norm_phase(0, list(range(NT)))
            if "A" in phases:
                attn_phase("A", l, need_ctx)
            if "N" in phases:
                attn_phase("N", l, need_ctx)
            if "S" in phases:
                ssd_phase(l, need_ctx)
            if "M" in phases:
                moe_phase(l, need_ctx)
        final_phase()
        S.emit()
    return nc


def _prep_shared(inp):
    f = np.float32
    sh = {}
    sh["w_mod"] = np.ascontiguousarray(inp["w_mod"], f)
    sh["b_mod"] = np.ascontiguousarray(inp["b_mod"], f)
    sh["b_modT"] = np.ascontiguousarray(inp["b_mod"].reshape(DEPTH, 48, 128).transpose(0, 2, 1), f)
    sh["g_mixT"] = np.ascontiguousarray(inp["g_mix"].reshape(DEPTH, 8, 128).transpose(0, 2, 1), f)
    sh["g_ffnT"] = np.ascontiguousarray(inp["g_ffn"].reshape(DEPTH, 8, 128).transpose(0, 2, 1), f)
    sh["g_final"] = np.ascontiguousarray(inp["g_final"].reshape(1, D), f)
    w_in = np.asarray(inp["w_in"], f)
    sw = _swap_idx()
    qcol = lambda h: np.arange(64 * h, 64 * h + 64)
    kcol = lambda g: 256 + np.arange(64 * g, 64 * g + 64)
    Q02 = np.concatenate([qcol(0), qcol(2)])
    Q13 = np.concatenate([qcol(1), qcol(3)])
    K01 = np.concatenate([kcol(0), kcol(1)])
    Q02s = np.concatenate([qcol(0)[sw], qcol(2)[sw]])
    Q13s = np.concatenate([qcol(1)[sw], qcol(3)[sw]])
    K01s = np.concatenate([kcol(0)[sw], kcol(1)[sw]])
    Vc = 384 + np.arange(128)
    colsA = np.concatenate([Q02, Q13, K01, Q02s, Q13s, K01s, Vc])
    sh["w_inA"] = np.ascontiguousarray(w_in[:, :, colsA])
    sh["w_in"] = np.ascontiguousarray(w_in)
    rows = np.concatenate([np.arange(0, 64), np.arange(128, 192), np.arange(64, 128), np.arange(192, 256), np.arange(256, 1024)])
    sh["w_outP"] = np.ascontiguousarray(np.asarray(inp["w_out"], f)[:, rows, :])
    sk = np.asarray(inp["attn_sink"], f)
    sinkE = np.zeros((DEPTH, 128, 2), f)
    sinkE[:, 0:64, 0] = sk[:, 0:1]; sinkE[:, 64:128, 0] = sk[:, 2:3]
    sinkE[:, 0:64, 1] = sk[:, 1:2]; sinkE[:, 64:128, 1] = sk[:, 3:4]
    sh["sinkE"] = sinkE
    cw = np.asarray(inp["ssd_conv_w"], f)
    sh["convw"] = np.ascontiguousarray(cw.reshape(DEPTH, 5, 6, 128).transpose(0, 3, 2, 1).reshape(DEPTH, 128, 30))
    sh["convb"] = np.ascontiguousarray(np.asarray(inp["ssd_conv_b"], f).reshape(DEPTH, 6, 128).transpose(0, 2, 1))
    sh["dtb"] = np.ascontiguousarray(np.asarray(inp["ssd_dt_bias"], f).reshape(DEPTH, 16))
    sh["alog"] = np.ascontiguousarray(np.asarray(inp["ssd_a_log"], f).reshape(DEPTH, 16))
    sh["ssdd"] = np.ascontiguousarray(np.asarray(inp["ssd_d"], f))
    sh["normgT"] = np.ascontiguousarray(np.asarray(inp["ssd_norm_g"], f).reshape(DEPTH, 4, 128).transpose(0, 2, 1))
    rpb = np.asarray(inp["na_rpb"], f)
    sh["naTab"] = np.stack([_na_table(rpb[l]).reshape(128, 6144) for l in range(DEPTH)], 0)
    sh["w_rt"] = np.ascontiguousarray(np.concatenate([np.asarray(inp["w_router_group"], f), np.asarray(inp["w_router_expert"], f)], -1))
    sh["b_rt"] = np.ascontiguousarray(np.concatenate([np.asarray(inp["b_router_group"], f), np.asarray(inp["b_router_expert"], f)], -1))
    sh["w_exp_gate"] = np.ascontiguousarray(np.asarray(inp["w_exp_gate"], f).reshape(DEPTH, 32, D, 256))
    sh["w_exp_up"] = np.ascontiguousarray(np.asarray(inp["w_exp_up"], f).reshape(DEPTH, 32, D, 256))
    sh["w_exp_down"] = np.ascontiguousarray(np.asarray(inp["w_exp_down"], f).reshape(DEPTH, 32, 256, D))
    ct = _const_tables()
    ct["maskA"] = ct["maskA"].reshape(128, 384)
    sh.update(ct)
    return sh


def _run(inp, cfg, cores=None):
    sh = _prep_shared(inp)
    x = np.asarray(inp["x"], np.float32)
    c = np.asarray(inp["c"], np.float32)
    ctx = np.asarray(inp["ctx"], np.float32)
    c_ctx = np.asarray(inp["c_ctx"], np.float32)
    cores = list(range(8)) if cores is None else cores
    in_maps = []
    for b in cores:
        m = dict(sh)
        m["x"] = np.ascontiguousarray(x[b])
        m["ctx"] = np.ascontiguousarray(ctx[b])
        scT = np.zeros((128, 8, 2), np.float32)
        scT[:, :, 0] = c[b].reshape(8, 128).T
        scT[:, :, 1] = c_ctx.reshape(8, 128).T
        m["scT"] = scT.reshape(128, 16)
        in_maps.append(m)
    nc = build_program(cfg)
    res = run_bass_kernel_spmd(nc, in_maps, core_ids=list(range(len(cores))))
    return res


def kernel(**inputs):
    res = _run(inputs, {})
    return np.stack([r["out"] for r in res.results], 0).astype(np.float32)
```

```python
import contextlib
import math
import numpy as np
import concourse.bass as bass
import concourse.mybir as mybir
from concourse.bass_utils import run_bass_kernel_spmd

F32 = mybir.dt.float32
BF16 = mybir.dt.bfloat16
AF = mybir.ActivationFunctionType
ALU = mybir.AluOpType

EPOCH = 24000
NEG = -30000.0
D = 1024
NLAT = 16
NT = 18
TOK = NT * 128
DEPTH = 4
REGN = 37632


class Res:
    __slots__ = ("name", "w", "r", "psum")

    def __init__(self, name="", psum=False):
        self.name = name
        self.w = None
        self.r = []
        self.psum = psum


class Sched:
    ENGS = ("pe", "act", "dve", "pool", "sp")

    def __init__(self, nc):
        self.nc = nc
        self.streams = {e: [] for e in self.ENGS}
        self.cnt = {e: 0 for e in self.ENGS}
        self.waited = {e: {} for e in self.ENGS}
        self.semkeys = []
        self.semset = set()
        self.dmacnt = {}
        self.last = {}

    def _need(self, key):
        if key not in self.semset:
            self.semset.add(key)
            self.semkeys.append(key)

    def _wait(self, eng, k, v):
        if k[0] == eng and eng == "pe":
            return
        wd = self.waited[eng]
        if wd.get(k, 0) < v:
            wd[k] = v
            self.streams[eng].append(("wait", k, v))

    def _deps(self, eng, reads, writes):
        for r in reads:
            if r.w is not None:
                self._wait(eng, *r.w)
        for w in writes:
            if w.w is not None:
                self._wait(eng, *w.w)
            for t in w.r:
                self._wait(eng, *t)

    def _post(self, tok, reads, writes):
        self.last[tok[0]] = tok[1]
        for r in reads:
            r.r.append(tok)
            if len(r.r) > 64:
                best = {}
                for (k, v) in r.r:
                    if best.get(k, 0) < v:
                        best[k] = v
                r.r = list(best.items())
        for w in writes:
            w.w = tok
            w.r = []

    def op(self, eng, fn, reads=(), writes=()):
        if eng != "pe":
            ex = [r for r in reads if r.psum]
            if ex:
                writes = list(writes) + ex
        self._deps(eng, reads, writes)
        c = self.cnt[eng]
        key = (eng, c // EPOCH)
        val = c % EPOCH + 1
        self._need(key)
        self.cnt[eng] = c + 1
        self.streams[eng].append(("op", fn, key, 1))
        tok = (key, val)
        self._post(tok, reads, writes)
        return tok

    def dma(self, eng, fns, semname, reads=(), writes=()):
        self._deps(eng, reads, writes)
        key = ("dma", semname)
        self._need(key)
        c = self.dmacnt.get(key, 0)
        for fn in fns:
            self.streams[eng].append(("op", fn, key, 16))
            c += 16
        self.dmacnt[key] = c
        tok = (key, c)
        self._post(tok, reads, writes)
        return tok

    def barrier(self):
        toks = list(self.last.items())
        for eng in self.ENGS:
            for (k, v) in toks:
                self._wait(eng, k, v)

    def emit(self):
        nc = self.nc
        with contextlib.ExitStack() as es:
            sems = {}
            for i, k in enumerate(self.semkeys):
                sems[k] = es.enter_context(nc.semaphore("s%d" % i))
            block = es.enter_context(nc.Block())

            def runner(stream):
                def f(e):
                    for it in stream:
                        if it[0] == "wait":
                            e.wait_ge(sems[it[1]], it[2])
                        else:
                            it[1](e).then_inc(sems[it[2]], it[3])
                return f

            block.tensor(runner(self.streams["pe"]))
            block.scalar(runner(self.streams["act"]))
            block.vector(runner(self.streams["dve"]))
            block.gpsimd(runner(self.streams["pool"]))
            block.sync(runner(self.streams["sp"]))


def _rope_tables():
    t = np.arange(2048)
    rows = (t // 64).astype(np.float64)
    cols = (t % 64).astype(np.float64)
    inv = 1.0 / (10000.0 ** (np.arange(0, 32, 2, dtype=np.float64) / 32.0))
    C = np.zeros((64, 2048), np.float64)
    S = np.zeros((64, 2048), np.float64)
    ar = rows[None, :] * inv[:, None]
    ac = cols[None, :] * inv[:, None]
    C[0:16] = np.cos(ar); C[16:32] = np.cos(ar); C[32:48] = np.cos(ac); C[48:64] = np.cos(ac)
    S[0:16] = -np.sin(ar); S[16:32] = np.sin(ar); S[32:48] = -np.sin(ac); S[48:64] = np.sin(ac)
    C = np.concatenate([C, C], 0).astype(np.float32)
    S = np.concatenate([S, S], 0).astype(np.float32)
    return C, S


def _swap_idx():
    i = np.arange(64)
    return np.where(i < 16, i + 16, np.where(i < 32, i - 16, np.where(i < 48, i + 16, i - 16)))


def _const_tables():
    k = np.arange(128)[:, None]
    i = np.arange(128)[None, :]
    c = {}
    c["ident"] = np.eye(128, dtype=np.float32)
    c["triU"] = (k <= i).astype(np.float32)
    c["triL"] = (k >= i).astype(np.float32)
    c["maskF"] = np.where(k <= i, 0.0, NEG).astype(np.float32)
    c["maskB"] = np.where(k >= i, 0.0, NEG).astype(np.float32)
    mA = np.zeros((128, 3, 128), np.float32)
    mA[:, 0, :] = np.where(i <= k, 0.0, NEG)
    mA[:, 2, :] = np.where(k <= i, 0.0, NEG)
    c["maskA"] = mA
    C, S = _rope_tables()
    c["ropeC"] = C
    c["ropeS"] = S
    return c


def _na_table(rpb):
    kr = (np.arange(128) // 64)[:, None]
    kc = (np.arange(128) % 64)[:, None]
    qr = (np.arange(128) // 64)[None, :]
    qc = (np.arange(128) % 64)[None, :]
    cs = np.clip(qc - 8, 0, 48)
    colv = (kc >= cs) & (kc < cs + 16)
    coff = np.clip(kc - qc, -15, 15) + 15
    out = np.full((128, 4, 12, 128), NEG, np.float32)
    for v in range(12):
        if v < 5:
            dj = v - 2
            dr = 2 * dj + kr - qr
            rowv = (dr >= -4) & (dr <= 3)
        else:
            dj = v - 5 - 3
            dr = 2 * dj + kr - qr
            rowv = np.abs(dr) <= 7
        valid = rowv & colv
        drc = np.clip(dr + 7, 0, 14)
        for h in range(4):
            g = rpb[h][drc, coff]
            out[:, h, v, :] = np.where(valid, g, np.float32(NEG))
    return out


def _na_keys(t):
    if t < 2:
        return [(j, 5 + (j - t) + 3) for j in range(0, 4)]
    if t >= 14:
        return [(j, 5 + (j - t) + 3) for j in range(12, 16)]
    return [(j, (j - t) + 2) for j in range(t - 2, t + 3)]


def build_program(cfg):
    nlayers = cfg.get("nlayers", DEPTH)
    phases = cfg.get("phases", "ANSM")
    final_norm = cfg.get("final_norm", True)

    nc = bass.Bass("TRN2", target_bir_lowering=False)

    def din(name, shape):
        return nc.dram_tensor(name, list(shape), F32, kind="ExternalInput").ap()

    x_d = din("x", [2048, D])
    ctx_d = din("ctx", [256, D])
    scT_d = din("scT", [128, 16])
    w_mod_d = din("w_mod", [DEPTH, D, 6 * D])
    b_modT_d = din("b_modT", [DEPTH, 128, 48])
    b_mod_d = din("b_mod", [DEPTH, 6 * D])
    g_mixT_d = din("g_mixT", [DEPTH, 128, 8])
    g_ffnT_d = din("g_ffnT", [DEPTH, 128, 8])
    g_final_d = din("g_final", [1, D])
    w_inA_d = din("w_inA", [DEPTH, D, 896])
    w_in_d = din("w_in", [DEPTH, D, 2576])
    w_outP_d = din("w_outP", [DEPTH, D, D])
    sinkE_d = din("sinkE", [DEPTH, 128, 2])
    convw_d = din("convw", [DEPTH, 128, 30])
    convb_d = din("convb", [DEPTH, 128, 6])
    dtb_d = din("dtb", [DEPTH, 16])
    alog_d = din("alog", [DEPTH, 16])
    ssdd_d = din("ssdd", [DEPTH, 8])
    normgT_d = din("normgT", [DEPTH, 128, 4])
    naTab_d = din("naTab", [DEPTH, 128, 6144])
    w_rt_d = din("w_rt", [DEPTH, D, 36])
    b_rt_d = din("b_rt", [DEPTH, 36])
    wg_d = din("w_exp_gate", [DEPTH, 32, D, 256])
    wu_d = din("w_exp_up", [DEPTH, 32, D, 256])
    wd_d = din("w_exp_down", [DEPTH, 32, 256, D])
    ident_d = din("ident", [128, 128])
    triU_d = din("triU", [128, 128])
    triL_d = din("triL", [128, 128])
    maskF_d = din("maskF", [128, 128])
    maskB_d = din("maskB", [128, 128])
    maskA_d = din("maskA", [128, 384])
    ropeC_d = din("ropeC", [128, 2048])
    ropeS_d = din("ropeS", [128, 2048])
    out_d = nc.dram_tensor("out", [2048, D], F32, kind="ExternalOutput").ap()
    dbg_d = None
    if cfg.get("dump_ctx"):
        dbg_d = nc.dram_tensor("out_ctx", [256, D], F32, kind="ExternalOutput").ap()

    es = contextlib.ExitStack()
    with es:
        def sb(name, shape, dt):
            return es.enter_context(nc.sbuf_tensor("sb_" + name, list(shape), dt))

        S = Sched(nc)

        X = sb("X", [128, NT, D], F32)
        HT = sb("HT", [128, 8, TOK], BF16)
        REG = sb("REG", [128, REGN], BF16)
        GATE = sb("GATE", [128, 4, D], F32)
        ident_f = sb("ident_f", [128, 128], F32)
        ident_b = sb("ident_b", [128, 128], BF16)
        triU = sb("triU", [128, 128], F32)
        triL = sb("triL", [128, 128], F32)
        maskF = sb("maskF", [128, 128], F32)
        maskB = sb("maskB", [128, 128], F32)
        maskA = sb("maskA", [128, 3, 128], BF16)
        ones_f = sb("ones_f", [128, 128], F32)
        ones_b = sb("ones_b", [128, 128], BF16)
        scT = sb("scT", [128, 16], F32)
        sc_b = sb("sc_b", [128, 8, 2], BF16)
        sc_rep = sb("sc_rep", [128, 2, 8, 128], BF16)
        modT = sb("modT", [128, 48, 2], F32)
        bmT = sb("bmT", [128, 48], F32)
        gT = sb("gT", [128, 16], F32)
        AV = sb("AV", [128, 2, 2, 8], F32)
        SV = sb("SV", [128, 2, 2, 8], F32)
        small = sb("small", [128, 64], F32)

        banks = [es.enter_context(nc.psum_tensor("bank%d" % i, [128, 512], F32)) for i in range(8)]
        RB = [Res("bank%d" % i, psum=True) for i in range(8)]

        RX = [Res("X%d" % t) for t in range(NT)]
        RH = [Res("H%d" % t) for t in range(NT)]
        Rc = {}

        def R(name):
            if name not in Rc:
                Rc[name] = Res(name)
            return Rc[name]

        class Carver:
            def __init__(self, base, nbytes):
                self.base = base
                self.off = 0
                self.nbytes = nbytes

            def take(self, shape, dt):
                esz = 2 if dt == BF16 else 4
                n = int(np.prod(shape[1:]))
                nb = n * esz
                nb_al = (nb + 63) // 64 * 64
                assert self.off + nb_al <= self.nbytes, ("region overflow", self.off, nb_al, self.nbytes)
                a = self.base[:, self.off // 2:(self.off + nb) // 2]
                self.off += nb_al
                if dt != BF16:
                    a = a.bitcast(dt)
                if len(shape) > 2:
                    names = " ".join("d%d" % i for i in range(1, len(shape)))
                    kw = {"d%d" % i: shape[i] for i in range(1, len(shape))}
                    a = a.rearrange("p (%s) -> p %s" % (names, names), **kw)
                return a

        def region():
            return Carver(REG, REGN * 2)

        def ht_region():
            return Carver(HT[:].rearrange("p k t -> p (k t)"), 8 * TOK * 2)

        def mm(out, lhsT, rhs, start, stop, reads, writes, sgc=False):
            if sgc:
                S.op("pe", lambda e: e.matmul(out, lhsT=lhsT, rhs=rhs, start=start, stop=stop, skip_group_check=True), reads, writes)
            else:
                S.op("pe", lambda e: e.matmul(out, lhsT=lhsT, rhs=rhs, start=start, stop=stop), reads, writes)

        def tr(out, in_, ident, reads, writes):
            S.op("pe", lambda e: e.transpose(out=out, in_=in_, identity=ident), reads, writes)

        def act(out, in_, func, reads, writes, bias=None, scale=None, accum_out=None):
            kw = {}
            if bias is not None:
                kw["bias"] = bias
            if scale is not None:
                kw["scale"] = scale
            if accum_out is not None:
                kw["accum_out"] = accum_out
            S.op("act", lambda e: e.activation(out=out, in_=in_, func=func, **kw), reads, writes)

        def tt(eng, out, in0, in1, op, reads, writes):
            S.op(eng, lambda e: e.tensor_tensor(out=out, in0=in0, in1=in1, op=op), reads, writes)

        def ts(eng, out, in0, s1, op0, reads, writes, s2=None, op1=None):
            if op1 is None:
                S.op(eng, lambda e: e.tensor_scalar(out=out, in0=in0, scalar1=s1, scalar2=None, op0=op0), reads, writes)
            else:
                S.op(eng, lambda e: e.tensor_scalar(out=out, in0=in0, scalar1=s1, scalar2=s2, op0=op0, op1=op1), reads, writes)

        def stt(out, in0, scalar, in1, op0, op1, reads, writes):
            S.op("dve", lambda e: e.scalar_tensor_tensor(out=out, in0=in0, scalar=scalar, in1=in1, op0=op0, op1=op1), reads, writes)

        def cp(eng, out, in_, reads, writes):
            if eng == "act_copy":
                S.op("act", lambda e: e.activation(out=out, in_=in_, func=AF.Copy), reads, writes)
            else:
                S.op(eng, lambda e: e.tensor_copy(out=out, in_=in_), reads, writes)

        def memset(eng, ap, val, writes):
            S.op(eng, lambda e: e.memset(ap, val), (), writes)

        dma_ctr = [0]

        def load(q, out, in_, writes, reads=(), sem=None):
            if sem is None:
                sem = "u%d" % (dma_ctr[0] % 40)
                dma_ctr[0] += 1
            S.dma(q, [lambda e: e.dma_start(out=out, in_=in_)], sem, reads, writes)

        def wview(w2d, ncols_lo, ncols_hi):
            return w2d[:, ncols_lo:ncols_hi].rearrange("(k p) n -> p k n", p=128)

        xv = x_d.rearrange("(t p) d -> p t d", p=128)
        cv = ctx_d.rearrange("(t p) d -> p t d", p=128)
        for t in range(NLAT):
            load("sp", X[:, t, :], xv[:, t, :], [RX[t]])
        for t in range(2):
            load("sp", X[:, NLAT + t, :], cv[:, t, :], [RX[NLAT + t]])
        load("sp", ident_f[:], ident_d, [R("ident_f")])
        load("pool", ident_b[:], ident_d, [R("ident_b")])
        load("sp", triU[:], triU_d, [R("triU")])
        load("sp", triL[:], triL_d, [R("triL")])
        load("sp", maskF[:], maskF_d, [R("maskF")])
        load("sp", maskB[:], maskB_d, [R("maskB")])
        load("pool", maskA[:].rearrange("p a b -> p (a b)"), maskA_d, [R("maskA")])
        load("sp", scT[:], scT_d, [R("scT")])
        memset("dve", ones_f[:], 1.0, [R("ones_f")])
        memset("dve", ones_b[:], 1.0, [R("ones_b")])
        act(scT[:], scT[:], AF.Silu, [R("scT")], [R("scT")])
        cp("dve", sc_b[:].rearrange("p k j -> p (k j)"), scT[:], [R("scT")], [R("sc_b")])
        for j in range(2):
            for k in range(8):
                ts("dve", sc_rep[:, j, k, :], ones_f[:], scT[:, 2 * k + j:2 * k + j + 1], ALU.mult,
                   [R("scT"), R("ones_f")], [R("sc_rep")])

        def mod_phase(l):
            S.barrier()
            rg = region()
            wb = [rg.take([128, 8, 512], BF16) for _ in range(3)]
            Rw = [Res("modw%d" % i) for i in range(3)]
            brow = rg.take([128, 4, 512], F32)
            load("sp", bmT[:], b_modT_d[l], [R("bmT")])
            load("sp", gT[:, 0:8], g_mixT_d[l], [R("gT")])
            load("sp", gT[:, 8:16], g_ffnT_d[l], [R("gT")])
            gate_chunks = {4: (0, 0), 5: (0, 1), 10: (1, 0), 11: (1, 1)}
            gi = 0
            for ch in range(12):
                i = ch % 3
                load("pool", wb[i], wview(w_mod_d[l], ch * 512, ch * 512 + 512), [Rw[i]], sem="modw%d" % i)
                if ch in gate_chunks:
                    g, half = gate_chunks[ch]
                    load("sp", brow[:, gi, :], b_mod_d[l:l + 1, ch * 512:ch * 512 + 512].partition_broadcast(128),
                         [R("brow")])
                    for j in range(2):
                        bk = banks[j]
                        for k in range(8):
                            mm(bk[:, :], sc_rep[:, j, k, :], wb[i][:, k, :], k == 0, k == 7,
                               [R("sc_rep"), Rw[i]], [RB[j]])
                        tt("dve", GATE[:, 2 * g + j, half * 512:half * 512 + 512], bk[:, :], brow[:, gi, :], ALU.add,
                           [RB[j], R("brow")], [R("GATE")])
                    gi += 1
                else:
                    for sub in range(4):
                        jn = ch * 4 + sub
                        for k in range(8):
                            mm(banks[2][:, 2 * jn:2 * jn + 2], wb[i][:, k, sub * 128:sub * 128 + 128], sc_b[:, k, :],
                               k == 0, k == 7, [R("sc_b"), Rw[i]], [RB[2]])
            mp = banks[2][:, 0:96].rearrange("p (j c) -> p j c", c=2)
            for j in range(2):
                tt("dve", modT[:, :, j], mp[:, :, j], bmT[:], ALU.add, [RB[2], R("bmT")], [R("modT")])
            for n in range(2):
                base = 0 if n == 0 else 24
                for j in range(2):
                    stt(AV[:, n, j, :], modT[:, base + 8:base + 16, j], 1.0, gT[:, 8 * n:8 * n + 8], ALU.add, ALU.mult,
                        [R("modT"), R("gT")], [R("AV")])
                    cp("dve", SV[:, n, j, :], modT[:, base:base + 8, j], [R("modT")], [R("SV")])

        def norm_phase(n, tiles, router=None, rg=None):
            if rg is None:
                rg = region()
            junk = rg.take([128, D], BF16)
            XN = rg.take([128, 2, D], F32)
            HF = rg.take([128, 2, 8, 128], F32)
            for idx, t in enumerate(tiles):
                j = 0 if t < NLAT else 1
                b = idx % 2
                rxn, rhf = R("XN%d" % b), R("HF%d" % b)
                act(junk[:], X[:, t, :], AF.Square, [RX[t]], [R("junk"), R("ss%d" % t)], accum_out=small[:, t:t + 1])
                act(small[:, 32 + t:33 + t], small[:, t:t + 1], AF.Ln, [R("ss%d" % t)], [R("rs%d" % t)], bias=1e-6, scale=1.0 / D)
                act(small[:, 32 + t:33 + t], small[:, 32 + t:33 + t], AF.Exp, [R("rs%d" % t)], [R("rs%d" % t)], scale=-0.5)
                ts("dve", XN[:, b, :], X[:, t, :], small[:, 32 + t:33 + t], ALU.mult, [RX[t], R("rs%d" % t)], [rxn])
                pb = [banks[2 * b], banks[2 * b + 1]]
                for k in range(8):
                    bk = pb[k // 4]
                    mm(bk[:, (k % 4) * 128:(k % 4) * 128 + 128], XN[:, b, k * 128:k * 128 + 128], ident_f[:], True, True,
                       [rxn, R("ident_f")], [RB[2 * b + k // 4]])
                for k in range(8):
                    bk = pb[k // 4]
                    if k % 4 < 2:
                        act(HF[:, b, k, :], bk[:, (k % 4) * 128:(k % 4) * 128 + 128], AF.Identity,
                            [RB[2 * b + k // 4], R("AV"), R("SV")], [rhf],
                            bias=SV[:, n, j, k:k + 1], scale=AV[:, n, j, k:k + 1])
                    else:
                        ts("dve", HF[:, b, k, :], bk[:, (k % 4) * 128:(k % 4) * 128 + 128], AV[:, n, j, k:k + 1], ALU.mult,
                           [RB[2 * b + k // 4], R("AV"), R("SV")], [rhf], s2=SV[:, n, j, k:k + 1], op1=ALU.add)
                cp("pool", HT[:, :, t * 128:t * 128 + 128], HF[:, b, :, :], [rhf], [RH[t]])
                if router is not None:
                    router(t, HF[:, b, :, :], rhf)

        def x_update(t, ps_lo, ps_hi, rb_lo, rb_hi, g, tmp, rtmp):
            for half, (ps, rb) in enumerate(((ps_lo, rb_lo), (ps_hi, rb_hi))):
                sl = slice(half * 512, half * 512 + 512)
                tt("dve", tmp[:, sl], ps, GATE[:, g, sl], ALU.mult, [rb, R("GATE")], [rtmp])
                tt("dve", X[:, t, sl], X[:, t, sl], tmp[:, sl], ALU.add, [rtmp, RX[t]], [RX[t]])

        def proj_fm(dst, rdst, wt, rw, col0, tiles_blocks, evac):
            for bi, (t0, ntile) in enumerate(tiles_blocks):
                bk = banks[bi % 2]
                n = ntile * 128
                for k in range(8):
                    mm(bk[:, 0:n], wt[:, k, col0:col0 + 128], HT[:, k, t0 * 128:t0 * 128 + n], k == 0, k == 7,
                       [rw] + RH[t0:t0 + ntile], [RB[bi % 2]])
                evac(bk[:, 0:n], RB[bi % 2], t0, n)

        BLOCKS = [(0, 4), (4, 4), (8, 4), (12, 4), (16, 2)]

        def attn_phase(kind, l, need_ctx):
            S.barrier()
            rg = region()
            if kind == "A":
                ncolw = 896
                wt = rg.take([128, 8, ncolw], BF16)
                load("pool", wt, wview(w_inA_d[l], 0, 896), [R("wt")], sem="wt")
                ropeC = rg.take([128, 2048], F32)
                ropeS = rg.take([128, 2048], F32)
                load("sp", ropeC, ropeC_d, [R("ropeC")])
                load("sp", ropeS, ropeS_d, [R("ropeS")])
                nq = 2
                Qs = [rg.take([128, TOK], BF16) for _ in range(2)]
                Ks = [rg.take([128, TOK], BF16)]
                Ks = [Ks[0], Ks[0]]
                vcols = 128
                Vtm = rg.take([128, NT, vcols], BF16)
                t1 = rg.take([128, 512], F32)
                t2 = rg.take([128, 512], F32)
                wo = rg.take([128, 2, D], BF16)
                load("pool", wo, w_outP_d[l][0:256, :].rearrange("(k p) n -> p k n", p=128), [R("wo")], sem="wo")
                esink = rg.take([128, 2], F32)
                load("sp", esink, sinkE_d[l], [R("esink")])
                act(esink, esink, AF.Exp, [R("esink")], [R("esink")])
                scale = 0.125
                nkmax = 5
                for ci, (dst, rn) in enumerate(((Qs[0], "Q0"), (Qs[1], "Q1"), (Ks[0], "K0"))):
                    for bi, (t0, ntile) in enumerate(BLOCKS):
                        n = ntile * 128
                        b0, b1 = banks[2 * (bi % 2)], banks[2 * (bi % 2) + 1]
                        r0, r1 = RB[2 * (bi % 2)], RB[2 * (bi % 2) + 1]
                        for k in range(8):
                            mm(b0[:, 0:n], wt[:, k, ci * 128:ci * 128 + 128], HT[:, k, t0 * 128:t0 * 128 + n], k == 0, k == 7,
                               [R("wt")] + RH[t0:t0 + ntile], [r0])
                        if t0 < NLAT:
                            for k in range(8):
                                mm(b1[:, 0:n], wt[:, k, (ci + 3) * 128:(ci + 3) * 128 + 128], HT[:, k, t0 * 128:t0 * 128 + n],
                                   k == 0, k == 7, [R("wt")] + RH[t0:t0 + ntile], [r1])
                            tt("dve", t1[:, 0:n], b0[:, 0:n], ropeC[:, t0 * 128:t0 * 128 + n], ALU.mult, [r0, R("ropeC")], [R("t1")])
                            tt("dve", t2[:, 0:n], b1[:, 0:n], ropeS[:, t0 * 128:t0 * 128 + n], ALU.mult, [r1, R("ropeS")], [R("t2")])
                            tt("pool", dst[:, t0 * 128:t0 * 128 + n], t1[:, 0:n], t2[:, 0:n], ALU.add, [R("t1"), R("t2")], [R(rn)])
                        else:
                            act(dst[:, t0 * 128:t0 * 128 + n], b0[:, 0:n], AF.Copy, [r0], [R(rn)])
                Rc["K1"] = Rc["K0"]
                vcol0 = 768
            else:
                wt = rg.take([128, 8, 768], BF16)
                load("pool", wt, wview(w_in_d[l], 1808, 2576), [R("wt")], sem="wt")
                tab = rg.take([128, 4, 12, 128], BF16)
                load("pool", tab[:].rearrange("p a b c -> p (a b c)"), naTab_d[l], [R("biasT")], sem="tab")
                Qs = [rg.take([128, TOK], BF16) for _ in range(2)]
                Ks = [rg.take([128, TOK], BF16) for _ in range(2)]
                vcols = 256
                Vtm = rg.take([128, NT, vcols], BF16)
                wo = rg.take([128, 2, D], BF16)
                load("pool", wo, w_outP_d[l][768:1024, :].rearrange("(k p) n -> p k n", p=128), [R("wo")], sem="wo")
                scale = 1.0
                nkmax = 7
                for ci, (dst, rn, sc_) in enumerate(((Qs[0], "Q0", 0.125), (Qs[1], "Q1", 0.125), (Ks[0], "K0", 1.0), (Ks[1], "K1", 1.0))):
                    for bi, (t0, ntile) in enumerate(BLOCKS):
                        n = ntile * 128
                        b0, r0 = banks[bi % 4], RB[bi % 4]
                        for k in range(8):
                            mm(b0[:, 0:n], wt[:, k, ci * 128:ci * 128 + 128], HT[:, k, t0 * 128:t0 * 128 + n], k == 0, k == 7,
                               [R("wt")] + RH[t0:t0 + ntile], [r0])
                        ts("dve", dst[:, t0 * 128:t0 * 128 + n], b0[:, 0:n], sc_, ALU.mult, [r0], [R(rn)])
                vcol0 = 512
            for t in range(NT):
                bk, rb = banks[4 + t % 4], RB[4 + t % 4]
                for k in range(8):
                    mm(bk[:, 0:vcols], HT[:, k, t * 128:t * 128 + 128], wt[:, k, vcol0:vcol0 + vcols], k == 0, k == 7,
                       [R("wt"), RH[t]], [rb])
                cp("dve", Vtm[:, t, :], bk[:, 0:vcols], [rb], [R("V")])

            PT = [rg.take([128, nkmax * 128], BF16) for _ in range(2)]
            RPT = [Res("PT%d" % i) for i in range(2)]
            OT = [rg.take([128, 128], BF16) for _ in range(4)]
            ROT = [Res("OT%d" % i) for i in range(4)]
            RDn = [rg.take([128, 128], F32) for _ in range(2)]
            RRD = [Res("RD%d" % i) for i in range(2)]
            tmp = rg.take([128, D], F32)
            rtmp = Res("updtmp")

            def keys_of(t):
                if t >= NLAT:
                    return [(16, None), (17, None)]
                if kind == "A":
                    ks = [(j, j - t + 1) for j in (t - 1, t, t + 1) if 0 <= j < NLAT]
                    ks = [(j, (b if b != 1 else None)) for (j, b) in ks]
                else:
                    ks = _na_keys(t)
                return ks + [(16, None), (17, None)]

            def bias_ap(pr, half, bid):
                if kind == "A":
                    return maskA[:, bid, :]
                return tab[:, 2 * pr + half, bid, :]

            qtiles = list(range(NLAT)) + ([16, 17] if need_ctx else [])
            work = [(t, pr, half) for t in qtiles for pr in range(2) for half in range(2)]

            def scores(i):
                t, pr, half = work[i]
                keys = keys_of(t)
                ps_ = slice(64 * half, 64 * half + 64)
                for kk, (j, bid) in enumerate(keys):
                    col = kk * 128
                    bi_ = 2 * (i % 2) + col // 512
                    c0 = col % 512
                    bk, rb = banks[bi_], RB[bi_]
                    has_b = bid is not None
                    mm(bk[:, c0:c0 + 128], Ks[pr][ps_, j * 128:j * 128 + 128], Qs[pr][ps_, t * 128:t * 128 + 128],
                       True, not has_b, [R("K%d" % pr), R("Q%d" % pr)], [rb])
                    if has_b:
                        mm(bk[:, c0:c0 + 128], ident_b[:], bias_ap(pr, half, bid), False, True,
                           [R("ident_b"), R("biasT"), R("maskA")], [rb])

            def expo(i):
                t, pr, half = work[i]
                ncol = len(keys_of(t)) * 128
                for q in range((ncol + 511) // 512):
                    n = min(512, ncol - q * 512)
                    bi_ = 2 * (i % 2) + q
                    act(PT[i % 2][:, q * 512:q * 512 + n], banks[bi_][:, 0:n], AF.Exp, [RB[bi_]], [RPT[i % 2]], scale=scale)

            def pv(i):
                t, pr, half = work[i]
                ip = i // 2
                keys = keys_of(t)
                nk = len(keys)
                ob, rob = banks[4 + (ip % 2)], RB[4 + (ip % 2)]
                ps_ = slice(64 * half, 64 * half + 64)
                if kind == "A":
                    vsl = slice(64 * half, 64 * half + 64)
                else:
                    h = 2 * pr + half
                    vsl = slice(64 * h, 64 * h + 64)
                for kk, (j, bid) in enumerate(keys):
                    p_ = PT[i % 2][:, kk * 128:kk * 128 + 128]
                    mm(ob[ps_, 0:128], Vtm[:, j, vsl], p_, kk == 0, kk == nk - 1, [R("V"), RPT[i % 2]], [rob])
                for kk, (j, bid) in enumerate(keys):
                    p_ = PT[i % 2][:, kk * 128:kk * 128 + 128]
                    mm(ob[ps_, 128:256], ones_b[:, 0:64], p_, kk == 0, kk == nk - 1, [R("ones_b"), RPT[i % 2]], [rob])
                if half == 0:
                    return
                rd, rrd = RDn[ip % 2], RRD[ip % 2]
                if kind == "A":
                    ts("dve", rd, ob[:, 128:256], esink[:, pr:pr + 1], ALU.add, [rob, R("esink")], [rrd])
                    S.op("dve", lambda e: e.reciprocal(out=rd, in_=rd), [rrd], [rrd])
                else:
                    S.op("dve", lambda e: e.reciprocal(out=rd, in_=ob[:, 128:256]), [rob], [rrd])
                tt("dve", OT[ip % 4], ob[:, 0:128], rd, ALU.mult, [rob, rrd], [ROT[ip % 4]])
                if pr == 0:
                    return
                pending.append((t, ip))

            pending = []

            def proj_tile(t, ip):
                j = 0 if t < NLAT else 1
                o0, o1 = OT[(ip - 1) % 4], OT[ip % 4]
                ro0, ro1 = ROT[(ip - 1) % 4], ROT[ip % 4]
                for hf in range(2):
                    bk, rb = banks[6 + hf], RB[6 + hf]
                    sl = slice(hf * 512, hf * 512 + 512)
                    mm(bk[:, :], o0, wo[:, 0, sl], True, False, [ro0, R("wo")], [rb])
                    mm(bk[:, :], o1, wo[:, 1, sl], False, True, [ro1, R("wo")], [rb])
                x_update(t, banks[6][:, :], banks[7][:, :], RB[6], RB[7], 0 + j, tmp, rtmp)

            n = len(work)
            scores(0)
            for i in range(n):
                expo(i)
                if i + 1 < n:
                    scores(i + 1)
                if len(pending) and work[i][1:] == (0, 1):
                    proj_tile(*pending.pop(0))
                pv(i)
            while pending:
                proj_tile(*pending.pop(0))

        def bc_mid(ap2d, n):
            return ap2d.unsqueeze(1).to_broadcast([128, n, ap2d.shape[1]])

        def bc_last(ap2d, n):
            return ap2d.unsqueeze(2).to_broadcast([128, ap2d.shape[1], n])

        def ssd_phase(l, need_ctx):
            S.barrier()
            rg = region()
            xs_tm = rg.take([128, NT, 512], BF16)
            B_tm = rg.take([128, NT, 128], BF16)
            BT = rg.take([128, TOK], BF16)
            CT = rg.take([128, TOK], BF16)
            dt = rg.take([128, NT, 16], F32)
            da = rg.take([128, NT, 16], F32)
            acs = rg.take([128, NT, 16], F32)
            scw = rg.take([128, NT, 16], F32)
            etg = rg.take([128, NT, 8], F32)
            convw = rg.take([128, 30], F32)
            convb = rg.take([128, 6], F32)
            dtb_b = rg.take([128, 16], F32)
            a_b = rg.take([128, 16], F32)
            Db = rg.take([128, 8], F32)
            normg = rg.take([128, 4], F32)
            mark = rg.off
            load("sp", convw, convw_d[l], [R("convw")])
            load("sp", convb, convb_d[l], [R("convb")])
            load("sp", dtb_b, dtb_d[l:l + 1, :].partition_broadcast(128), [R("dtb_b")])
            load("sp", a_b, alog_d[l:l + 1, :].partition_broadcast(128), [R("a_b")])
            load("sp", Db, ssdd_d[l:l + 1, :].partition_broadcast(128), [R("Db")])
            load("sp", normg, normgT_d[l], [R("normg")])
            act(a_b, a_b, AF.Exp, [R("a_b")], [R("a_b")])
            ts("dve", a_b, a_b, -1.0, ALU.mult, [R("a_b")], [R("a_b")])

            wx = [rg.take([128, 8, 128], BF16) for _ in range(2)]
            Rwx = [Res("wx%d" % i) for i in range(2)]
            pre = rg.take([128, TOK], F32)
            acc = rg.take([128, TOK], F32)
            post = rg.take([128, TOK], BF16)
            SEGS = [(0, 2048), (2048, 2304)]
            for ci in range(6):
                i = ci % 2
                load("pool", wx[i], wview(w_in_d[l], 1024 + ci * 128, 1024 + ci * 128 + 128), [Rwx[i]], sem="wx%d" % i)
                for bi, (t0, ntile) in enumerate(BLOCKS):
                    n = ntile * 128
                    bk, rb = banks[bi % 4], RB[bi % 4]
                    for k in range(8):
                        mm(bk[:, 0:n], wx[i][:, k, :], HT[:, k, t0 * 128:t0 * 128 + n], k == 0, k == 7,
                           [Rwx[i]] + RH[t0:t0 + ntile], [rb])
                    act(pre[:, t0 * 128:t0 * 128 + n], bk[:, 0:n], AF.Copy, [rb], [R("pre")])
                for (a, b) in SEGS:
                    ts("dve", acc[:, a:b], pre[:, a:b], convw[:, ci * 5 + 2:ci * 5 + 3], ALU.mult, [R("pre"), R("convw"), R("convb")], [R("acc")],
                       s2=convb[:, ci:ci + 1], op1=ALU.add)
                    for kk in (0, 1, 3, 4):
                        s = kk - 2
                        lo = max(a, a - s)
                        hi = min(b, b - s)
                        stt(acc[:, lo:hi], pre[:, lo + s:hi + s], convw[:, ci * 5 + kk:ci * 5 + kk + 1], acc[:, lo:hi], ALU.mult, ALU.add,
                            [R("pre"), R("convw"), R("acc")], [R("acc")])
                if ci < 4 or ci == 4:
                    dst_fm = post if ci < 4 else BT
                    rdst = R("post") if ci < 4 else R("BT")
                else:
                    dst_fm, rdst = CT, R("CT")
                act(dst_fm, acc, AF.Silu, [R("acc")], [rdst])
                if ci <= 4:
                    for g0 in range(0, NT, 8):
                        ng = min(8, NT - g0)
                        bi_ = 4 + (g0 // 8) % 2 + 2 * (ci % 2)
                        bk, rb = banks[bi_], RB[bi_]
                        bv = bk.bitcast(BF16)
                        for q in range(ng):
                            t = g0 + q
                            tr(bv[:, q * 128:q * 128 + 128], dst_fm[:, t * 128:t * 128 + 128], ident_b[:], [rdst, R("ident_b")], [rb])
                        src = bv[:, 0:ng * 128].rearrange("p (q c) -> p q c", c=128)
                        if ci < 4:
                            cp("act_copy", xs_tm[:, g0:g0 + ng, ci * 128:ci * 128 + 128], src, [rb], [R("xs_tm")])
                        else:
                            cp("act_copy", B_tm[:, g0:g0 + ng, :], src, [rb], [R("B_tm")])

            if cfg.get("ssd_stop", 9) <= 1:
                return
            S.barrier()
            rg.off = mark
            zs = rg.take([128, NT, 512], BF16)
            mark2 = rg.off
            wz = rg.take([128, 8, 512], BF16)
            wdt = rg.take([128, 8, 16], BF16)
            tot = rg.take([128, NT, 16], F32)
            etot = rg.take([128, NT, 16], F32)
            load("pool", wz, wview(w_in_d[l], 512, 1024), [R("wz")], sem="wz")
            load("pool", wdt, wview(w_in_d[l], 1792, 1808), [R("wdt")], sem="wdt")
            for t in range(NT):
                for k in range(8):
                    mm(banks[0][:, t * 16:t * 16 + 16], HT[:, k, t * 128:t * 128 + 128], wdt[:, k, :], k == 0, k == 7,
                       [R("wdt"), RH[t]], [RB[0]])
            p0 = banks[0][:, 0:NT * 16].rearrange("p (t c) -> p t c", c=16)
            tt("dve", dt, p0, bc_mid(dtb_b, NT), ALU.add, [RB[0], R("dtb_b")], [R("dt")])
            act(dt, dt, AF.Exp, [R("dt")], [R("dt")])
            act(dt, dt, AF.Ln, [R("dt")], [R("dt")], bias=1.0, scale=1.0)
            tt("dve", da, dt, bc_mid(a_b, NT), ALU.mult, [R("dt"), R("a_b")], [R("da")])
            for t in range(NT):
                mm(banks[1][:, t * 16:t * 16 + 8], triU[:], da[:, t, 0:8], True, True, [R("triU"), R("da")], [RB[1]])
                mm(banks[1][:, t * 16 + 8:t * 16 + 16], triL[:], da[:, t, 8:16], True, True, [R("triL"), R("da")], [RB[1]])
                mm(banks[2][:, t * 16:t * 16 + 16], ones_f[:], da[:, t, :], True, True, [R("ones_f"), R("da")], [RB[2]])
            p1 = banks[1][:, 0:NT * 16].rearrange("p (t c) -> p t c", c=16)
            p2 = banks[2][:, 0:NT * 16].rearrange("p (t c) -> p t c", c=16)
            cp("dve", acs, p1, [RB[1]], [R("acs")])
            cp("dve", tot, p2, [RB[2]], [R("tot")])
            tt("dve", scw, tot, acs, ALU.subtract, [R("tot"), R("acs")], [R("scw")])
            act(scw, scw, AF.Exp, [R("scw")], [R("scw")])
            tt("dve", scw, scw, dt, ALU.mult, [R("scw"), R("dt")], [R("scw")])
            act(etot, tot, AF.Exp, [R("tot")], [R("etot")])
            for d_ in range(2):
                for g in range(2):
                    cp("dve", etg[64 * g:64 * g + 64, :, 4 * d_:4 * d_ + 4], etot[64 * g:64 * g + 64, :, 8 * d_ + 4 * g:8 * d_ + 4 * g + 4],
                       [R("etot")], [R("etg")])
            for t in range(NT):
                if t >= NLAT and not need_ctx:
                    continue
                bk, rb = banks[4 + t % 4], RB[4 + t % 4]
                for k in range(8):
                    mm(bk[:, :], HT[:, k, t * 128:t * 128 + 128], wz[:, k, :], k == 0, k == 7, [R("wz"), RH[t]], [rb])
                act(zs[:, t, :], bk[:, :], AF.Silu, [rb], [R("zs")])

            if cfg.get("ssd_stop", 9) <= 2:
                return
            S.barrier()
            rg.off = mark2
            prevB = rg.take([128, NT, 256], BF16)
            woS = rg.take([128, 4, D], BF16)
            load("pool", woS, w_outP_d[l][256:768, :].rearrange("(k p) n -> p k n", p=128), [R("woS")], sem="woS")
            hr = ht_region()
            xw = [hr.take([128, 512], BF16) for _ in range(2)]
            Rxw = [Res("xw%d" % i) for i in range(2)]
            xdt = [hr.take([128, 512], BF16) for _ in range(2)]
            Rxdt = [Res("xdt%d" % i) for i in range(2)]
            rhsU = [hr.take([128, 8, 128], F32) for _ in range(2)]
            RrhsU = [Res("rhsU%d" % i) for i in range(2)]
            E = [hr.take([128, 8, 128], BF16) for _ in range(2)]
            RE = [Res("E%d" % i) for i in range(2)]
            Eb = [hr.take([128, 8, 128], BF16) for _ in range(2)]
            REb = [Res("Eb%d" % i) for i in range(2)]
            Gm = [hr.take([128, 2, 128], BF16) for _ in range(2)]
            RGm = [Res("Gm%d" % i) for i in range(2)]
            nacs = hr.take([128, NT, 16], F32)
            Sf = hr.take([128, 256], F32)
            Sf_bf = hr.take([128, 256], BF16)
            Sb = hr.take([128, 256], F32)
            tmpD = hr.take([128, 512], F32)
            ytot = hr.take([128, 512], F32)
            yn = hr.take([128, 512], BF16)
            oT = hr.take([128, 4, 128], BF16)
            upd = hr.take([128, 512], F32)
            sjunk = hr.take([128, 512], BF16)
            ssq = hr.take([128, 64], F32)
            rupd = Res("updS")
            for t in range(NT):
                RH[t] = Res("H%d" % t)

            memset("dve", Sb, 0.0, [R("Sb")])
            memset("dve", Sf, 0.0, [R("Sf")])
            memset("dve", Sf_bf, 0.0, [R("Sf_bf")])
            ts("dve", nacs, acs, -1.0, ALU.mult, [R("acs")], [R("nacs")])

            def xs3(c):
                return xs_tm[:, c, :].rearrange("p (h q) -> p h q", q=64)

            STB0 = 5

            def states(c, d_, i, Sacc, rS, alt=False):
                STB = 7 if (alt and i % 2 == 1) else STB0
                tt("pool", xw[i][:].rearrange("p (h q) -> p h q", q=64), xs3(c), bc_last(scw[:, c, 8 * d_:8 * d_ + 8], 64), ALU.mult,
                   [R("xs_tm"), R("scw")], [Rxw[i]])
                for g in range(2):
                    mm(banks[STB][64 * g:64 * g + 64, 256:512], B_tm[:, c, 64 * g:64 * g + 64], xw[i][:, 256 * g:256 * g + 256], True, True,
                       [R("B_tm"), Rxw[i]], [RB[STB]])
                for hl in range(4):
                    sl = slice(64 * hl, 64 * hl + 64)
                    stt(Sacc[:, sl], Sacc[:, sl], etg[:, c, 4 * d_ + hl:4 * d_ + hl + 1], banks[STB][:, 256 + 64 * hl:256 + 64 * hl + 64], ALU.mult, ALU.add,
                        [rS, R("etg"), RB[STB]], [rS])

            for n_, c in enumerate([17, 16] + list(range(15, -1, -1))):
                cp("act_copy", prevB[:, c, :], Sb, [R("Sb")], [R("prevB")])
                if c != 0:
                    states(c, 1, n_ % 2, Sb, R("Sb"), alt=True)

            order2 = [16, 17] + list(range(NLAT))

            def front(n_, c):
                emit_out = need_ctx or c < NLAT
                csl = slice(c * 128, c * 128 + 128)
                if emit_out:
                    gbanks = ((banks[2][:, 0:128], RB[2]), (banks[3][:, 0:128], RB[3]))
                    for g in range(2):
                        ps_ = slice(64 * g, 64 * g + 64)
                        mm(gbanks[g][0], BT[ps_, csl], CT[ps_, csl], True, True, [R("BT"), R("CT")], [gbanks[g][1]])
                    for d_ in range(2):
                        tri = triU if d_ == 0 else triL
                        for g in range(2):
                            tt("dve", Gm[d_][:, g, :], gbanks[g][0], tri[:], ALU.mult, [gbanks[g][1], R("triU"), R("triL")], [RGm[d_]])
                    for d_ in range(2):
                        tri = triU if d_ == 0 else triL
                        tt("pool", rhsU[d_], bc_mid(tri[:], 8), bc_last(da[:, c, 8 * d_:8 * d_ + 8], 128), ALU.mult,
                           [R("triU"), R("triL"), R("da")], [RrhsU[d_]])
                    for d_ in range(2):
                        tt("dve", xdt[d_][:].rearrange("p (h q) -> p h q", q=64), xs3(c), bc_last(dt[:, c, 8 * d_:8 * d_ + 8], 64), ALU.mult,
                           [R("xs_tm"), R("dt")], [Rxdt[d_]])
                    for d_ in range(2):
                        for h in range(8):
                            bi_ = h // 4
                            mm(banks[bi_][:, (h % 4) * 128:(h % 4) * 128 + 128], ones_f[:], rhsU[d_][:, h, :], True, True,
                               [R("ones_f"), RrhsU[d_]], [RB[bi_]])
                        for q in range(2):
                            bi_ = q
                            for h in range(4 * q, 4 * q + 4):
                                act(E[d_][:, h, :], banks[bi_][:, (h % 4) * 128:(h % 4) * 128 + 128], AF.Exp, [RB[bi_], R("nacs")], [RE[d_]],
                                    bias=nacs[:, c, 8 * d_ + h:8 * d_ + h + 1], scale=1.0)
                            act(Eb[d_][:, 4 * q:4 * q + 4, :].rearrange("p h i -> p (h i)"), banks[bi_][:, :], AF.Exp, [RB[bi_]], [REb[d_]])
                    for d_ in range(2):
                        ts("dve", E[d_], E[d_], 1.0, ALU.min, [RE[d_]], [RE[d_]])
                        for g in range(2):
                            tt("dve", E[d_][:, 4 * g:4 * g + 4, :], E[d_][:, 4 * g:4 * g + 4, :], bc_mid(Gm[d_][:, g, :], 4), ALU.mult,
                               [RE[d_], RGm[d_]], [RE[d_]])
                        tt("pool", Eb[d_], Eb[d_], bc_mid(CT[:, csl], 8), ALU.mult, [REb[d_], R("CT")], [REb[d_]])

            def front_y(n_, c):
                emit_out = need_ctx or c < NLAT
                if emit_out:
                    for d_ in range(2):
                        for h in range(8):
                            g, hl = h // 4, h % 4
                            ysl = slice(64 * h, 64 * h + 64)
                            mm(banks[4][:, ysl], E[d_][:, h, :], xdt[d_][:, ysl], d_ == 0 and h == 0, False, [RE[d_], Rxdt[d_]], [RB[4]], sgc=True)
                            st_ = Sf_bf if d_ == 0 else prevB[:, c, :]
                            rst = R("Sf_bf") if d_ == 0 else R("prevB")
                            mm(banks[4][:, ysl], Eb[d_][64 * g:64 * g + 64, h, :], st_[64 * g:64 * g + 64, 64 * hl:64 * hl + 64], False, d_ == 1,
                               [REb[d_], rst], [RB[4]], sgc=True)

            def fstates(n_, c):
                if c != NLAT - 1:
                    states(c, 0, n_ % 2, Sf, R("Sf"))
                    cp("act_copy", Sf_bf, Sf, [R("Sf")], [R("Sf_bf")])

            def back1(n_, c):
                emit_out = need_ctx or c < NLAT
                if not emit_out:
                    return
                tt("dve", tmpD[:].rearrange("p (h q) -> p h q", q=64), xs3(c), bc_last(Db, 64), ALU.mult, [R("xs_tm"), R("Db")], [R("tmpD")])
                tt("dve", ytot, banks[4][:, :], tmpD, ALU.add, [RB[4], R("tmpD")], [R("ytot")])

            def back(n_, c):
                emit_out = need_ctx or c < NLAT
                if not emit_out:
                    return
                j = 0 if c < NLAT else 1
                tt("dve", ytot, ytot, zs[:, c, :], ALU.mult, [R("ytot"), R("zs")], [R("ytot")])
                act(sjunk, ytot, AF.Square, [R("ytot")], [R("sjunk"), R("ssq")], accum_out=ssq[:, c:c + 1])
                act(ssq[:, 32 + c:33 + c], ssq[:, c:c + 1], AF.Ln, [R("ssq")], [R("ssq")], bias=1e-6, scale=1.0 / 512)
                act(ssq[:, 32 + c:33 + c], ssq[:, 32 + c:33 + c], AF.Exp, [R("ssq")], [R("ssq")], scale=-0.5)
                ts("dve", yn, ytot, ssq[:, 32 + c:33 + c], ALU.mult, [R("ytot"), R("ssq")], [R("yn")])
                b5 = banks[5].bitcast(BF16)
                for kc in range(4):
                    tr(b5[:, kc * 128:kc * 128 + 128], yn[:, kc * 128:kc * 128 + 128], ident_b[:], [R("yn"), R("ident_b")], [RB[5]])
                for kc in range(4):
                    ts("dve", oT[:, kc, :], b5[:, kc * 128:kc * 128 + 128], normg[:, kc:kc + 1], ALU.mult, [RB[5], R("normg")], [R("oT")])
                for hf in range(2):
                    sl = slice(hf * 512, hf * 512 + 512)
                    for kc in range(4):
                        mm(banks[6 + hf][:, :], oT[:, kc, :], woS[:, kc, sl], kc == 0, kc == 3, [R("oT"), R("woS")], [RB[6 + hf]])
                for hf in range(2):
                    sl = slice(hf * 512, hf * 512 + 512)
                    tt("dve", upd, banks[6 + hf][:, :], GATE[:, j, sl], ALU.mult, [RB[6 + hf], R("GATE")], [rupd])
                    tt("dve", X[:, c, sl], X[:, c, sl], upd, ALU.add, [rupd, RX[c]], [RX[c]])

            front(0, order2[0])
            front_y(0, order2[0])
            fstates(0, order2[0])
            for n_, c in enumerate(order2):
                if n_ + 1 < len(order2):
                    front(n_ + 1, order2[n_ + 1])
                back1(n_, c)
                if n_ + 1 < len(order2):
                    front_y(n_ + 1, order2[n_ + 1])
                back(n_, c)
                if n_ + 1 < len(order2):
                    fstates(n_ + 1, order2[n_ + 1])


        def tree(op, dst, src, width, n1, tmpbuf):
            cur = src
            w = width
            while w > 1:
                h = w // 2
                out = dst.unsqueeze(2) if h == 1 else tmpbuf[:, :, 0:h]
                tt("dve", out, cur[:, :, 0:h], cur[:, :, h:w], op, [R("rt")], [R("rt")])
                cur = out
                w = h

        def moe_phase(l, need_ctx):
            tiles = list(range(NT)) if need_ctx else list(range(NLAT))
            ntl = len(tiles)
            S.barrier()
            rg = region()
            comb = rg.take([128, NT, 32], F32)
            lg = rg.take([128, NT, 36], F32)
            mark = rg.off
            w_rt = rg.take([128, 8, 36], F32)
            brt = rg.take([128, 36], F32)
            load("sp", w_rt, w_rt_d[l].rearrange("(k p) n -> p k n", p=128), [R("w_rt")])
            load("sp", brt, b_rt_d[l:l + 1, :].partition_broadcast(128), [R("brt")])

            def router(t, hf, rhf):
                bi_ = 4 + t // 9
                c0 = (t % 9) * 36
                for k in range(8):
                    mm(banks[bi_][:, c0:c0 + 36], hf[:, k, :], w_rt[:, k, :], k == 0, k == 7, [rhf, R("w_rt")], [RB[bi_]])

            norm_phase(1, tiles, rg=rg, router=router)
            for q in range(2):
                t0, t1 = 9 * q, min(9 * q + 9, ntl)
                if t1 <= t0:
                    continue
                n_ = t1 - t0
                src = banks[4 + q][:, 0:n_ * 36].rearrange("p (t c) -> p t c", c=36)
                tt("dve", lg[:, t0:t1, :], src, bc_mid(brt, n_), ALU.add, [RB[4 + q], R("brt")], [R("rt")])
            S.barrier()
            rg.off = mark
            gl = lg[:, 0:ntl, 0:4]
            el = lg[:, 0:ntl, 4:36]
            t4 = rg.take([128, NT, 4], F32)
            t32 = rg.take([128, NT, 32], F32)
            elm = rg.take([128, NT, 32], F32)
            m1b = rg.take([128, NT, 32], F32)
            m2b = rg.take([128, NT, 32], F32)
            gmax = rg.take([128, NT], F32)
            gw = rg.take([128, NT], F32)
            m1 = rg.take([128, NT], F32)
            m2 = rg.take([128, NT], F32)
            w1 = rg.take([128, NT], F32)
            w2 = rg.take([128, NT], F32)
            RT = [R("rt")]
            n = ntl
            tree(ALU.max, gmax[:, 0:n], gl, 4, n, t4[:, 0:n, :])
            tt("dve", t4[:, 0:n, :], gl, bc_last(gmax[:, 0:n], 4), ALU.subtract, RT, RT)
            act(t4[:, 0:n, :], t4[:, 0:n, :], AF.Exp, RT, RT)
            tree(ALU.add, gw[:, 0:n], t4[:, 0:n, :], 4, n, t32[:, 0:n, 0:4])
            S.op("dve", lambda e: e.reciprocal(out=gw[:, 0:n], in_=gw[:, 0:n]), RT, RT)
            tt("dve", t4[:, 0:n, :], gl, bc_last(gmax[:, 0:n], 4), ALU.is_equal, RT, RT)
            ts("dve", t4[:, 0:n, :], t4[:, 0:n, :], -1.0, ALU.add, RT, RT, s2=-NEG, op1=ALU.mult)
            for g in range(4):
                tt("dve", elm[:, 0:n, 8 * g:8 * g + 8], el[:, :, 8 * g:8 * g + 8], t4[:, 0:n, g:g + 1].to_broadcast([128, n, 8]), ALU.add, RT, RT)
            tree(ALU.max, m1[:, 0:n], elm[:, 0:n, :], 32, n, t32[:, 0:n, :])
            tt("dve", m1b[:, 0:n, :], elm[:, 0:n, :], bc_last(m1[:, 0:n], 32), ALU.is_equal, RT, RT)
            stt(elm[:, 0:n, :], m1b[:, 0:n, :], 2 * NEG, elm[:, 0:n, :], ALU.mult, ALU.add, RT, RT)
            tree(ALU.max, m2[:, 0:n], elm[:, 0:n, :], 32, n, t32[:, 0:n, :])
            tt("dve", m2b[:, 0:n, :], elm[:, 0:n, :], bc_last(m2[:, 0:n], 32), ALU.is_equal, RT, RT)
            tt("dve", w2[:, 0:n], m2[:, 0:n], m1[:, 0:n], ALU.subtract, RT, RT)
            act(w2[:, 0:n], w2[:, 0:n], AF.Exp, RT, RT)
            ts("dve", w1[:, 0:n], w2[:, 0:n], 1.0, ALU.add, RT, RT)
            S.op("dve", lambda e: e.reciprocal(out=w1[:, 0:n], in_=w1[:, 0:n]), RT, RT)
            tt("dve", w1[:, 0:n], w1[:, 0:n], gw[:, 0:n], ALU.mult, RT, RT)
            tt("dve", w2[:, 0:n], w2[:, 0:n], w1[:, 0:n], ALU.mult, RT, RT)
            tt("dve", m1b[:, 0:n, :], m1b[:, 0:n, :], bc_last(w1[:, 0:n], 32), ALU.mult, RT, RT)
            tt("dve", m2b[:, 0:n, :], m2b[:, 0:n, :], bc_last(w2[:, 0:n], 32), ALU.mult, RT, RT)
            tt("dve", comb[:, 0:n, :], m1b[:, 0:n, :], m2b[:, 0:n, :], ALU.add, RT, [R("comb")])
            S.barrier()
            rg.off = mark
            NS = 3
            WGU = [rg.take([128, 8, 512], BF16) for _ in range(NS)]
            WD = [rg.take([128, 2, D], BF16) for _ in range(NS)]
            WDx = [rg.take([128, 2, D], BF16) for _ in range(NS)]
            WDc = [rg.take([128, 2, D], BF16) for _ in range(NS)]
            Rgu = [Res("wgu%d" % i) for i in range(NS)]
            Rwd = [Res("wd%d" % i) for i in range(NS)]
            Rwdx = [Res("wdx%d" % i) for i in range(NS)]
            s_sb = [rg.take([128, 256], F32) for _ in range(2)]
            Rs = [Res("s%d" % i) for i in range(2)]
            hid = [rg.take([128, 256], BF16) for _ in range(2)]
            Rhid = [Res("hid%d" % i) for i in range(2)]
            hidT = [rg.take([128, 2, 128], BF16) for _ in range(2)]
            RhT = [Res("hidT%d" % i) for i in range(2)]

            def load_expert(e):
                sl = e % NS
                S.dma("pool", [lambda en: en.dma_start(out=WGU[sl][:, :, 0:256], in_=wg_d[l, e].rearrange("(k p) n -> p k n", p=128)),
                               lambda en: en.dma_start(out=WGU[sl][:, :, 256:512], in_=wu_d[l, e].rearrange("(k p) n -> p k n", p=128))],
                      "wgu%d" % sl, [], [Rgu[sl]])
                S.dma("pool", [lambda en: en.dma_start(out=WD[sl], in_=wd_d[l, e].rearrange("(k p) n -> p k n", p=128))],
                      "wd%d" % sl, [], [Rwd[sl]])
                for fc in range(2):
                    tt("pool", WDx[sl][:, fc, :], WD[sl][:, fc, :], GATE[:, 2, :], ALU.mult, [Rwd[sl], R("GATE")], [Rwdx[sl]])
                    if need_ctx:
                        tt("pool", WDc[sl][:, fc, :], WD[sl][:, fc, :], GATE[:, 3, :], ALU.mult, [Rwd[sl], R("GATE")], [Rwdx[sl]])

            items = [(e, t) for e in range(32) for t in tiles]
            nit = len(items)
            b2 = banks[2].bitcast(BF16)

            def stageG(i):
                e, t = items[i]
                sl = e % NS
                bk, rb = banks[i % 2], RB[i % 2]
                for k in range(8):
                    mm(bk[:, :], HT[:, k, t * 128:t * 128 + 128], WGU[sl][:, k, :], k == 0, k == 7, [RH[t], Rgu[sl]], [rb])
                act(s_sb[i % 2], bk[:, 0:256], AF.Silu, [rb], [Rs[i % 2]])
                stt(hid[i % 2], bk[:, 256:512], comb[:, t, e:e + 1], s_sb[i % 2], ALU.mult, ALU.mult, [rb, R("comb"), Rs[i % 2]], [Rhid[i % 2]])

            def stageT(i):
                for fc in range(2):
                    tr(b2[:, (i % 2) * 256 + fc * 128:(i % 2) * 256 + fc * 128 + 128], hid[i % 2][:, fc * 128:fc * 128 + 128], ident_b[:],
                       [Rhid[i % 2], R("ident_b")], [RB[2]])
                cp("act_copy", hidT[i % 2][:].rearrange("p a b -> p (a b)"), b2[:, (i % 2) * 256:(i % 2) * 256 + 256], [RB[2]], [RhT[i % 2]])

            def stageD(i):
                e, t = items[i]
                sl = e % NS
                wdd = WDx[sl] if t < NLAT else WDc[sl]
                for fc in range(2):
                    for hf in range(2):
                        bi_ = 4 + 2 * (i % 2) + hf
                        mm(banks[bi_][:, :], hidT[i % 2][:, fc, :], wdd[:, fc, hf * 512:hf * 512 + 512], fc == 0, fc == 1,
                           [RhT[i % 2], Rwdx[sl]], [RB[bi_]])
                for hf in range(2):
                    bi_ = 4 + 2 * (i % 2) + hf
                    sl_ = slice(hf * 512, hf * 512 + 512)
                    tt("dve", X[:, t, sl_], X[:, t, sl_], banks[bi_][:, :], ALU.add, [RX[t], RB[bi_]], [RX[t]])

            for e in range(NS):
                load_expert(e)
            for i in range(nit + 2):
                if i < nit:
                    stageG(i)
                if 0 <= i - 1 < nit:
                    stageT(i - 1)
                if 0 <= i - 2 < nit:
                    stageD(i - 2)
                    e, t = items[i - 2]
                    if t == tiles[-1] and e + NS < 32:
                        load_expert(e + NS)


        def final_phase():
            S.barrier()
            rg = region()
            gfb = rg.take([128, D], F32)
            junk = rg.take([128, D], BF16)
            load("sp", gfb, g_final_d.partition_broadcast(128), [R("gfb")])
            ot = [rg.take([128, D], F32) for _ in range(2)]
            rot = [Res("fo%d" % i) for i in range(2)]
            ov = out_d.rearrange("(t p) d -> p t d", p=128)
            for t in range(NLAT):
                b = t % 2
                if final_norm:
                    act(junk[:], X[:, t, :], AF.Square, [RX[t]], [R("junk"), R("ss%d" % t)], accum_out=small[:, t:t + 1])
                    act(small[:, 32 + t:33 + t], small[:, t:t + 1], AF.Ln, [R("ss%d" % t)], [R("rs%d" % t)], bias=1e-6, scale=1.0 / D)
                    act(small[:, 32 + t:33 + t], small[:, 32 + t:33 + t], AF.Exp, [R("rs%d" % t)], [R("rs%d" % t)], scale=-0.5)
                    stt(ot[b], X[:, t, :], small[:, 32 + t:33 + t], gfb, ALU.mult, ALU.mult, [RX[t], R("rs%d" % t), R("gfb")], [rot[b]])
                else:
                    cp("dve", ot[b], X[:, t, :], [RX[t]], [rot[b]])
                S.dma("sp", [lambda e, b=b, t=t: e.dma_start(out=ov[:, t, :], in_=ot[b])], "outst", [rot[b]], [])
            if dbg_d is not None:
                dv = dbg_d.rearrange("(t p) d -> p t d", p=128)
                for t in range(2):
                    S.dma("sp", [lambda e, t=t: e.dma_start(out=dv[:, t, :], in_=X[:, NLAT + t, :])], "outst", [RX[NLAT + t]], [])
            S.streams["sp"].append(("wait", ("dma", "outst"), S.dmacnt[("dma", "outst")]))

        for l in range(nlayers):
            need_ctx = l < DEPTH - 1
            mod_phase(l)
            S.barrier()
            norm_phase(0, list(range(NT)))
            if "A" in phases:
                attn_phase("A", l, need_ctx)
            if "N" in phases:
                attn_phase("N", l, need_ctx)
            if "S" in phases:
                ssd_phase(l, need_ctx)
            if "M" in phases:
                moe_phase(l, need_ctx)
        final_phase()
        S.emit()
    return nc


def _prep_shared(inp):
    f = np.float32
    sh = {}
    sh["w_mod"] = np.ascontiguousarray(inp["w_mod"], f)
    sh["b_mod"] = np.ascontiguousarray(inp["b_mod"], f)
    sh["b_modT"] = np.ascontiguousarray(inp["b_mod"].reshape(DEPTH, 48, 128).transpose(0, 2, 1), f)
    sh["g_mixT"] = np.ascontiguousarray(inp["g_mix"].reshape(DEPTH, 8, 128).transpose(0, 2, 1), f)
    sh["g_ffnT"] = np.ascontiguousarray(inp["g_ffn"].reshape(DEPTH, 8, 128).transpose(0, 2, 1), f)
    sh["g_final"] = np.ascontiguousarray(inp["g_final"].reshape(1, D), f)
    w_in = np.asarray(inp["w_in"], f)
    sw = _swap_idx()
    qcol = lambda h: np.arange(64 * h, 64 * h + 64)
    kcol = lambda g: 256 + np.arange(64 * g, 64 * g + 64)
    Q02 = np.concatenate([qcol(0), qcol(2)])
    Q13 = np.concatenate([qcol(1), qcol(3)])
    K01 = np.concatenate([kcol(0), kcol(1)])
    Q02s = np.concatenate([qcol(0)[sw], qcol(2)[sw]])
    Q13s = np.concatenate([qcol(1)[sw], qcol(3)[sw]])
    K01s = np.concatenate([kcol(0)[sw], kcol(1)[sw]])
    Vc = 384 + np.arange(128)
    colsA = np.concatenate([Q02, Q13, K01, Q02s, Q13s, K01s, Vc])
    sh["w_inA"] = np.ascontiguousarray(w_in[:, :, colsA])
    sh["w_in"] = np.ascontiguousarray(w_in)
    rows = np.concatenate([np.arange(0, 64), np.arange(128, 192), np.arange(64, 128), np.arange(192, 256), np.arange(256, 1024)])
    sh["w_outP"] = np.ascontiguousarray(np.asarray(inp["w_out"], f)[:, rows, :])
    sk = np.asarray(inp["attn_sink"], f)
    sinkE = np.zeros((DEPTH, 128, 2), f)
    sinkE[:, 0:64, 0] = sk[:, 0:1]; sinkE[:, 64:128, 0] = sk[:, 2:3]
    sinkE[:, 0:64, 1] = sk[:, 1:2]; sinkE[:, 64:128, 1] = sk[:, 3:4]
    sh["sinkE"] = sinkE
    cw = np.asarray(inp["ssd_conv_w"], f)
    sh["convw"] = np.ascontiguousarray(cw.reshape(DEPTH, 5, 6, 128).transpose(0, 3, 2, 1).reshape(DEPTH, 128, 30))
    sh["convb"] = np.ascontiguousarray(np.asarray(inp["ssd_conv_b"], f).reshape(DEPTH, 6, 128).transpose(0, 2, 1))
    sh["dtb"] = np.ascontiguousarray(np.asarray(inp["ssd_dt_bias"], f).reshape(DEPTH, 16))
    sh["alog"] = np.ascontiguousarray(np.asarray(inp["ssd_a_log"], f).reshape(DEPTH, 16))
    sh["ssdd"] = np.ascontiguousarray(np.asarray(inp["ssd_d"], f))
    sh["normgT"] = np.ascontiguousarray(np.asarray(inp["ssd_norm_g"], f).reshape(DEPTH, 4, 128).transpose(0, 2, 1))
    rpb = np.asarray(inp["na_rpb"], f)
    sh["naTab"] = np.stack([_na_table(rpb[l]).reshape(128, 6144) for l in range(DEPTH)], 0)
    sh["w_rt"] = np.ascontiguousarray(np.concatenate([np.asarray(inp["w_router_group"], f), np.asarray(inp["w_router_expert"], f)], -1))
    sh["b_rt"] = np.ascontiguousarray(np.concatenate([np.asarray(inp["b_router_group"], f), np.asarray(inp["b_router_expert"], f)], -1))
    sh["w_exp_gate"] = np.ascontiguousarray(np.asarray(inp["w_exp_gate"], f).reshape(DEPTH, 32, D, 256))
    sh["w_exp_up"] = np.ascontiguousarray(np.asarray(inp["w_exp_up"], f).reshape(DEPTH, 32, D, 256))
    sh["w_exp_down"] = np.ascontiguousarray(np.asarray(inp["w_exp_down"], f).reshape(DEPTH, 32, 256, D))
    ct = _const_tables()
    ct["maskA"] = ct["maskA"].reshape(128, 384)
    sh.update(ct)
    return sh


def _run(inp, cfg, cores=None):
    sh = _prep_shared(inp)
    x = np.asarray(inp["x"], np.float32)
    c = np.asarray(inp["c"], np.float32)
    ctx = np.asarray(inp["ctx"], np.float32)
    c_ctx = np.asarray(inp["c_ctx"], np.float32)
    cores = list(range(8)) if cores is None else cores
    in_maps = []
    for b in cores:
        m = dict(sh)
        m["x"] = np.ascontiguousarray(x[b])
        m["ctx"] = np.ascontiguousarray(ctx[b])
        scT = np.zeros((128, 8, 2), np.float32)
        scT[:, :, 0] = c[b].reshape(8, 128).T
        scT[:, :, 1] = c_ctx.reshape(8, 128).T
        m["scT"] = scT.reshape(128, 16)
        in_maps.append(m)
    nc = build_program(cfg)
    res = run_bass_kernel_spmd(nc, in_maps, core_ids=list(range(len(cores))))
    return res


def kernel(**inputs):
    res = _run(inputs, {})
    return np.stack([r["out"] for r in res.results], 0).astype(np.float32)
```

```python
import contextlib
import math
import numpy as np
import concourse.bass as bass
import concourse.mybir as mybir
from concourse.bass_utils import run_bass_kernel_spmd

F32 = mybir.dt.float32
BF16 = mybir.dt.bfloat16
AF = mybir.ActivationFunctionType
ALU = mybir.AluOpType

EPOCH = 24000
NEG = -30000.0
D = 1024
NLAT = 16
NT = 18
TOK = NT * 128
DEPTH = 4
REGN = 37632


class Res:
    __slots__ = ("name", "w", "r", "psum")

    def __init__(self, name="", psum=False):
        self.name = name
        self.w = None
        self.r = []
        self.psum = psum


class Sched:
    ENGS = ("pe", "act", "dve", "pool", "sp")

    def __init__(self, nc):
        self.nc = nc
        self.streams = {e: [] for e in self.ENGS}
        self.cnt = {e: 0 for e in self.ENGS}
        self.waited = {e: {} for e in self.ENGS}
        self.semkeys = []
        self.semset = set()
        self.dmacnt = {}
        self.last = {}

    def _need(self, key):
        if key not in self.semset:
            self.semset.add(key)
            self.semkeys.append(key)

    def _wait(self, eng, k, v):
        if k[0] == eng and eng == "pe":
            return
        wd = self.waited[eng]
        if wd.get(k, 0) < v:
            wd[k] = v
            self.streams[eng].append(("wait", k, v))

    def _deps(self, eng, reads, writes):
        for r in reads:
            if r.w is not None:
                self._wait(eng, *r.w)
        for w in writes:
            if w.w is not None:
                self._wait(eng, *w.w)
            for t in w.r:
                self._wait(eng, *t)

    def _post(self, tok, reads, writes):
        self.last[tok[0]] = tok[1]
        for r in reads:
            r.r.append(tok)
            if len(r.r) > 64:
                best = {}
                for (k, v) in r.r:
                    if best.get(k, 0) < v:
                        best[k] = v
                r.r = list(best.items())
        for w in writes:
            w.w = tok
            w.r = []

    def op(self, eng, fn, reads=(), writes=()):
        if eng != "pe":
            ex = [r for r in reads if r.psum]
            if ex:
                writes = list(writes) + ex
        self._deps(eng, reads, writes)
        c = self.cnt[eng]
        key = (eng, c // EPOCH)
        val = c % EPOCH + 1
        self._need(key)
        self.cnt[eng] = c + 1
        self.streams[eng].append(("op", fn, key, 1))
        tok = (key, val)
        self._post(tok, reads, writes)
        return tok

    def dma(self, eng, fns, semname, reads=(), writes=()):
        self._deps(eng, reads, writes)
        key = ("dma", semname)
        self._need(key)
        c = self.dmacnt.get(key, 0)
        for fn in fns:
            self.streams[eng].append(("op", fn, key, 16))
            c += 16
        self.dmacnt[key] = c
        tok = (key, c)
        self._post(tok, reads, writes)
        return tok

    def barrier(self):
        toks = list(self.last.items())
        for eng in self.ENGS:
            for (k, v) in toks:
                self._wait(eng, k, v)

    def emit(self):
        nc = self.nc
        with contextlib.ExitStack() as es:
            sems = {}
            for i, k in enumerate(self.semkeys):
                sems[k] = es.enter_context(nc.semaphore("s%d" % i))
            block = es.enter_context(nc.Block())

            def runner(stream):
                def f(e):
                    for it in stream:
                        if it[0] == "wait":
                            e.wait_ge(sems[it[1]], it[2])
                        else:
                            it[1](e).then_inc(sems[it[2]], it[3])
                return f

            block.tensor(runner(self.streams["pe"]))
            block.scalar(runner(self.streams["act"]))
            block.vector(runner(self.streams["dve"]))
            block.gpsimd(runner(self.streams["pool"]))
            block.sync(runner(self.streams["sp"]))


def _rope_tables():
    t = np.arange(2048)
    rows = (t // 64).astype(np.float64)
    cols = (t % 64).astype(np.float64)
    inv = 1.0 / (10000.0 ** (np.arange(0, 32, 2, dtype=np.float64) / 32.0))
    C = np.zeros((64, 2048), np.float64)
    S = np.zeros((64, 2048), np.float64)
    ar = rows[None, :] * inv[:, None]
    ac = cols[None, :] * inv[:, None]
    C[0:16] = np.cos(ar); C[16:32] = np.cos(ar); C[32:48] = np.cos(ac); C[48:64] = np.cos(ac)
    S[0:16] = -np.sin(ar); S[16:32] = np.sin(ar); S[32:48] = -np.sin(ac); S[48:64] = np.sin(ac)
    C = np.concatenate([C, C], 0).astype(np.float32)
    S = np.concatenate([S, S], 0).astype(np.float32)
    return C, S


def _swap_idx():
    i = np.arange(64)
    return np.where(i < 16, i + 16, np.where(i < 32, i - 16, np.where(i < 48, i + 16, i - 16)))


def _const_tables():
    k = np.arange(128)[:, None]
    i = np.arange(128)[None, :]
    c = {}
    c["ident"] = np.eye(128, dtype=np.float32)
    c["triU"] = (k <= i).astype(np.float32)
    c["triL"] = (k >= i).astype(np.float32)
    c["maskF"] = np.where(k <= i, 0.0, NEG).astype(np.float32)
    c["maskB"] = np.where(k >= i, 0.0, NEG).astype(np.float32)
    mA = np.zeros((128, 3, 128), np.float32)
    mA[:, 0, :] = np.where(i <= k, 0.0, NEG)
    mA[:, 2, :] = np.where(k <= i, 0.0, NEG)
    c["maskA"] = mA
    C, S = _rope_tables()
    c["ropeC"] = C
    c["ropeS"] = S
    return c


def _na_table(rpb):
    kr = (np.arange(128) // 64)[:, None]
    kc = (np.arange(128) % 64)[:, None]
    qr = (np.arange(128) // 64)[None, :]
    qc = (np.arange(128) % 64)[None, :]
    cs = np.clip(qc - 8, 0, 48)
    colv = (kc >= cs) & (kc < cs + 16)
    coff = np.clip(kc - qc, -15, 15) + 15
    out = np.full((128, 4, 12, 128), NEG, np.float32)
    for v in range(12):
        if v < 5:
            dj = v - 2
            dr = 2 * dj + kr - qr
            rowv = (dr >= -4) & (dr <= 3)
        else:
            dj = v - 5 - 3
            dr = 2 * dj + kr - qr
            rowv = np.abs(dr) <= 7
        valid = rowv & colv
        drc = np.clip(dr + 7, 0, 14)
        for h in range(4):
            g = rpb[h][drc, coff]
            out[:, h, v, :] = np.where(valid, g, np.float32(NEG))
    return out


def _na_keys(t):
    if t < 2:
        return [(j, 5 + (j - t) + 3) for j in range(0, 4)]
    if t >= 14:
        return [(j, 5 + (j - t) + 3) for j in range(12, 16)]
    return [(j, (j - t) + 2) for j in range(t - 2, t + 3)]


def build_program(cfg):
    nlayers = cfg.get("nlayers", DEPTH)
    phases = cfg.get("phases", "ANSM")
    final_norm = cfg.get("final_norm", True)

    nc = bass.Bass("TRN2", target_bir_lowering=False)

    def din(name, shape):
        return nc.dram_tensor(name, list(shape), F32, kind="ExternalInput").ap()

    x_d = din("x", [2048, D])
    ctx_d = din("ctx", [256, D])
    scT_d = din("scT", [128, 16])
    w_mod_d = din("w_mod", [DEPTH, D, 6 * D])
    b_modT_d = din("b_modT", [DEPTH, 128, 48])
    b_mod_d = din("b_mod", [DEPTH, 6 * D])
    g_mixT_d = din("g_mixT", [DEPTH, 128, 8])
    g_ffnT_d = din("g_ffnT", [DEPTH, 128, 8])
    g_final_d = din("g_final", [1, D])
    w_inA_d = din("w_inA", [DEPTH, D, 896])
    w_in_d = din("w_in", [DEPTH, D, 2576])
    w_outP_d = din("w_outP", [DEPTH, D, D])
    sinkE_d = din("sinkE", [DEPTH, 128, 2])
    convw_d = din("convw", [DEPTH, 128, 30])
    convb_d = din("convb", [DEPTH, 128, 6])
    dtb_d = din("dtb", [DEPTH, 16])
    alog_d = din("alog", [DEPTH, 16])
    ssdd_d = din("ssdd", [DEPTH, 8])
    normgT_d = din("normgT", [DEPTH, 128, 4])
    naTab_d = din("naTab", [DEPTH, 128, 6144])
    w_rt_d = din("w_rt", [DEPTH, D, 36])
    b_rt_d = din("b_rt", [DEPTH, 36])
    wg_d = din("w_exp_gate", [DEPTH, 32, D, 256])
    wu_d = din("w_exp_up", [DEPTH, 32, D, 256])
    wd_d = din("w_exp_down", [DEPTH, 32, 256, D])
    ident_d = din("ident", [128, 128])
    triU_d = din("triU", [128, 128])
    triL_d = din("triL", [128, 128])
    maskF_d = din("maskF", [128, 128])
    maskB_d = din("maskB", [128, 128])
    maskA_d = din("maskA", [128, 384])
    ropeC_d = din("ropeC", [128, 2048])
    ropeS_d = din("ropeS", [128, 2048])
    out_d = nc.dram_tensor("out", [2048, D], F32, kind="ExternalOutput").ap()
    dbg_d = None
    if cfg.get("dump_ctx"):
        dbg_d = nc.dram_tensor("out_ctx", [256, D], F32, kind="ExternalOutput").ap()

    es = contextlib.ExitStack()
    with es:
        def sb(name, shape, dt):
            return es.enter_context(nc.sbuf_tensor("sb_" + name, list(shape), dt))

        S = Sched(nc)

        X = sb("X", [128, NT, D], F32)
        HT = sb("HT", [128, 8, TOK], BF16)
        REG = sb("REG", [128, REGN], BF16)
        GATE = sb("GATE", [128, 4, D], F32)
        ident_f = sb("ident_f", [128, 128], F32)
        ident_b = sb("ident_b", [128, 128], BF16)
        triU = sb("triU", [128, 128], F32)
        triL = sb("triL", [128, 128], F32)
        maskF = sb("maskF", [128, 128], F32)
        maskB = sb("maskB", [128, 128], F32)
        maskA = sb("maskA", [128, 3, 128], BF16)
        ones_f = sb("ones_f", [128, 128], F32)
        ones_b = sb("ones_b", [128, 128], BF16)
        scT = sb("scT", [128, 16], F32)
        sc_b = sb("sc_b", [128, 8, 2], BF16)
        sc_rep = sb("sc_rep", [128, 2, 8, 128], BF16)
        modT = sb("modT", [128, 48, 2], F32)
        bmT = sb("bmT", [128, 48], F32)
        gT = sb("gT", [128, 16], F32)
        AV = sb("AV", [128, 2, 2, 8], F32)
        SV = sb("SV", [128, 2, 2, 8], F32)
        small = sb("small", [128, 64], F32)

        banks = [es.enter_context(nc.psum_tensor("bank%d" % i, [128, 512], F32)) for i in range(8)]
        RB = [Res("bank%d" % i, psum=True) for i in range(8)]

        RX = [Res("X%d" % t) for t in range(NT)]
        RH = [Res("H%d" % t) for t in range(NT)]
        Rc = {}

        def R(name):
            if name not in Rc:
                Rc[name] = Res(name)
            return Rc[name]

        class Carver:
            def __init__(self, base, nbytes):
                self.base = base
                self.off = 0
                self.nbytes = nbytes

            def take(self, shape, dt):
                esz = 2 if dt == BF16 else 4
                n = int(np.prod(shape[1:]))
                nb = n * esz
                nb_al = (nb + 63) // 64 * 64
                assert self.off + nb_al <= self.nbytes, ("region overflow", self.off, nb_al, self.nbytes)
                a = self.base[:, self.off // 2:(self.off + nb) // 2]
                self.off += nb_al
                if dt != BF16:
                    a = a.bitcast(dt)
                if len(shape) > 2:
                    names = " ".join("d%d" % i for i in range(1, len(shape)))
                    kw = {"d%d" % i: shape[i] for i in range(1, len(shape))}
                    a = a.rearrange("p (%s) -> p %s" % (names, names), **kw)
                return a

        def region():
            return Carver(REG, REGN * 2)

        def ht_region():
            return Carver(HT[:].rearrange("p k t -> p (k t)"), 8 * TOK * 2)

        def mm(out, lhsT, rhs, start, stop, reads, writes, sgc=False):
            if sgc:
                S.op("pe", lambda e: e.matmul(out, lhsT=lhsT, rhs=rhs, start=start, stop=stop, skip_group_check=True), reads, writes)
            else:
                S.op("pe", lambda e: e.matmul(out, lhsT=lhsT, rhs=rhs, start=start, stop=stop), reads, writes)

        def tr(out, in_, ident, reads, writes):
            S.op("pe", lambda e: e.transpose(out=out, in_=in_, identity=ident), reads, writes)

        def act(out, in_, func, reads, writes, bias=None, scale=None, accum_out=None):
            kw = {}
            if bias is not None:
                kw["bias"] = bias
            if scale is not None:
                kw["scale"] = scale
            if accum_out is not None:
                kw["accum_out"] = accum_out
            S.op("act", lambda e: e.activation(out=out, in_=in_, func=func, **kw), reads, writes)

        def tt(eng, out, in0, in1, op, reads, writes):
            S.op(eng, lambda e: e.tensor_tensor(out=out, in0=in0, in1=in1, op=op), reads, writes)

        def ts(eng, out, in0, s1, op0, reads, writes, s2=None, op1=None):
            if op1 is None:
                S.op(eng, lambda e: e.tensor_scalar(out=out, in0=in0, scalar1=s1, scalar2=None, op0=op0), reads, writes)
            else:
                S.op(eng, lambda e: e.tensor_scalar(out=out, in0=in0, scalar1=s1, scalar2=s2, op0=op0, op1=op1), reads, writes)

        def stt(out, in0, scalar, in1, op0, op1, reads, writes):
            S.op("dve", lambda e: e.scalar_tensor_tensor(out=out, in0=in0, scalar=scalar, in1=in1, op0=op0, op1=op1), reads, writes)

        def cp(eng, out, in_, reads, writes):
            if eng == "act_copy":
                S.op("act", lambda e: e.activation(out=out, in_=in_, func=AF.Copy), reads, writes)
            else:
                S.op(eng, lambda e: e.tensor_copy(out=out, in_=in_), reads, writes)

        def memset(eng, ap, val, writes):
            S.op(eng, lambda e: e.memset(ap, val), (), writes)

        dma_ctr = [0]

        def load(q, out, in_, writes, reads=(), sem=None):
            if sem is None:
                sem = "u%d" % (dma_ctr[0] % 40)
                dma_ctr[0] += 1
            S.dma(q, [lambda e: e.dma_start(out=out, in_=in_)], sem, reads, writes)

        def wview(w2d, ncols_lo, ncols_hi):
            return w2d[:, ncols_lo:ncols_hi].rearrange("(k p) n -> p k n", p=128)

        xv = x_d.rearrange("(t p) d -> p t d", p=128)
        cv = ctx_d.rearrange("(t p) d -> p t d", p=128)
        for t in range(NLAT):
            load("sp", X[:, t, :], xv[:, t, :], [RX[t]])
        for t in range(2):
            load("sp", X[:, NLAT + t, :], cv[:, t, :], [RX[NLAT + t]])
        load("sp", ident_f[:], ident_d, [R("ident_f")])
        load("pool", ident_b[:], ident_d, [R("ident_b")])
        load("sp", triU[:], triU_d, [R("triU")])
        load("sp", triL[:], triL_d, [R("triL")])
        load("sp", maskF[:], maskF_d, [R("maskF")])
        load("sp", maskB[:], maskB_d, [R("maskB")])
        load("pool", maskA[:].rearrange("p a b -> p (a b)"), maskA_d, [R("maskA")])
        load("sp", scT[:], scT_d, [R("scT")])
        memset("dve", ones_f[:], 1.0, [R("ones_f")])
        memset("dve", ones_b[:], 1.0, [R("ones_b")])
        act(scT[:], scT[:], AF.Silu, [R("scT")], [R("scT")])
        cp("dve", sc_b[:].rearrange("p k j -> p (k j)"), scT[:], [R("scT")], [R("sc_b")])
        for j in range(2):
            for k in range(8):
                ts("dve", sc_rep[:, j, k, :], ones_f[:], scT[:, 2 * k + j:2 * k + j + 1], ALU.mult,
                   [R("scT"), R("ones_f")], [R("sc_rep")])

        def mod_phase(l):
            S.barrier()
            rg = region()
            wb = [rg.take([128, 8, 512], BF16) for _ in range(3)]
            Rw = [Res("modw%d" % i) for i in range(3)]
            brow = rg.take([128, 4, 512], F32)
            load("sp", bmT[:], b_modT_d[l], [R("bmT")])
            load("sp", gT[:, 0:8], g_mixT_d[l], [R("gT")])
            load("sp", gT[:, 8:16], g_ffnT_d[l], [R("gT")])
            gate_chunks = {4: (0, 0), 5: (0, 1), 10: (1, 0), 11: (1, 1)}
            gi = 0
            for ch in range(12):
                i = ch % 3
                load("pool", wb[i], wview(w_mod_d[l], ch * 512, ch * 512 + 512), [Rw[i]], sem="modw%d" % i)
                if ch in gate_chunks:
                    g, half = gate_chunks[ch]
                    load("sp", brow[:, gi, :], b_mod_d[l:l + 1, ch * 512:ch * 512 + 512].partition_broadcast(128),
                         [R("brow")])
                    for j in range(2):
                        bk = banks[j]
                        for k in range(8):
                            mm(bk[:, :], sc_rep[:, j, k, :], wb[i][:, k, :], k == 0, k == 7,
                               [R("sc_rep"), Rw[i]], [RB[j]])
                        tt("dve", GATE[:, 2 * g + j, half * 512:half * 512 + 512], bk[:, :], brow[:, gi, :], ALU.add,
                           [RB[j], R("brow")], [R("GATE")])
                    gi += 1
                else:
                    for sub in range(4):
                        jn = ch * 4 + sub
                        for k in range(8):
                            mm(banks[2][:, 2 * jn:2 * jn + 2], wb[i][:, k, sub * 128:sub * 128 + 128], sc_b[:, k, :],
                               k == 0, k == 7, [R("sc_b"), Rw[i]], [RB[2]])
            mp = banks[2][:, 0:96].rearrange("p (j c) -> p j c", c=2)
            for j in range(2):
                tt("dve", modT[:, :, j], mp[:, :, j], bmT[:], ALU.add, [RB[2], R("bmT")], [R("modT")])
            for n in range(2):
                base = 0 if n == 0 else 24
                for j in range(2):
                    stt(AV[:, n, j, :], modT[:, base + 8:base + 16, j], 1.0, gT[:, 8 * n:8 * n + 8], ALU.add, ALU.mult,
                        [R("modT"), R("gT")], [R("AV")])
                    cp("dve", SV[:, n, j, :], modT[:, base:base + 8, j], [R("modT")], [R("SV")])

        def norm_phase(n, tiles, router=None, rg=None):
            if rg is None:
                rg = region()
            junk = rg.take([128, D], BF16)
            XN = rg.take([128, 2, D], F32)
            HF = rg.take([128, 2, 8, 128], F32)
            def stats(t):
                act(junk[:], X[:, t, :], AF.Square, [RX[t]], [R("junk"), R("ss%d" % t)], accum_out=small[:, t:t + 1])
                act(small[:, 32 + t:33 + t], small[:, t:t + 1], AF.Ln, [R("ss%d" % t)], [R("rs%d" % t)], bias=1e-6, scale=1.0 / D)
                act(small[:, 32 + t:33 + t], small[:, 32 + t:33 + t], AF.Exp, [R("rs%d" % t)], [R("rs%d" % t)], scale=-0.5)

            stats(tiles[0])
            if len(tiles) > 1:
                stats(tiles[1])
            for idx, t in enumerate(tiles):
                if idx + 2 < len(tiles):
                    stats(tiles[idx + 2])
                j = 0 if t < NLAT else 1
                b = idx % 2
                rxn, rhf = R("XN%d" % b), R("HF%d" % b)
                ts("dve", XN[:, b, :], X[:, t, :], small[:, 32 + t:33 + t], ALU.mult, [RX[t], R("rs%d" % t)], [rxn])
                pb = [banks[2 * b], banks[2 * b + 1]]
                for k in range(8):
                    bk = pb[k // 4]
                    mm(bk[:, (k % 4) * 128:(k % 4) * 128 + 128], XN[:, b, k * 128:k * 128 + 128], ident_f[:], True, True,
                       [rxn, R("ident_f")], [RB[2 * b + k // 4]])
                for k in range(8):
                    bk = pb[k // 4]
                    if k % 4 < 2:
                        act(HF[:, b, k, :], bk[:, (k % 4) * 128:(k % 4) * 128 + 128], AF.Identity,
                            [RB[2 * b + k // 4], R("AV"), R("SV")], [rhf],
                            bias=SV[:, n, j, k:k + 1], scale=AV[:, n, j, k:k + 1])
                    else:
                        ts("dve", HF[:, b, k, :], bk[:, (k % 4) * 128:(k % 4) * 128 + 128], AV[:, n, j, k:k + 1], ALU.mult,
                           [RB[2 * b + k // 4], R("AV"), R("SV")], [rhf], s2=SV[:, n, j, k:k + 1], op1=ALU.add)
                cp("pool", HT[:, :, t * 128:t * 128 + 128], HF[:, b, :, :], [rhf], [RH[t]])
                if router is not None:
                    router(t, HF[:, b, :, :], rhf)

        def x_update(t, ps_lo, ps_hi, rb_lo, rb_hi, g, tmp, rtmp):
            for half, (ps, rb) in enumerate(((ps_lo, rb_lo), (ps_hi, rb_hi))):
                sl = slice(half * 512, half * 512 + 512)
                tt("dve", tmp[:, sl], ps, GATE[:, g, sl], ALU.mult, [rb, R("GATE")], [rtmp])
                tt("dve", X[:, t, sl], X[:, t, sl], tmp[:, sl], ALU.add, [rtmp, RX[t]], [RX[t]])

        def proj_fm(dst, rdst, wt, rw, col0, tiles_blocks, evac):
            for bi, (t0, ntile) in enumerate(tiles_blocks):
                bk = banks[bi % 2]
                n = ntile * 128
                for k in range(8):
                    mm(bk[:, 0:n], wt[:, k, col0:col0 + 128], HT[:, k, t0 * 128:t0 * 128 + n], k == 0, k == 7,
                       [rw] + RH[t0:t0 + ntile], [RB[bi % 2]])
                evac(bk[:, 0:n], RB[bi % 2], t0, n)

        BLOCKS = [(0, 4), (4, 4), (8, 4), (12, 4), (16, 2)]

        def attn_phase(kind, l, need_ctx):
            S.barrier()
            rg = region()
            if kind == "A":
                ncolw = 896
                wt = rg.take([128, 8, ncolw], BF16)
                load("pool", wt, wview(w_inA_d[l], 0, 896), [R("wt")], sem="wt")
                ropeC = rg.take([128, 2048], F32)
                ropeS = rg.take([128, 2048], F32)
                load("sp", ropeC, ropeC_d, [R("ropeC")])
                load("sp", ropeS, ropeS_d, [R("ropeS")])
                nq = 2
                Qs = [rg.take([128, TOK], BF16) for _ in range(2)]
                Ks = [rg.take([128, TOK], BF16)]
                Ks = [Ks[0], Ks[0]]
                vcols = 128
                Vtm = rg.take([128, NT, vcols], BF16)
                t1s = [rg.take([128, 512], F32) for _ in range(2)]
                t2s = [rg.take([128, 512], F32) for _ in range(2)]
                ropectr = [0]
                wo = rg.take([128, 2, D], BF16)
                load("pool", wo, w_outP_d[l][0:256, :].rearrange("(k p) n -> p k n", p=128), [R("wo")], sem="wo")
                esink = rg.take([128, 2], F32)
                load("sp", esink, sinkE_d[l], [R("esink")])
                act(esink, esink, AF.Exp, [R("esink")], [R("esink")])
                scale = 0.125
                nkmax = 5
                for ci, (dst, rn) in enumerate(((Qs[0], "Q0"), (Qs[1], "Q1"), (Ks[0], "K0"))):
                    for bi, (t0, ntile) in enumerate(BLOCKS):
                        n = ntile * 128
                        b0, b1 = banks[2 * (bi % 2)], banks[2 * (bi % 2) + 1]
                        r0, r1 = RB[2 * (bi % 2)], RB[2 * (bi % 2) + 1]
                        for k in range(8):
                            mm(b0[:, 0:n], wt[:, k, ci * 128:ci * 128 + 128], HT[:, k, t0 * 128:t0 * 128 + n], k == 0, k == 7,
                               [R("wt")] + RH[t0:t0 + ntile], [r0])
                        if t0 < NLAT:
                            for k in range(8):
                                mm(b1[:, 0:n], wt[:, k, (ci + 3) * 128:(ci + 3) * 128 + 128], HT[:, k, t0 * 128:t0 * 128 + n],
                                   k == 0, k == 7, [R("wt")] + RH[t0:t0 + ntile], [r1])
                            rb_ = ropectr[0] % 2
                            ropectr[0] += 1
                            t1, t2 = t1s[rb_], t2s[rb_]
                            tt("dve", t1[:, 0:n], b0[:, 0:n], ropeC[:, t0 * 128:t0 * 128 + n], ALU.mult, [r0, R("ropeC")], [R("t1_%d" % rb_)])
                            tt("dve", t2[:, 0:n], b1[:, 0:n], ropeS[:, t0 * 128:t0 * 128 + n], ALU.mult, [r1, R("ropeS")], [R("t2_%d" % rb_)])
                            tt("pool", dst[:, t0 * 128:t0 * 128 + n], t1[:, 0:n], t2[:, 0:n], ALU.add, [R("t1_%d" % rb_), R("t2_%d" % rb_)], [R(rn)])
                        else:
                            act(dst[:, t0 * 128:t0 * 128 + n], b0[:, 0:n], AF.Copy, [r0], [R(rn)])
                Rc["K1"] = Rc["K0"]
                vcol0 = 768
            else:
                wt = rg.take([128, 8, 768], BF16)
                load("pool", wt, wview(w_in_d[l], 1808, 2576), [R("wt")], sem="wt")
                tab = rg.take([128, 4, 12, 128], BF16)
                load("pool", tab[:].rearrange("p a b c -> p (a b c)"), naTab_d[l], [R("biasT")], sem="tab")
                Qs = [rg.take([128, TOK], BF16) for _ in range(2)]
                Ks = [rg.take([128, TOK], BF16) for _ in range(2)]
                vcols = 256
                Vtm = rg.take([128, NT, vcols], BF16)
                wo = rg.take([128, 2, D], BF16)
                load("pool", wo, w_outP_d[l][768:1024, :].rearrange("(k p) n -> p k n", p=128), [R("wo")], sem="wo")
                scale = 1.0
                nkmax = 7
                for ci, (dst, rn, sc_) in enumerate(((Qs[0], "Q0", 0.125), (Qs[1], "Q1", 0.125), (Ks[0], "K0", 1.0), (Ks[1], "K1", 1.0))):
                    for bi, (t0, ntile) in enumerate(BLOCKS):
                        n = ntile * 128
                        b0, r0 = banks[bi % 4], RB[bi % 4]
                        for k in range(8):
                            mm(b0[:, 0:n], wt[:, k, ci * 128:ci * 128 + 128], HT[:, k, t0 * 128:t0 * 128 + n], k == 0, k == 7,
                               [R("wt")] + RH[t0:t0 + ntile], [r0])
                        ts("dve", dst[:, t0 * 128:t0 * 128 + n], b0[:, 0:n], sc_, ALU.mult, [r0], [R(rn)])
                vcol0 = 512
            for t in range(NT):
                bk, rb = banks[4 + t % 4], RB[4 + t % 4]
                for k in range(8):
                    mm(bk[:, 0:vcols], HT[:, k, t * 128:t * 128 + 128], wt[:, k, vcol0:vcol0 + vcols], k == 0, k == 7,
                       [R("wt"), RH[t]], [rb])
                cp("dve", Vtm[:, t, :], bk[:, 0:vcols], [rb], [R("V")])

            PT = [rg.take([128, nkmax * 128], BF16) for _ in range(2)]
            RPT = [Res("PT%d" % i) for i in range(2)]
            OT = [rg.take([128, 128], BF16) for _ in range(4)]
            ROT = [Res("OT%d" % i) for i in range(4)]
            RDn = [rg.take([128, 128], F32) for _ in range(2)]
            RRD = [Res("RD%d" % i) for i in range(2)]
            tmp = rg.take([128, D], F32)
            rtmp = Res("updtmp")

            def keys_of(t):
                if t >= NLAT:
                    return [(16, None), (17, None)]
                if kind == "A":
                    ks = [(j, j - t + 1) for j in (t - 1, t, t + 1) if 0 <= j < NLAT]
                    ks = [(j, (b if b != 1 else None)) for (j, b) in ks]
                else:
                    ks = _na_keys(t)
                return ks + [(16, None), (17, None)]

            def bias_ap(pr, half, bid):
                if kind == "A":
                    return maskA[:, bid, :]
                return tab[:, 2 * pr + half, bid, :]

            qtiles = list(range(NLAT)) + ([16, 17] if need_ctx else [])
            work = [(t, pr, half) for t in qtiles for pr in range(2) for half in range(2)]

            def scores(i):
                t, pr, half = work[i]
                keys = keys_of(t)
                ps_ = slice(64 * half, 64 * half + 64)
                for kk, (j, bid) in enumerate(keys):
                    col = kk * 128
                    bi_ = 2 * (i % 2) + col // 512
                    c0 = col % 512
                    bk, rb = banks[bi_], RB[bi_]
                    has_b = bid is not None
                    mm(bk[:, c0:c0 + 128], Ks[pr][ps_, j * 128:j * 128 + 128], Qs[pr][ps_, t * 128:t * 128 + 128],
                       True, not has_b, [R("K%d" % pr), R("Q%d" % pr)], [rb])
                    if has_b:
                        mm(bk[:, c0:c0 + 128], ident_b[:], bias_ap(pr, half, bid), False, True,
                           [R("ident_b"), R("biasT"), R("maskA")], [rb])

            def expo(i):
                t, pr, half = work[i]
                ncol = len(keys_of(t)) * 128
                for q in range((ncol + 511) // 512):
                    n = min(512, ncol - q * 512)
                    bi_ = 2 * (i % 2) + q
                    act(PT[i % 2][:, q * 512:q * 512 + n], banks[bi_][:, 0:n], AF.Exp, [RB[bi_]], [RPT[i % 2]], scale=scale)

            def pv(i):
                t, pr, half = work[i]
                ip = i // 2
                keys = keys_of(t)
                nk = len(keys)
                ob, rob = banks[4 + (ip % 2)], RB[4 + (ip % 2)]
                ps_ = slice(64 * half, 64 * half + 64)
                if kind == "A":
                    vsl = slice(64 * half, 64 * half + 64)
                else:
                    h = 2 * pr + half
                    vsl = slice(64 * h, 64 * h + 64)
                for kk, (j, bid) in enumerate(keys):
                    p_ = PT[i % 2][:, kk * 128:kk * 128 + 128]
                    mm(ob[ps_, 0:128], Vtm[:, j, vsl], p_, kk == 0, kk == nk - 1, [R("V"), RPT[i % 2]], [rob])
                for kk, (j, bid) in enumerate(keys):
                    p_ = PT[i % 2][:, kk * 128:kk * 128 + 128]
                    mm(ob[ps_, 128:256], ones_b[:, 0:64], p_, kk == 0, kk == nk - 1, [R("ones_b"), RPT[i % 2]], [rob])
                if half == 0:
                    return
                rd, rrd = RDn[ip % 2], RRD[ip % 2]
                if kind == "A":
                    ts("dve", rd, ob[:, 128:256], esink[:, pr:pr + 1], ALU.add, [rob, R("esink")], [rrd])
                    S.op("dve", lambda e: e.reciprocal(out=rd, in_=rd), [rrd], [rrd])
                else:
                    S.op("dve", lambda e: e.reciprocal(out=rd, in_=ob[:, 128:256]), [rob], [rrd])
                tt("dve", OT[ip % 4], ob[:, 0:128], rd, ALU.mult, [rob, rrd], [ROT[ip % 4]])
                if pr == 0:
                    return
                pending.append((t, ip))

            pending = []

            def proj_tile(t, ip):
                j = 0 if t < NLAT else 1
                o0, o1 = OT[(ip - 1) % 4], OT[ip % 4]
                ro0, ro1 = ROT[(ip - 1) % 4], ROT[ip % 4]
                for hf in range(2):
                    bk, rb = banks[6 + hf], RB[6 + hf]
                    sl = slice(hf * 512, hf * 512 + 512)
                    mm(bk[:, :], o0, wo[:, 0, sl], True, False, [ro0, R("wo")], [rb])
                    mm(bk[:, :], o1, wo[:, 1, sl], False, True, [ro1, R("wo")], [rb])
                x_update(t, banks[6][:, :], banks[7][:, :], RB[6], RB[7], 0 + j, tmp, rtmp)

            n = len(work)
            scores(0)
            for i in range(n):
                expo(i)
                if i + 1 < n:
                    scores(i + 1)
                if len(pending) and work[i][1:] == (0, 1):
                    proj_tile(*pending.pop(0))
                pv(i)
            while pending:
                proj_tile(*pending.pop(0))

        def bc_mid(ap2d, n):
            return ap2d.unsqueeze(1).to_broadcast([128, n, ap2d.shape[1]])

        def bc_last(ap2d, n):
            return ap2d.unsqueeze(2).to_broadcast([128, ap2d.shape[1], n])

        def ssd_phase(l, need_ctx):
            S.barrier()
            rg = region()
            xs_tm = rg.take([128, NT, 512], BF16)
            B_tm = rg.take([128, NT, 128], BF16)
            BT = rg.take([128, TOK], BF16)
            CT = rg.take([128, TOK], BF16)
            dt = rg.take([128, NT, 16], F32)
            da = rg.take([128, NT, 16], F32)
            acs = rg.take([128, NT, 16], F32)
            scw = rg.take([128, NT, 16], F32)
            etg = rg.take([128, NT, 8], F32)
            convw = rg.take([128, 30], F32)
            convb = rg.take([128, 6], F32)
            dtb_b = rg.take([128, 16], F32)
            a_b = rg.take([128, 16], F32)
            Db = rg.take([128, 8], F32)
            normg = rg.take([128, 4], F32)
            mark = rg.off
            load("sp", convw, convw_d[l], [R("convw")])
            load("sp", convb, convb_d[l], [R("convb")])
            load("sp", dtb_b, dtb_d[l:l + 1, :].partition_broadcast(128), [R("dtb_b")])
            load("sp", a_b, alog_d[l:l + 1, :].partition_broadcast(128), [R("a_b")])
            load("sp", Db, ssdd_d[l:l + 1, :].partition_broadcast(128), [R("Db")])
            load("sp", normg, normgT_d[l], [R("normg")])
            act(a_b, a_b, AF.Exp, [R("a_b")], [R("a_b")])
            ts("dve", a_b, a_b, -1.0, ALU.mult, [R("a_b")], [R("a_b")])

            wx = [rg.take([128, 8, 128], BF16) for _ in range(2)]
            Rwx = [Res("wx%d" % i) for i in range(2)]
            pre2 = [rg.take([128, TOK], F32) for _ in range(2)]
            Rpre = [Res("pre%d" % i) for i in range(2)]
            acc = rg.take([128, TOK], F32)
            post = rg.take([128, TOK], BF16)
            SEGS = [(0, 2048), (2048, 2304)]
            def stageP(ci):
                i = ci % 2
                pre, rpre = pre2[i], Rpre[i]
                load("pool", wx[i], wview(w_in_d[l], 1024 + ci * 128, 1024 + ci * 128 + 128), [Rwx[i]], sem="wx%d" % i)
                for bi, (t0, ntile) in enumerate(BLOCKS):
                    n = ntile * 128
                    bk, rb = banks[bi % 4], RB[bi % 4]
                    for k in range(8):
                        mm(bk[:, 0:n], wx[i][:, k, :], HT[:, k, t0 * 128:t0 * 128 + n], k == 0, k == 7,
                           [Rwx[i]] + RH[t0:t0 + ntile], [rb])
                    act(pre[:, t0 * 128:t0 * 128 + n], bk[:, 0:n], AF.Copy, [rb], [rpre])

            def stageC(ci):
                i = ci % 2
                pre, rpre = pre2[i], Rpre[i]
                for (a, b) in SEGS:
                    ts("dve", acc[:, a:b], pre[:, a:b], convw[:, ci * 5 + 2:ci * 5 + 3], ALU.mult, [rpre, R("convw"), R("convb")], [R("acc")],
                       s2=convb[:, ci:ci + 1], op1=ALU.add)
                    for kk in (0, 1, 3, 4):
                        s = kk - 2
                        lo = max(a, a - s)
                        hi = min(b, b - s)
                        stt(acc[:, lo:hi], pre[:, lo + s:hi + s], convw[:, ci * 5 + kk:ci * 5 + kk + 1], acc[:, lo:hi], ALU.mult, ALU.add,
                            [rpre, R("convw"), R("acc")], [R("acc")])

            def stageF(ci):
                if ci < 4:
                    dst_fm, rdst = post, R("post")
                elif ci == 4:
                    dst_fm, rdst = BT, R("BT")
                else:
                    dst_fm, rdst = CT, R("CT")
                act(dst_fm, acc, AF.Silu, [R("acc")], [rdst])
                if ci <= 4:
                    for g0 in range(0, NT, 8):
                        ng = min(8, NT - g0)
                        bi_ = 4 + (g0 // 8) % 2 + 2 * (ci % 2)
                        bk, rb = banks[bi_], RB[bi_]
                        bv = bk.bitcast(BF16)
                        for q in range(ng):
                            t = g0 + q
                            tr(bv[:, q * 128:q * 128 + 128], dst_fm[:, t * 128:t * 128 + 128], ident_b[:], [rdst, R("ident_b")], [rb])
                        src = bv[:, 0:ng * 128].rearrange("p (q c) -> p q c", c=128)
                        if ci < 4:
                            cp("act_copy", xs_tm[:, g0:g0 + ng, ci * 128:ci * 128 + 128], src, [rb], [R("xs_tm")])
                        else:
                            cp("act_copy", B_tm[:, g0:g0 + ng, :], src, [rb], [R("B_tm")])

            stageP(0)
            for ci in range(6):
                if ci + 1 < 6:
                    stageP(ci + 1)
                stageC(ci)
                stageF(ci)

            if cfg.get("ssd_stop", 9) <= 1:
                return
            S.barrier()
            rg.off = mark
            zs = rg.take([128, NT, 512], BF16)
            mark2 = rg.off
            wz = rg.take([128, 8, 512], BF16)
            wdt = rg.take([128, 8, 16], BF16)
            tot = rg.take([128, NT, 16], F32)
            etot = rg.take([128, NT, 16], F32)
            load("pool", wz, wview(w_in_d[l], 512, 1024), [R("wz")], sem="wz")
            load("pool", wdt, wview(w_in_d[l], 1792, 1808), [R("wdt")], sem="wdt")
            for t in range(NT):
                for k in range(8):
                    mm(banks[0][:, t * 16:t * 16 + 16], HT[:, k, t * 128:t * 128 + 128], wdt[:, k, :], k == 0, k == 7,
                       [R("wdt"), RH[t]], [RB[0]])
            p0 = banks[0][:, 0:NT * 16].rearrange("p (t c) -> p t c", c=16)
            tt("dve", dt, p0, bc_mid(dtb_b, NT), ALU.add, [RB[0], R("dtb_b")], [R("dt")])
            act(dt, dt, AF.Exp, [R("dt")], [R("dt")])
            act(dt, dt, AF.Ln, [R("dt")], [R("dt")], bias=1.0, scale=1.0)
            tt("dve", da, dt, bc_mid(a_b, NT), ALU.mult, [R("dt"), R("a_b")], [R("da")])
            for t in range(NT):
                mm(banks[1][:, t * 16:t * 16 + 8], triU[:], da[:, t, 0:8], True, True, [R("triU"), R("da")], [RB[1]])
                mm(banks[1][:, t * 16 + 8:t * 16 + 16], triL[:], da[:, t, 8:16], True, True, [R("triL"), R("da")], [RB[1]])
                mm(banks[2][:, t * 16:t * 16 + 16], ones_f[:], da[:, t, :], True, True, [R("ones_f"), R("da")], [RB[2]])
            p1 = banks[1][:, 0:NT * 16].rearrange("p (t c) -> p t c", c=16)
            p2 = banks[2][:, 0:NT * 16].rearrange("p (t c) -> p t c", c=16)
            cp("dve", acs, p1, [RB[1]], [R("acs")])
            cp("dve", tot, p2, [RB[2]], [R("tot")])
            tt("dve", scw, tot, acs, ALU.subtract, [R("tot"), R("acs")], [R("scw")])
            act(scw, scw, AF.Exp, [R("scw")], [R("scw")])
            tt("dve", scw, scw, dt, ALU.mult, [R("scw"), R("dt")], [R("scw")])
            act(etot, tot, AF.Exp, [R("tot")], [R("etot")])
            for d_ in range(2):
                for g in range(2):
                    cp("dve", etg[64 * g:64 * g + 64, :, 4 * d_:4 * d_ + 4], etot[64 * g:64 * g + 64, :, 8 * d_ + 4 * g:8 * d_ + 4 * g + 4],
                       [R("etot")], [R("etg")])
            for t in range(NT):
                if t >= NLAT and not need_ctx:
                    continue
                bk, rb = banks[4 + t % 4], RB[4 + t % 4]
                for k in range(8):
                    mm(bk[:, :], HT[:, k, t * 128:t * 128 + 128], wz[:, k, :], k == 0, k == 7, [R("wz"), RH[t]], [rb])
                act(zs[:, t, :], bk[:, :], AF.Silu, [rb], [R("zs")])

            if cfg.get("ssd_stop", 9) <= 2:
                return
            S.barrier()
            rg.off = mark2
            prevB = rg.take([128, NT, 256], BF16)
            woS = rg.take([128, 4, D], BF16)
            load("pool", woS, w_outP_d[l][256:768, :].rearrange("(k p) n -> p k n", p=128), [R("woS")], sem="woS")
            hr = ht_region()
            xw = [hr.take([128, 512], BF16) for _ in range(2)]
            Rxw = [Res("xw%d" % i) for i in range(2)]
            xdt = [hr.take([128, 512], BF16) for _ in range(2)]
            Rxdt = [Res("xdt%d" % i) for i in range(2)]
            rhsU = [hr.take([128, 8, 128], F32) for _ in range(2)]
            RrhsU = [Res("rhsU%d" % i) for i in range(2)]
            E = [hr.take([128, 8, 128], BF16) for _ in range(2)]
            RE = [Res("E%d" % i) for i in range(2)]
            Eb = [hr.take([128, 8, 128], BF16) for _ in range(2)]
            REb = [Res("Eb%d" % i) for i in range(2)]
            Gm = [hr.take([128, 2, 128], BF16) for _ in range(2)]
            RGm = [Res("Gm%d" % i) for i in range(2)]
            nacs = hr.take([128, NT, 16], F32)
            Sf = hr.take([128, 256], F32)
            Sf_bf = hr.take([128, 256], BF16)
            Sb = hr.take([128, 256], F32)
            tmpD = hr.take([128, 512], F32)
            ytot = hr.take([128, 512], F32)
            yn = hr.take([128, 512], BF16)
            oT = hr.take([128, 4, 128], BF16)
            upd = hr.take([128, 512], F32)
            sjunk = hr.take([128, 512], BF16)
            ssq = hr.take([128, 64], F32)
            rupd = Res("updS")
            for t in range(NT):
                RH[t] = Res("H%d" % t)

            memset("dve", Sb, 0.0, [R("Sb")])
            memset("dve", Sf, 0.0, [R("Sf")])
            memset("dve", Sf_bf, 0.0, [R("Sf_bf")])
            ts("dve", nacs, acs, -1.0, ALU.mult, [R("acs")], [R("nacs")])

            def xs3(c):
                return xs_tm[:, c, :].rearrange("p (h q) -> p h q", q=64)

            STB0 = 5

            def states(c, d_, i, Sacc, rS, alt=False):
                STB = 7 if (alt and i % 2 == 1) else STB0
                tt("pool", xw[i][:].rearrange("p (h q) -> p h q", q=64), xs3(c), bc_last(scw[:, c, 8 * d_:8 * d_ + 8], 64), ALU.mult,
                   [R("xs_tm"), R("scw")], [Rxw[i]])
                for g in range(2):
                    mm(banks[STB][64 * g:64 * g + 64, 256:512], B_tm[:, c, 64 * g:64 * g + 64], xw[i][:, 256 * g:256 * g + 256], True, True,
                       [R("B_tm"), Rxw[i]], [RB[STB]])
                for hl in range(4):
                    sl = slice(64 * hl, 64 * hl + 64)
                    stt(Sacc[:, sl], Sacc[:, sl], etg[:, c, 4 * d_ + hl:4 * d_ + hl + 1], banks[STB][:, 256 + 64 * hl:256 + 64 * hl + 64], ALU.mult, ALU.add,
                        [rS, R("etg"), RB[STB]], [rS])

            for n_, c in enumerate([17, 16] + list(range(15, -1, -1))):
                cp("act_copy", prevB[:, c, :], Sb, [R("Sb")], [R("prevB")])
                if c != 0:
                    states(c, 1, n_ % 2, Sb, R("Sb"), alt=True)

            order2 = [16, 17] + list(range(NLAT))

            def front(n_, c):
                emit_out = need_ctx or c < NLAT
                csl = slice(c * 128, c * 128 + 128)
                if emit_out:
                    gbanks = ((banks[2][:, 0:128], RB[2]), (banks[3][:, 0:128], RB[3]))
                    for g in range(2):
                        ps_ = slice(64 * g, 64 * g + 64)
                        mm(gbanks[g][0], BT[ps_, csl], CT[ps_, csl], True, True, [R("BT"), R("CT")], [gbanks[g][1]])
                    for d_ in range(2):
                        tri = triU if d_ == 0 else triL
                        for g in range(2):
                            tt("dve", Gm[d_][:, g, :], gbanks[g][0], tri[:], ALU.mult, [gbanks[g][1], R("triU"), R("triL")], [RGm[d_]])
                    for d_ in range(2):
                        tri = triU if d_ == 0 else triL
                        tt("pool", rhsU[d_], bc_mid(tri[:], 8), bc_last(da[:, c, 8 * d_:8 * d_ + 8], 128), ALU.mult,
                           [R("triU"), R("triL"), R("da")], [RrhsU[d_]])
                    for d_ in range(2):
                        tt("dve", xdt[d_][:].rearrange("p (h q) -> p h q", q=64), xs3(c), bc_last(dt[:, c, 8 * d_:8 * d_ + 8], 64), ALU.mult,
                           [R("xs_tm"), R("dt")], [Rxdt[d_]])
                    for d_ in range(2):
                        for h in range(8):
                            bi_ = h // 4
                            mm(banks[bi_][:, (h % 4) * 128:(h % 4) * 128 + 128], ones_f[:], rhsU[d_][:, h, :], True, True,
                               [R("ones_f"), RrhsU[d_]], [RB[bi_]])
                        for q in range(2):
                            bi_ = q
                            for h in range(4 * q, 4 * q + 4):
                                act(E[d_][:, h, :], banks[bi_][:, (h % 4) * 128:(h % 4) * 128 + 128], AF.Exp, [RB[bi_], R("nacs")], [RE[d_]],
                                    bias=nacs[:, c, 8 * d_ + h:8 * d_ + h + 1], scale=1.0)
                            act(Eb[d_][:, 4 * q:4 * q + 4, :].rearrange("p h i -> p (h i)"), banks[bi_][:, :], AF.Exp, [RB[bi_]], [REb[d_]])
                    for d_ in range(2):
                        ts("dve", E[d_], E[d_], 1.0, ALU.min, [RE[d_]], [RE[d_]])
                        for g in range(2):
                            tt("dve", E[d_][:, 4 * g:4 * g + 4, :], E[d_][:, 4 * g:4 * g + 4, :], bc_mid(Gm[d_][:, g, :], 4), ALU.mult,
                               [RE[d_], RGm[d_]], [RE[d_]])
                        tt("pool", Eb[d_], Eb[d_], bc_mid(CT[:, csl], 8), ALU.mult, [REb[d_], R("CT")], [REb[d_]])

            def front_y(n_, c):
                emit_out = need_ctx or c < NLAT
                if emit_out:
                    for d_ in range(2):
                        for h in range(8):
                            g, hl = h // 4, h % 4
                            ysl = slice(64 * h, 64 * h + 64)
                            mm(banks[4][:, ysl], E[d_][:, h, :], xdt[d_][:, ysl], d_ == 0 and h == 0, False, [RE[d_], Rxdt[d_]], [RB[4]], sgc=True)
                            st_ = Sf_bf if d_ == 0 else prevB[:, c, :]
                            rst = R("Sf_bf") if d_ == 0 else R("prevB")
                            mm(banks[4][:, ysl], Eb[d_][64 * g:64 * g + 64, h, :], st_[64 * g:64 * g + 64, 64 * hl:64 * hl + 64], False, d_ == 1,
                               [REb[d_], rst], [RB[4]], sgc=True)

            def fstates(n_, c):
                if c != NLAT - 1:
                    states(c, 0, n_ % 2, Sf, R("Sf"))
                    cp("dve", Sf_bf, Sf, [R("Sf")], [R("Sf_bf")])

            def back1(n_, c):
                emit_out = need_ctx or c < NLAT
                if not emit_out:
                    return
                tt("dve", tmpD[:].rearrange("p (h q) -> p h q", q=64), xs3(c), bc_last(Db, 64), ALU.mult, [R("xs_tm"), R("Db")], [R("tmpD")])
                tt("dve", ytot, banks[4][:, :], tmpD, ALU.add, [RB[4], R("tmpD")], [R("ytot")])

            def back(n_, c):
                emit_out = need_ctx or c < NLAT
                if not emit_out:
                    return
                j = 0 if c < NLAT else 1
                tt("dve", ytot, ytot, zs[:, c, :], ALU.mult, [R("ytot"), R("zs")], [R("ytot")])
                act(sjunk, ytot, AF.Square, [R("ytot")], [R("sjunk"), R("ssq")], accum_out=ssq[:, c:c + 1])
                act(ssq[:, 32 + c:33 + c], ssq[:, c:c + 1], AF.Ln, [R("ssq")], [R("ssq")], bias=1e-6, scale=1.0 / 512)
                act(ssq[:, 32 + c:33 + c], ssq[:, 32 + c:33 + c], AF.Exp, [R("ssq")], [R("ssq")], scale=-0.5)
                ts("dve", yn, ytot, ssq[:, 32 + c:33 + c], ALU.mult, [R("ytot"), R("ssq")], [R("yn")])
                b5 = banks[5].bitcast(BF16)
                for kc in range(4):
                    tr(b5[:, kc * 128:kc * 128 + 128], yn[:, kc * 128:kc * 128 + 128], ident_b[:], [R("yn"), R("ident_b")], [RB[5]])
                for kc in range(4):
                    ts("dve", oT[:, kc, :], b5[:, kc * 128:kc * 128 + 128], normg[:, kc:kc + 1], ALU.mult, [RB[5], R("normg")], [R("oT")])
                for hf in range(2):
                    sl = slice(hf * 512, hf * 512 + 512)
                    for kc in range(4):
                        mm(banks[6 + hf][:, :], oT[:, kc, :], woS[:, kc, sl], kc == 0, kc == 3, [R("oT"), R("woS")], [RB[6 + hf]])
                for hf in range(2):
                    sl = slice(hf * 512, hf * 512 + 512)
                    tt("dve", upd, banks[6 + hf][:, :], GATE[:, j, sl], ALU.mult, [RB[6 + hf], R("GATE")], [rupd])
                    tt("dve", X[:, c, sl], X[:, c, sl], upd, ALU.add, [rupd, RX[c]], [RX[c]])

            front(0, order2[0])
            front_y(0, order2[0])
            fstates(0, order2[0])
            for n_, c in enumerate(order2):
                if n_ + 1 < len(order2):
                    front(n_ + 1, order2[n_ + 1])
                back1(n_, c)
                if n_ + 1 < len(order2):
                    front_y(n_ + 1, order2[n_ + 1])
                back(n_, c)
                if n_ + 1 < len(order2):
                    fstates(n_ + 1, order2[n_ + 1])


        def tree(op, dst, src, width, n1, tmpbuf):
            cur = src
            w = width
            while w > 1:
                h = w // 2
                out = dst.unsqueeze(2) if h == 1 else tmpbuf[:, :, 0:h]
                tt("dve", out, cur[:, :, 0:h], cur[:, :, h:w], op, [R("rt")], [R("rt")])
                cur = out
                w = h

        def moe_phase(l, need_ctx):
            tiles = list(range(NT)) if need_ctx else list(range(NLAT))
            ntl = len(tiles)
            S.barrier()
            rg = region()
            comb = rg.take([128, NT, 32], F32)
            lg = rg.take([128, NT, 36], F32)
            mark = rg.off
            w_rt = rg.take([128, 8, 36], F32)
            brt = rg.take([128, 36], F32)
            load("sp", w_rt, w_rt_d[l].rearrange("(k p) n -> p k n", p=128), [R("w_rt")])
            load("sp", brt, b_rt_d[l:l + 1, :].partition_broadcast(128), [R("brt")])

            def router(t, hf, rhf):
                bi_ = 4 + t // 9
                c0 = (t % 9) * 36
                for k in range(8):
                    mm(banks[bi_][:, c0:c0 + 36], hf[:, k, :], w_rt[:, k, :], k == 0, k == 7, [rhf, R("w_rt")], [RB[bi_]])

            norm_phase(1, tiles, rg=rg, router=router)
            for q in range(2):
                t0, t1 = 9 * q, min(9 * q + 9, ntl)
                if t1 <= t0:
                    continue
                n_ = t1 - t0
                src = banks[4 + q][:, 0:n_ * 36].rearrange("p (t c) -> p t c", c=36)
                tt("dve", lg[:, t0:t1, :], src, bc_mid(brt, n_), ALU.add, [RB[4 + q], R("brt")], [R("rt")])
            S.barrier()
            rg.off = mark
            gl = lg[:, 0:ntl, 0:4]
            el = lg[:, 0:ntl, 4:36]
            t4 = rg.take([128, NT, 4], F32)
            t32 = rg.take([128, NT, 32], F32)
            elm = rg.take([128, NT, 32], F32)
            m1b = rg.take([128, NT, 32], F32)
            m2b = rg.take([128, NT, 32], F32)
            gmax = rg.take([128, NT], F32)
            gw = rg.take([128, NT], F32)
            m1 = rg.take([128, NT], F32)
            m2 = rg.take([128, NT], F32)
            w1 = rg.take([128, NT], F32)
            w2 = rg.take([128, NT], F32)
            RT = [R("rt")]
            n = ntl
            tree(ALU.max, gmax[:, 0:n], gl, 4, n, t4[:, 0:n, :])
            tt("dve", t4[:, 0:n, :], gl, bc_last(gmax[:, 0:n], 4), ALU.subtract, RT, RT)
            act(t4[:, 0:n, :], t4[:, 0:n, :], AF.Exp, RT, RT)
            tree(ALU.add, gw[:, 0:n], t4[:, 0:n, :], 4, n, t32[:, 0:n, 0:4])
            S.op("dve", lambda e: e.reciprocal(out=gw[:, 0:n], in_=gw[:, 0:n]), RT, RT)
            tt("dve", t4[:, 0:n, :], gl, bc_last(gmax[:, 0:n], 4), ALU.is_equal, RT, RT)
            ts("dve", t4[:, 0:n, :], t4[:, 0:n, :], -1.0, ALU.add, RT, RT, s2=-NEG, op1=ALU.mult)
            for g in range(4):
                tt("dve", elm[:, 0:n, 8 * g:8 * g + 8], el[:, :, 8 * g:8 * g + 8], t4[:, 0:n, g:g + 1].to_broadcast([128, n, 8]), ALU.add, RT, RT)
            tree(ALU.max, m1[:, 0:n], elm[:, 0:n, :], 32, n, t32[:, 0:n, :])
            tt("dve", m1b[:, 0:n, :], elm[:, 0:n, :], bc_last(m1[:, 0:n], 32), ALU.is_equal, RT, RT)
            stt(elm[:, 0:n, :], m1b[:, 0:n, :], 2 * NEG, elm[:, 0:n, :], ALU.mult, ALU.add, RT, RT)
            tree(ALU.max, m2[:, 0:n], elm[:, 0:n, :], 32, n, t32[:, 0:n, :])
            tt("dve", m2b[:, 0:n, :], elm[:, 0:n, :], bc_last(m2[:, 0:n], 32), ALU.is_equal, RT, RT)
            tt("dve", w2[:, 0:n], m2[:, 0:n], m1[:, 0:n], ALU.subtract, RT, RT)
            act(w2[:, 0:n], w2[:, 0:n], AF.Exp, RT, RT)
            ts("dve", w1[:, 0:n], w2[:, 0:n], 1.0, ALU.add, RT, RT)
            S.op("dve", lambda e: e.reciprocal(out=w1[:, 0:n], in_=w1[:, 0:n]), RT, RT)
            tt("dve", w1[:, 0:n], w1[:, 0:n], gw[:, 0:n], ALU.mult, RT, RT)
            tt("dve", w2[:, 0:n], w2[:, 0:n], w1[:, 0:n], ALU.mult, RT, RT)
            tt("dve", m1b[:, 0:n, :], m1b[:, 0:n, :], bc_last(w1[:, 0:n], 32), ALU.mult, RT, RT)
            tt("dve", m2b[:, 0:n, :], m2b[:, 0:n, :], bc_last(w2[:, 0:n], 32), ALU.mult, RT, RT)
            tt("dve", comb[:, 0:n, :], m1b[:, 0:n, :], m2b[:, 0:n, :], ALU.add, RT, [R("comb")])
            S.barrier()
            rg.off = mark
            NS = 3
            WGU = [rg.take([128, 8, 512], BF16) for _ in range(NS)]
            WD = [rg.take([128, 2, D], BF16) for _ in range(NS)]
            WDx = [rg.take([128, 2, D], BF16) for _ in range(NS)]
            WDc = [rg.take([128, 2, D], BF16) for _ in range(NS)]
            Rgu = [Res("wgu%d" % i) for i in range(NS)]
            Rwd = [Res("wd%d" % i) for i in range(NS)]
            Rwdx = [Res("wdx%d" % i) for i in range(NS)]
            s_sb = [rg.take([128, 256], F32) for _ in range(2)]
            Rs = [Res("s%d" % i) for i in range(2)]
            hid = [rg.take([128, 256], BF16) for _ in range(2)]
            Rhid = [Res("hid%d" % i) for i in range(2)]
            hidT = [rg.take([128, 2, 128], BF16) for _ in range(2)]
            RhT = [Res("hidT%d" % i) for i in range(2)]

            def load_expert(e):
                sl = e % NS
                S.dma("pool", [lambda en: en.dma_start(out=WGU[sl][:, :, 0:256], in_=wg_d[l, e].rearrange("(k p) n -> p k n", p=128)),
                               lambda en: en.dma_start(out=WGU[sl][:, :, 256:512], in_=wu_d[l, e].rearrange("(k p) n -> p k n", p=128))],
                      "wgu%d" % sl, [], [Rgu[sl]])
                S.dma("pool", [lambda en: en.dma_start(out=WD[sl], in_=wd_d[l, e].rearrange("(k p) n -> p k n", p=128))],
                      "wd%d" % sl, [], [Rwd[sl]])
                for fc in range(2):
                    tt("pool", WDx[sl][:, fc, :], WD[sl][:, fc, :], GATE[:, 2, :], ALU.mult, [Rwd[sl], R("GATE")], [Rwdx[sl]])
                    if need_ctx:
                        tt("pool", WDc[sl][:, fc, :], WD[sl][:, fc, :], GATE[:, 3, :], ALU.mult, [Rwd[sl], R("GATE")], [Rwdx[sl]])

            items = [(e, t) for e in range(32) for t in tiles]
            nit = len(items)
            b2 = banks[2].bitcast(BF16)

            def stageG(i):
                e, t = items[i]
                sl = e % NS
                bk, rb = banks[i % 2], RB[i % 2]
                for k in range(8):
                    mm(bk[:, :], HT[:, k, t * 128:t * 128 + 128], WGU[sl][:, k, :], k == 0, k == 7, [RH[t], Rgu[sl]], [rb])
                act(s_sb[i % 2], bk[:, 0:256], AF.Silu, [rb], [Rs[i % 2]])
                stt(hid[i % 2], bk[:, 256:512], comb[:, t, e:e + 1], s_sb[i % 2], ALU.mult, ALU.mult, [rb, R("comb"), Rs[i % 2]], [Rhid[i % 2]])

            def stageT(i):
                for fc in range(2):
                    tr(b2[:, (i % 2) * 256 + fc * 128:(i % 2) * 256 + fc * 128 + 128], hid[i % 2][:, fc * 128:fc * 128 + 128], ident_b[:],
                       [Rhid[i % 2], R("ident_b")], [RB[2]])
                cp("act_copy", hidT[i % 2][:].rearrange("p a b -> p (a b)"), b2[:, (i % 2) * 256:(i % 2) * 256 + 256], [RB[2]], [RhT[i % 2]])

            def stageD(i):
                e, t = items[i]
                sl = e % NS
                wdd = WDx[sl] if t < NLAT else WDc[sl]
                for fc in range(2):
                    for hf in range(2):
                        bi_ = 4 + 2 * (i % 2) + hf
                        mm(banks[bi_][:, :], hidT[i % 2][:, fc, :], wdd[:, fc, hf * 512:hf * 512 + 512], fc == 0, fc == 1,
                           [RhT[i % 2], Rwdx[sl]], [RB[bi_]])
                for hf in range(2):
                    bi_ = 4 + 2 * (i % 2) + hf
                    sl_ = slice(hf * 512, hf * 512 + 512)
                    tt("dve", X[:, t, sl_], X[:, t, sl_], banks[bi_][:, :], ALU.add, [RX[t], RB[bi_]], [RX[t]])

            for e in range(NS):
                load_expert(e)
            for i in range(nit + 2):
                if i < nit:
                    stageG(i)
                if 0 <= i - 1 < nit:
                    stageT(i - 1)
                if 0 <= i - 2 < nit:
                    stageD(i - 2)
                    e, t = items[i - 2]
                    if t == tiles[-1] and e + NS < 32:
                        load_expert(e + NS)


        def final_phase():
            S.barrier()
            rg = region()
            gfb = rg.take([128, D], F32)
            junk = rg.take([128, D], BF16)
            load("sp", gfb, g_final_d.partition_broadcast(128), [R("gfb")])
            ot = [rg.take([128, D], F32) for _ in range(2)]
            rot = [Res("fo%d" % i) for i in range(2)]
            ov = out_d.rearrange("(t p) d -> p t d", p=128)
            for t in range(NLAT):
                b = t % 2
                if final_norm:
                    act(junk[:], X[:, t, :], AF.Square, [RX[t]], [R("junk"), R("ss%d" % t)], accum_out=small[:, t:t + 1])
                    act(small[:, 32 + t:33 + t], small[:, t:t + 1], AF.Ln, [R("ss%d" % t)], [R("rs%d" % t)], bias=1e-6, scale=1.0 / D)
                    act(small[:, 32 + t:33 + t], small[:, 32 + t:33 + t], AF.Exp, [R("rs%d" % t)], [R("rs%d" % t)], scale=-0.5)
                    stt(ot[b], X[:, t, :], small[:, 32 + t:33 + t], gfb, ALU.mult, ALU.mult, [RX[t], R("rs%d" % t), R("gfb")], [rot[b]])
                else:
                    cp("dve", ot[b], X[:, t, :], [RX[t]], [rot[b]])
                S.dma("sp", [lambda e, b=b, t=t: e.dma_start(out=ov[:, t, :], in_=ot[b])], "outst", [rot[b]], [])
            if dbg_d is not None:
                dv = dbg_d.rearrange("(t p) d -> p t d", p=128)
                for t in range(2):
                    S.dma("sp", [lambda e, t=t: e.dma_start(out=dv[:, t, :], in_=X[:, NLAT + t, :])], "outst", [RX[NLAT + t]], [])
            S.streams["sp"].append(("wait", ("dma", "outst"), S.dmacnt[("dma", "outst")]))

        for l in range(nlayers):
            need_ctx = l < DEPTH - 1
            mod_phase(l)
            S.barrier()
            norm_phase(0, list(range(NT)))
            if "A" in phases:
                attn_phase("A", l, need_ctx)
            if "N" in phases:
                attn_phase("N", l, need_ctx)
            if "S" in phases:
                ssd_phase(l, need_ctx)
            if "M" in phases:
                moe_phase(l, need_ctx)
        final_phase()
        S.emit()
    return nc


def _prep_shared(inp):
    f = np.float32
    sh = {}
    sh["w_mod"] = np.ascontiguousarray(inp["w_mod"], f)
    sh["b_mod"] = np.ascontiguousarray(inp["b_mod"], f)
    sh["b_modT"] = np.ascontiguousarray(inp["b_mod"].reshape(DEPTH, 48, 128).transpose(0, 2, 1), f)
    sh["g_mixT"] = np.ascontiguousarray(inp["g_mix"].reshape(DEPTH, 8, 128).transpose(0, 2, 1), f)
    sh["g_ffnT"] = np.ascontiguousarray(inp["g_ffn"].reshape(DEPTH, 8, 128).transpose(0, 2, 1), f)
    sh["g_final"] = np.ascontiguousarray(inp["g_final"].reshape(1, D), f)
    w_in = np.asarray(inp["w_in"], f)
    sw = _swap_idx()
    qcol = lambda h: np.arange(64 * h, 64 * h + 64)
    kcol = lambda g: 256 + np.arange(64 * g, 64 * g + 64)
    Q02 = np.concatenate([qcol(0), qcol(2)])
    Q13 = np.concatenate([qcol(1), qcol(3)])
    K01 = np.concatenate([kcol(0), kcol(1)])
    Q02s = np.concatenate([qcol(0)[sw], qcol(2)[sw]])
    Q13s = np.concatenate([qcol(1)[sw], qcol(3)[sw]])
    K01s = np.concatenate([kcol(0)[sw], kcol(1)[sw]])
    Vc = 384 + np.arange(128)
    colsA = np.concatenate([Q02, Q13, K01, Q02s, Q13s, K01s, Vc])
    sh["w_inA"] = np.ascontiguousarray(w_in[:, :, colsA])
    sh["w_in"] = np.ascontiguousarray(w_in)
    rows = np.concatenate([np.arange(0, 64), np.arange(128, 192), np.arange(64, 128), np.arange(192, 256), np.arange(256, 1024)])
    sh["w_outP"] = np.ascontiguousarray(np.asarray(inp["w_out"], f)[:, rows, :])
    sk = np.asarray(inp["attn_sink"], f)
    sinkE = np.zeros((DEPTH, 128, 2), f)
    sinkE[:, 0:64, 0] = sk[:, 0:1]; sinkE[:, 64:128, 0] = sk[:, 2:3]
    sinkE[:, 0:64, 1] = sk[:, 1:2]; sinkE[:, 64:128, 1] = sk[:, 3:4]
    sh["sinkE"] = sinkE
    cw = np.asarray(inp["ssd_conv_w"], f)
    sh["convw"] = np.ascontiguousarray(cw.reshape(DEPTH, 5, 6, 128).transpose(0, 3, 2, 1).reshape(DEPTH, 128, 30))
    sh["convb"] = np.ascontiguousarray(np.asarray(inp["ssd_conv_b"], f).reshape(DEPTH, 6, 128).transpose(0, 2, 1))
    sh["dtb"] = np.ascontiguousarray(np.asarray(inp["ssd_dt_bias"], f).reshape(DEPTH, 16))
    sh["alog"] = np.ascontiguousarray(np.asarray(inp["ssd_a_log"], f).reshape(DEPTH, 16))
    sh["ssdd"] = np.ascontiguousarray(np.asarray(inp["ssd_d"], f))
    sh["normgT"] = np.ascontiguousarray(np.asarray(inp["ssd_norm_g"], f).reshape(DEPTH, 4, 128).transpose(0, 2, 1))
    rpb = np.asarray(inp["na_rpb"], f)
    sh["naTab"] = np.stack([_na_table(rpb[l]).reshape(128, 6144) for l in range(DEPTH)], 0)
    sh["w_rt"] = np.ascontiguousarray(np.concatenate([np.asarray(inp["w_router_group"], f), np.asarray(inp["w_router_expert"], f)], -1))
    sh["b_rt"] = np.ascontiguousarray(np.concatenate([np.asarray(inp["b_router_group"], f), np.asarray(inp["b_router_expert"], f)], -1))
    sh["w_exp_gate"] = np.ascontiguousarray(np.asarray(inp["w_exp_gate"], f).reshape(DEPTH, 32, D, 256))
    sh["w_exp_up"] = np.ascontiguousarray(np.asarray(inp["w_exp_up"], f).reshape(DEPTH, 32, D, 256))
    sh["w_exp_down"] = np.ascontiguousarray(np.asarray(inp["w_exp_down"], f).reshape(DEPTH, 32, 256, D))
    ct = _const_tables()
    ct["maskA"] = ct["maskA"].reshape(128, 384)
    sh.update(ct)
    return sh


def _run(inp, cfg, cores=None):
    sh = _prep_shared(inp)
    x = np.asarray(inp["x"], np.float32)
    c = np.asarray(inp["c"], np.float32)
    ctx = np.asarray(inp["ctx"], np.float32)
    c_ctx = np.asarray(inp["c_ctx"], np.float32)
    cores = list(range(8)) if cores is None else cores
    in_maps = []
    for b in cores:
        m = dict(sh)
        m["x"] = np.ascontiguousarray(x[b])
        m["ctx"] = np.ascontiguousarray(ctx[b])
        scT = np.zeros((128, 8, 2), np.float32)
        scT[:, :, 0] = c[b].reshape(8, 128).T
        scT[:, :, 1] = c_ctx.reshape(8, 128).T
        m["scT"] = scT.reshape(128, 16)
        in_maps.append(m)
    nc = build_program(cfg)
    res = run_bass_kernel_spmd(nc, in_maps, core_ids=list(range(len(cores))))
    return res


def kernel(**inputs):
    res = _run(inputs, {})
    return np.stack([r["out"] for r in res.results], 0).astype(np.float32)
```

```python
import contextlib
import math
import numpy as np
import concourse.bass as bass
import concourse.mybir as mybir
from concourse.bass_utils import run_bass_kernel_spmd

F32 = mybir.dt.float32
BF16 = mybir.dt.bfloat16
AF = mybir.ActivationFunctionType
ALU = mybir.AluOpType

EPOCH = 24000
NEG = -30000.0
D = 1024
NLAT = 16
NT = 18
TOK = NT * 128
DEPTH = 4
REGN = 37632


class Res:
    __slots__ = ("name", "w", "r", "psum")

    def __init__(self, name="", psum=False):
        self.name = name
        self.w = None
        self.r = []
        self.psum = psum


class Sched:
    ENGS = ("pe", "act", "dve", "pool", "sp")

    def __init__(self, nc):
        self.nc = nc
        self.streams = {e: [] for e in self.ENGS}
        self.cnt = {e: 0 for e in self.ENGS}
        self.waited = {e: {} for e in self.ENGS}
        self.semkeys = []
        self.semset = set()
        self.dmacnt = {}
        self.last = {}

    def _need(self, key):
        if key not in self.semset:
            self.semset.add(key)
            self.semkeys.append(key)

    def _wait(self, eng, k, v):
        if k[0] == eng and eng == "pe":
            return
        wd = self.waited[eng]
        if wd.get(k, 0) < v:
            wd[k] = v
            self.streams[eng].append(("wait", k, v))

    def _deps(self, eng, reads, writes):
        for r in reads:
            if r.w is not None:
                self._wait(eng, *r.w)
        for w in writes:
            if w.w is not None:
                self._wait(eng, *w.w)
            for t in w.r:
                self._wait(eng, *t)

    def _post(self, tok, reads, writes):
        self.last[tok[0]] = tok[1]
        for r in reads:
            r.r.append(tok)
            if len(r.r) > 64:
                best = {}
                for (k, v) in r.r:
                    if best.get(k, 0) < v:
                        best[k] = v
                r.r = list(best.items())
        for w in writes:
            w.w = tok
            w.r = []

    def op(self, eng, fn, reads=(), writes=()):
        if eng != "pe":
            ex = [r for r in reads if r.psum]
            if ex:
                writes = list(writes) + ex
        self._deps(eng, reads, writes)
        c = self.cnt[eng]
        key = (eng, c // EPOCH)
        val = c % EPOCH + 1
        self._need(key)
        self.cnt[eng] = c + 1
        self.streams[eng].append(("op", fn, key, 1))
        tok = (key, val)
        self._post(tok, reads, writes)
        return tok

    def dma(self, eng, fns, semname, reads=(), writes=()):
        self._deps(eng, reads, writes)
        key = ("dma", semname)
        self._need(key)
        c = self.dmacnt.get(key, 0)
        for fn in fns:
            self.streams[eng].append(("op", fn, key, 16))
            c += 16
        self.dmacnt[key] = c
        tok = (key, c)
        self._post(tok, reads, writes)
        return tok

    def barrier(self):
        toks = list(self.last.items())
        for eng in self.ENGS:
            for (k, v) in toks:
                self._wait(eng, k, v)

    def emit(self):
        nc = self.nc
        with contextlib.ExitStack() as es:
            sems = {}
            for i, k in enumerate(self.semkeys):
                sems[k] = es.enter_context(nc.semaphore("s%d" % i))
            block = es.enter_context(nc.Block())

            def runner(stream):
                def f(e):
                    for it in stream:
                        if it[0] == "wait":
                            e.wait_ge(sems[it[1]], it[2])
                        else:
                            it[1](e).then_inc(sems[it[2]], it[3])
                return f

            block.tensor(runner(self.streams["pe"]))
            block.scalar(runner(self.streams["act"]))
            block.vector(runner(self.streams["dve"]))
            block.gpsimd(runner(self.streams["pool"]))
            block.sync(runner(self.streams["sp"]))


def _rope_tables():
    t = np.arange(2048)
    rows = (t // 64).astype(np.float64)
    cols = (t % 64).astype(np.float64)
    inv = 1.0 / (10000.0 ** (np.arange(0, 32, 2, dtype=np.float64) / 32.0))
    C = np.zeros((64, 2048), np.float64)
    S = np.zeros((64, 2048), np.float64)
    ar = rows[None, :] * inv[:, None]
    ac = cols[None, :] * inv[:, None]
    C[0:16] = np.cos(ar); C[16:32] = np.cos(ar); C[32:48] = np.cos(ac); C[48:64] = np.cos(ac)
    S[0:16] = -np.sin(ar); S[16:32] = np.sin(ar); S[32:48] = -np.sin(ac); S[48:64] = np.sin(ac)
    C = np.concatenate([C, C], 0).astype(np.float32)
    S = np.concatenate([S, S], 0).astype(np.float32)
    return C, S


def _swap_idx():
    i = np.arange(64)
    return np.where(i < 16, i + 16, np.where(i < 32, i - 16, np.where(i < 48, i + 16, i - 16)))


def _const_tables():
    k = np.arange(128)[:, None]
    i = np.arange(128)[None, :]
    c = {}
    c["ident"] = np.eye(128, dtype=np.float32)
    c["triU"] = (k <= i).astype(np.float32)
    c["triL"] = (k >= i).astype(np.float32)
    c["maskF"] = np.where(k <= i, 0.0, NEG).astype(np.float32)
    c["maskB"] = np.where(k >= i, 0.0, NEG).astype(np.float32)
    mA = np.zeros((128, 3, 128), np.float32)
    mA[:, 0, :] = np.where(i <= k, 0.0, NEG)
    mA[:, 2, :] = np.where(k <= i, 0.0, NEG)
    c["maskA"] = mA
    C, S = _rope_tables()
    c["ropeC"] = C
    c["ropeS"] = S
    return c


def _na_table(rpb):
    kr = (np.arange(128) // 64)[:, None]
    kc = (np.arange(128) % 64)[:, None]
    qr = (np.arange(128) // 64)[None, :]
    qc = (np.arange(128) % 64)[None, :]
    cs = np.clip(qc - 8, 0, 48)
    colv = (kc >= cs) & (kc < cs + 16)
    coff = np.clip(kc - qc, -15, 15) + 15
    out = np.full((128, 4, 12, 128), NEG, np.float32)
    for v in range(12):
        if v < 5:
            dj = v - 2
            dr = 2 * dj + kr - qr
            rowv = (dr >= -4) & (dr <= 3)
        else:
            dj = v - 5 - 3
            dr = 2 * dj + kr - qr
            rowv = np.abs(dr) <= 7
        valid = rowv & colv
        drc = np.clip(dr + 7, 0, 14)
        for h in range(4):
            g = rpb[h][drc, coff]
            out[:, h, v, :] = np.where(valid, g, np.float32(NEG))
    return out


def _na_keys(t):
    if t < 2:
        return [(j, 5 + (j - t) + 3) for j in range(0, 4)]
    if t >= 14:
        return [(j, 5 + (j - t) + 3) for j in range(12, 16)]
    return [(j, (j - t) + 2) for j in range(t - 2, t + 3)]


def build_program(cfg):
    nlayers = cfg.get("nlayers", DEPTH)
    phases = cfg.get("phases", "ANSM")
    final_norm = cfg.get("final_norm", True)

    nc = bass.Bass("TRN2", target_bir_lowering=False)

    def din(name, shape):
        return nc.dram_tensor(name, list(shape), F32, kind="ExternalInput").ap()

    x_d = din("x", [2048, D])
    ctx_d = din("ctx", [256, D])
    scT_d = din("scT", [128, 16])
    w_mod_d = din("w_mod", [DEPTH, D, 6 * D])
    b_modT_d = din("b_modT", [DEPTH, 128, 48])
    b_mod_d = din("b_mod", [DEPTH, 6 * D])
    g_mixT_d = din("g_mixT", [DEPTH, 128, 8])
    g_ffnT_d = din("g_ffnT", [DEPTH, 128, 8])
    g_final_d = din("g_final", [1, D])
    w_inA_d = din("w_inA", [DEPTH, D, 896])
    w_in_d = din("w_in", [DEPTH, D, 2576])
    w_outP_d = din("w_outP", [DEPTH, D, D])
    sinkE_d = din("sinkE", [DEPTH, 128, 2])
    convw_d = din("convw", [DEPTH, 128, 30])
    convb_d = din("convb", [DEPTH, 128, 6])
    dtb_d = din("dtb", [DEPTH, 16])
    alog_d = din("alog", [DEPTH, 16])
    ssdd_d = din("ssdd", [DEPTH, 8])
    normgT_d = din("normgT", [DEPTH, 128, 4])
    naTab_d = din("naTab", [DEPTH, 128, 6144])
    w_rt_d = din("w_rt", [DEPTH, D, 36])
    b_rt_d = din("b_rt", [DEPTH, 36])
    wg_d = din("w_exp_gate", [DEPTH, 32, D, 256])
    wu_d = din("w_exp_up", [DEPTH, 32, D, 256])
    wd_d = din("w_exp_down", [DEPTH, 32, 256, D])
    ident_d = din("ident", [128, 128])
    triU_d = din("triU", [128, 128])
    triL_d = din("triL", [128, 128])
    maskF_d = din("maskF", [128, 128])
    maskB_d = din("maskB", [128, 128])
    maskA_d = din("maskA", [128, 384])
    ropeC_d = din("ropeC", [128, 2048])
    ropeS_d = din("ropeS", [128, 2048])
    out_d = nc.dram_tensor("out", [2048, D], F32, kind="ExternalOutput").ap()
    dbg_d = None
    if cfg.get("dump_ctx"):
        dbg_d = nc.dram_tensor("out_ctx", [256, D], F32, kind="ExternalOutput").ap()

    es = contextlib.ExitStack()
    with es:
        def sb(name, shape, dt):
            return es.enter_context(nc.sbuf_tensor("sb_" + name, list(shape), dt))

        S = Sched(nc)

        X = sb("X", [128, NT, D], F32)
        HT = sb("HT", [128, 8, TOK], BF16)
        REG = sb("REG", [128, REGN], BF16)
        GATE = sb("GATE", [128, 4, D], F32)
        ident_f = sb("ident_f", [128, 128], F32)
        ident_b = sb("ident_b", [128, 128], BF16)
        triU = sb("triU", [128, 128], F32)
        triL = sb("triL", [128, 128], F32)
        maskF = sb("maskF", [128, 128], F32)
        maskB = sb("maskB", [128, 128], F32)
        maskA = sb("maskA", [128, 3, 128], BF16)
        ones_f = sb("ones_f", [128, 128], F32)
        ones_b = sb("ones_b", [128, 128], BF16)
        scT = sb("scT", [128, 16], F32)
        sc_b = sb("sc_b", [128, 8, 2], BF16)
        sc_rep = sb("sc_rep", [128, 2, 8, 128], BF16)
        modT = sb("modT", [128, 48, 2], F32)
        bmT = sb("bmT", [128, 48], F32)
        gT = sb("gT", [128, 16], F32)
        AV = sb("AV", [128, 2, 2, 8], F32)
        SV = sb("SV", [128, 2, 2, 8], F32)
        small = sb("small", [128, 64], F32)

        banks = [es.enter_context(nc.psum_tensor("bank%d" % i, [128, 512], F32)) for i in range(8)]
        RB = [Res("bank%d" % i, psum=True) for i in range(8)]

        RX = [Res("X%d" % t) for t in range(NT)]
        RH = [Res("H%d" % t) for t in range(NT)]
        Rc = {}

        def R(name):
            if name not in Rc:
                Rc[name] = Res(name)
            return Rc[name]

        class Carver:
            def __init__(self, base, nbytes):
                self.base = base
                self.off = 0
                self.nbytes = nbytes

            def take(self, shape, dt):
                esz = 2 if dt == BF16 else 4
                n = int(np.prod(shape[1:]))
                nb = n * esz
                nb_al = (nb + 63) // 64 * 64
                assert self.off + nb_al <= self.nbytes, ("region overflow", self.off, nb_al, self.nbytes)
                a = self.base[:, self.off // 2:(self.off + nb) // 2]
                self.off += nb_al
                if dt != BF16:
                    a = a.bitcast(dt)
                if len(shape) > 2:
                    names = " ".join("d%d" % i for i in range(1, len(shape)))
                    kw = {"d%d" % i: shape[i] for i in range(1, len(shape))}
                    a = a.rearrange("p (%s) -> p %s" % (names, names), **kw)
                return a

        def region():
            return Carver(REG, REGN * 2)

        def ht_region():
            return Carver(HT[:].rearrange("p k t -> p (k t)"), 8 * TOK * 2)

        def mm(out, lhsT, rhs, start, stop, reads, writes, sgc=False):
            if sgc:
                S.op("pe", lambda e: e.matmul(out, lhsT=lhsT, rhs=rhs, start=start, stop=stop, skip_group_check=True), reads, writes)
            else:
                S.op("pe", lambda e: e.matmul(out, lhsT=lhsT, rhs=rhs, start=start, stop=stop), reads, writes)

        def tr(out, in_, ident, reads, writes):
            S.op("pe", lambda e: e.transpose(out=out, in_=in_, identity=ident), reads, writes)

        def act(out, in_, func, reads, writes, bias=None, scale=None, accum_out=None):
            kw = {}
            if bias is not None:
                kw["bias"] = bias
            if scale is not None:
                kw["scale"] = scale
            if accum_out is not None:
                kw["accum_out"] = accum_out
            S.op("act", lambda e: e.activation(out=out, in_=in_, func=func, **kw), reads, writes)

        def tt(eng, out, in0, in1, op, reads, writes):
            S.op(eng, lambda e: e.tensor_tensor(out=out, in0=in0, in1=in1, op=op), reads, writes)

        def ts(eng, out, in0, s1, op0, reads, writes, s2=None, op1=None):
            if op1 is None:
                S.op(eng, lambda e: e.tensor_scalar(out=out, in0=in0, scalar1=s1, scalar2=None, op0=op0), reads, writes)
            else:
                S.op(eng, lambda e: e.tensor_scalar(out=out, in0=in0, scalar1=s1, scalar2=s2, op0=op0, op1=op1), reads, writes)

        def stt(out, in0, scalar, in1, op0, op1, reads, writes):
            S.op("dve", lambda e: e.scalar_tensor_tensor(out=out, in0=in0, scalar=scalar, in1=in1, op0=op0, op1=op1), reads, writes)

        def cp(eng, out, in_, reads, writes):
            if eng == "act_copy":
                S.op("act", lambda e: e.activation(out=out, in_=in_, func=AF.Copy), reads, writes)
            else:
                S.op(eng, lambda e: e.tensor_copy(out=out, in_=in_), reads, writes)

        def memset(eng, ap, val, writes):
            S.op(eng, lambda e: e.memset(ap, val), (), writes)

        dma_ctr = [0]

        def load(q, out, in_, writes, reads=(), sem=None):
            if sem is None:
                sem = "u%d" % (dma_ctr[0] % 40)
                dma_ctr[0] += 1
            S.dma(q, [lambda e: e.dma_start(out=out, in_=in_)], sem, reads, writes)

        def wview(w2d, ncols_lo, ncols_hi):
            return w2d[:, ncols_lo:ncols_hi].rearrange("(k p) n -> p k n", p=128)

        xv = x_d.rearrange("(t p) d -> p t d", p=128)
        cv = ctx_d.rearrange("(t p) d -> p t d", p=128)
        for t in range(NLAT):
            load("sp", X[:, t, :], xv[:, t, :], [RX[t]])
        for t in range(2):
            load("sp", X[:, NLAT + t, :], cv[:, t, :], [RX[NLAT + t]])
        load("sp", ident_f[:], ident_d, [R("ident_f")])
        load("pool", ident_b[:], ident_d, [R("ident_b")])
        load("sp", triU[:], triU_d, [R("triU")])
        load("sp", triL[:], triL_d, [R("triL")])
        load("sp", maskF[:], maskF_d, [R("maskF")])
        load("sp", maskB[:], maskB_d, [R("maskB")])
        load("pool", maskA[:].rearrange("p a b -> p (a b)"), maskA_d, [R("maskA")])
        load("sp", scT[:], scT_d, [R("scT")])
        memset("dve", ones_f[:], 1.0, [R("ones_f")])
        memset("dve", ones_b[:], 1.0, [R("ones_b")])
        act(scT[:], scT[:], AF.Silu, [R("scT")], [R("scT")])
        cp("dve", sc_b[:].rearrange("p k j -> p (k j)"), scT[:], [R("scT")], [R("sc_b")])
        for j in range(2):
            for k in range(8):
                ts("dve", sc_rep[:, j, k, :], ones_f[:], scT[:, 2 * k + j:2 * k + j + 1], ALU.mult,
                   [R("scT"), R("ones_f")], [R("sc_rep")])

        def mod_phase(l):
            S.barrier()
            rg = region()
            wb = [rg.take([128, 8, 512], BF16) for _ in range(3)]
            Rw = [Res("modw%d" % i) for i in range(3)]
            brow = rg.take([128, 4, 512], F32)
            load("sp", bmT[:], b_modT_d[l], [R("bmT")])
            load("sp", gT[:, 0:8], g_mixT_d[l], [R("gT")])
            load("sp", gT[:, 8:16], g_ffnT_d[l], [R("gT")])
            gate_chunks = {4: (0, 0), 5: (0, 1), 10: (1, 0), 11: (1, 1)}
            gi = 0
            for ch in range(12):
                i = ch % 3
                load("pool", wb[i], wview(w_mod_d[l], ch * 512, ch * 512 + 512), [Rw[i]], sem="modw%d" % i)
                if ch in gate_chunks:
                    g, half = gate_chunks[ch]
                    load("sp", brow[:, gi, :], b_mod_d[l:l + 1, ch * 512:ch * 512 + 512].partition_broadcast(128),
                         [R("brow")])
                    for j in range(2):
                        bk = banks[j]
                        for k in range(8):
                            mm(bk[:, :], sc_rep[:, j, k, :], wb[i][:, k, :], k == 0, k == 7,
                               [R("sc_rep"), Rw[i]], [RB[j]])
                        tt("dve", GATE[:, 2 * g + j, half * 512:half * 512 + 512], bk[:, :], brow[:, gi, :], ALU.add,
                           [RB[j], R("brow")], [R("GATE")])
                    gi += 1
                else:
                    for sub in range(4):
                        jn = ch * 4 + sub
                        for k in range(8):
                            mm(banks[2][:, 2 * jn:2 * jn + 2], wb[i][:, k, sub * 128:sub * 128 + 128], sc_b[:, k, :],
                               k == 0, k == 7, [R("sc_b"), Rw[i]], [RB[2]])
            mp = banks[2][:, 0:96].rearrange("p (j c) -> p j c", c=2)
            for j in range(2):
                tt("dve", modT[:, :, j], mp[:, :, j], bmT[:], ALU.add, [RB[2], R("bmT")], [R("modT")])
            for n in range(2):
                base = 0 if n == 0 else 24
                for j in range(2):
                    stt(AV[:, n, j, :], modT[:, base + 8:base + 16, j], 1.0, gT[:, 8 * n:8 * n + 8], ALU.add, ALU.mult,
                        [R("modT"), R("gT")], [R("AV")])
                    cp("dve", SV[:, n, j, :], modT[:, base:base + 8, j], [R("modT")], [R("SV")])

        def norm_phase(n, tiles, router=None, rg=None):
            if rg is None:
                rg = region()
            junk = rg.take([128, D], BF16)
            XN = rg.take([128, 2, D], F32)
            HF = rg.take([128, 2, 8, 128], F32)
            def stats(t):
                act(junk[:], X[:, t, :], AF.Square, [RX[t]], [R("junk"), R("ss%d" % t)], accum_out=small[:, t:t + 1])
                act(small[:, 32 + t:33 + t], small[:, t:t + 1], AF.Ln, [R("ss%d" % t)], [R("rs%d" % t)], bias=1e-6, scale=1.0 / D)
                act(small[:, 32 + t:33 + t], small[:, 32 + t:33 + t], AF.Exp, [R("rs%d" % t)], [R("rs%d" % t)], scale=-0.5)

            stats(tiles[0])
            if len(tiles) > 1:
                stats(tiles[1])
            for idx, t in enumerate(tiles):
                if idx + 2 < len(tiles):
                    stats(tiles[idx + 2])
                j = 0 if t < NLAT else 1
                b = idx % 2
                rxn, rhf = R("XN%d" % b), R("HF%d" % b)
                ts("dve", XN[:, b, :], X[:, t, :], small[:, 32 + t:33 + t], ALU.mult, [RX[t], R("rs%d" % t)], [rxn])
                pb = [banks[2 * b], banks[2 * b + 1]]
                for k in range(8):
                    bk = pb[k // 4]
                    mm(bk[:, (k % 4) * 128:(k % 4) * 128 + 128], XN[:, b, k * 128:k * 128 + 128], ident_f[:], True, True,
                       [rxn, R("ident_f")], [RB[2 * b + k // 4]])
                for k in range(8):
                    bk = pb[k // 4]
                    if k % 4 < 2:
                        act(HF[:, b, k, :], bk[:, (k % 4) * 128:(k % 4) * 128 + 128], AF.Identity,
                            [RB[2 * b + k // 4], R("AV"), R("SV")], [rhf],
                            bias=SV[:, n, j, k:k + 1], scale=AV[:, n, j, k:k + 1])
                    else:
                        ts("dve", HF[:, b, k, :], bk[:, (k % 4) * 128:(k % 4) * 128 + 128], AV[:, n, j, k:k + 1], ALU.mult,
                           [RB[2 * b + k // 4], R("AV"), R("SV")], [rhf], s2=SV[:, n, j, k:k + 1], op1=ALU.add)
                cp("pool", HT[:, :, t * 128:t * 128 + 128], HF[:, b, :, :], [rhf], [RH[t]])
                if router is not None:
                    router(t, HF[:, b, :, :], rhf)

        def x_update(t, ps_lo, ps_hi, rb_lo, rb_hi, g, tmp, rtmp):
            for half, (ps, rb) in enumerate(((ps_lo, rb_lo), (ps_hi, rb_hi))):
                sl = slice(half * 512, half * 512 + 512)
                tt("dve", tmp[:, sl], ps, GATE[:, g, sl], ALU.mult, [rb, R("GATE")], [rtmp])
                tt("dve", X[:, t, sl], X[:, t, sl], tmp[:, sl], ALU.add, [rtmp, RX[t]], [RX[t]])

        def proj_fm(dst, rdst, wt, rw, col0, tiles_blocks, evac):
            for bi, (t0, ntile) in enumerate(tiles_blocks):
                bk = banks[bi % 2]
                n = ntile * 128
                for k in range(8):
                    mm(bk[:, 0:n], wt[:, k, col0:col0 + 128], HT[:, k, t0 * 128:t0 * 128 + n], k == 0, k == 7,
                       [rw] + RH[t0:t0 + ntile], [RB[bi % 2]])
                evac(bk[:, 0:n], RB[bi % 2], t0, n)

        BLOCKS = [(0, 4), (4, 4), (8, 4), (12, 4), (16, 2)]

        def attn_phase(kind, l, need_ctx):
            S.barrier()
            rg = region()
            if kind == "A":
                ncolw = 896
                wt = rg.take([128, 8, ncolw], BF16)
                load("pool", wt, wview(w_inA_d[l], 0, 896), [R("wt")], sem="wt")
                ropeC = rg.take([128, 2048], F32)
                ropeS = rg.take([128, 2048], F32)
                load("sp", ropeC, ropeC_d, [R("ropeC")])
                load("sp", ropeS, ropeS_d, [R("ropeS")])
                nq = 2
                Qs = [rg.take([128, TOK], BF16) for _ in range(2)]
                Ks = [rg.take([128, TOK], BF16)]
                Ks = [Ks[0], Ks[0]]
                vcols = 128
                Vtm = rg.take([128, NT, vcols], BF16)
                t1s = [rg.take([128, 512], F32) for _ in range(2)]
                t2s = [rg.take([128, 512], F32) for _ in range(2)]
                ropectr = [0]
                wo = rg.take([128, 2, D], BF16)
                load("pool", wo, w_outP_d[l][0:256, :].rearrange("(k p) n -> p k n", p=128), [R("wo")], sem="wo")
                esink = rg.take([128, 2], F32)
                load("sp", esink, sinkE_d[l], [R("esink")])
                act(esink, esink, AF.Exp, [R("esink")], [R("esink")])
                scale = 0.125
                nkmax = 5
                for ci, (dst, rn) in enumerate(((Qs[0], "Q0"), (Qs[1], "Q1"), (Ks[0], "K0"))):
                    for bi, (t0, ntile) in enumerate(BLOCKS):
                        n = ntile * 128
                        b0, b1 = banks[2 * (bi % 2)], banks[2 * (bi % 2) + 1]
                        r0, r1 = RB[2 * (bi % 2)], RB[2 * (bi % 2) + 1]
                        for k in range(8):
                            mm(b0[:, 0:n], wt[:, k, ci * 128:ci * 128 + 128], HT[:, k, t0 * 128:t0 * 128 + n], k == 0, k == 7,
                               [R("wt")] + RH[t0:t0 + ntile], [r0])
                        if t0 < NLAT:
                            for k in range(8):
                                mm(b1[:, 0:n], wt[:, k, (ci + 3) * 128:(ci + 3) * 128 + 128], HT[:, k, t0 * 128:t0 * 128 + n],
                                   k == 0, k == 7, [R("wt")] + RH[t0:t0 + ntile], [r1])
                            rb_ = ropectr[0] % 2
                            ropectr[0] += 1
                            t1, t2 = t1s[rb_], t2s[rb_]
                            tt("dve", t1[:, 0:n], b0[:, 0:n], ropeC[:, t0 * 128:t0 * 128 + n], ALU.mult, [r0, R("ropeC")], [R("t1_%d" % rb_)])
                            tt("dve", t2[:, 0:n], b1[:, 0:n], ropeS[:, t0 * 128:t0 * 128 + n], ALU.mult, [r1, R("ropeS")], [R("t2_%d" % rb_)])
                            tt("pool", dst[:, t0 * 128:t0 * 128 + n], t1[:, 0:n], t2[:, 0:n], ALU.add, [R("t1_%d" % rb_), R("t2_%d" % rb_)], [R(rn)])
                        else:
                            act(dst[:, t0 * 128:t0 * 128 + n], b0[:, 0:n], AF.Copy, [r0], [R(rn)])
                Rc["K1"] = Rc["K0"]
                vcol0 = 768
            else:
                wt = rg.take([128, 8, 768], BF16)
                load("pool", wt, wview(w_in_d[l], 1808, 2576), [R("wt")], sem="wt")
                tab = rg.take([128, 4, 12, 128], BF16)
                load("pool", tab[:].rearrange("p a b c -> p (a b c)"), naTab_d[l], [R("biasT")], sem="tab")
                Qs = [rg.take([128, TOK], BF16) for _ in range(2)]
                Ks = [rg.take([128, TOK], BF16) for _ in range(2)]
                vcols = 256
                Vtm = rg.take([128, NT, vcols], BF16)
                wo = rg.take([128, 2, D], BF16)
                load("pool", wo, w_outP_d[l][768:1024, :].rearrange("(k p) n -> p k n", p=128), [R("wo")], sem="wo")
                scale = 1.0
                nkmax = 7
                for ci, (dst, rn, sc_) in enumerate(((Qs[0], "Q0", 0.125), (Qs[1], "Q1", 0.125), (Ks[0], "K0", 1.0), (Ks[1], "K1", 1.0))):
                    for bi, (t0, ntile) in enumerate(BLOCKS):
                        n = ntile * 128
                        b0, r0 = banks[bi % 4], RB[bi % 4]
                        for k in range(8):
                            mm(b0[:, 0:n], wt[:, k, ci * 128:ci * 128 + 128], HT[:, k, t0 * 128:t0 * 128 + n], k == 0, k == 7,
                               [R("wt")] + RH[t0:t0 + ntile], [r0])
                        ts("dve", dst[:, t0 * 128:t0 * 128 + n], b0[:, 0:n], sc_, ALU.mult, [r0], [R(rn)])
                vcol0 = 512
            for t in range(NT):
                bk, rb = banks[4 + t % 4], RB[4 + t % 4]
                for k in range(8):
                    mm(bk[:, 0:vcols], HT[:, k, t * 128:t * 128 + 128], wt[:, k, vcol0:vcol0 + vcols], k == 0, k == 7,
                       [R("wt"), RH[t]], [rb])
                cp("dve", Vtm[:, t, :], bk[:, 0:vcols], [rb], [R("V")])

            PT = [rg.take([128, nkmax * 128], BF16) for _ in range(2)]
            RPT = [Res("PT%d" % i) for i in range(2)]
            OT = [rg.take([128, 128], BF16) for _ in range(4)]
            ROT = [Res("OT%d" % i) for i in range(4)]
            RDn = [rg.take([128, 128], F32) for _ in range(2)]
            RRD = [Res("RD%d" % i) for i in range(2)]
            tmp = rg.take([128, D], F32)
            rtmp = Res("updtmp")

            def keys_of(t):
                if t >= NLAT:
                    return [(16, None), (17, None)]
                if kind == "A":
                    ks = [(j, j - t + 1) for j in (t - 1, t, t + 1) if 0 <= j < NLAT]
                    ks = [(j, (b if b != 1 else None)) for (j, b) in ks]
                else:
                    ks = _na_keys(t)
                return ks + [(16, None), (17, None)]

            def bias_ap(pr, half, bid):
                if kind == "A":
                    return maskA[:, bid, :]
                return tab[:, 2 * pr + half, bid, :]

            qtiles = list(range(NLAT)) + ([16, 17] if need_ctx else [])
            work = [(t, pr, half) for t in qtiles for pr in range(2) for half in range(2)]

            def scores(i):
                t, pr, half = work[i]
                keys = keys_of(t)
                ps_ = slice(64 * half, 64 * half + 64)
                for kk, (j, bid) in enumerate(keys):
                    col = kk * 128
                    bi_ = 2 * (i % 2) + col // 512
                    c0 = col % 512
                    bk, rb = banks[bi_], RB[bi_]
                    has_b = bid is not None
                    mm(bk[:, c0:c0 + 128], Ks[pr][ps_, j * 128:j * 128 + 128], Qs[pr][ps_, t * 128:t * 128 + 128],
                       True, not has_b, [R("K%d" % pr), R("Q%d" % pr)], [rb])
                    if has_b:
                        mm(bk[:, c0:c0 + 128], ident_b[:], bias_ap(pr, half, bid), False, True,
                           [R("ident_b"), R("biasT"), R("maskA")], [rb])

            def expo(i):
                t, pr, half = work[i]
                ncol = len(keys_of(t)) * 128
                for q in range((ncol + 511) // 512):
                    n = min(512, ncol - q * 512)
                    bi_ = 2 * (i % 2) + q
                    act(PT[i % 2][:, q * 512:q * 512 + n], banks[bi_][:, 0:n], AF.Exp, [RB[bi_]], [RPT[i % 2]], scale=scale)

            def pv(i):
                t, pr, half = work[i]
                ip = i // 2
                keys = keys_of(t)
                nk = len(keys)
                ob, rob = banks[4 + (ip % 2)], RB[4 + (ip % 2)]
                ps_ = slice(64 * half, 64 * half + 64)
                if kind == "A":
                    vsl = slice(64 * half, 64 * half + 64)
                else:
                    h = 2 * pr + half
                    vsl = slice(64 * h, 64 * h + 64)
                for kk, (j, bid) in enumerate(keys):
                    p_ = PT[i % 2][:, kk * 128:kk * 128 + 128]
                    mm(ob[ps_, 0:128], Vtm[:, j, vsl], p_, kk == 0, kk == nk - 1, [R("V"), RPT[i % 2]], [rob])
                for kk, (j, bid) in enumerate(keys):
                    p_ = PT[i % 2][:, kk * 128:kk * 128 + 128]
                    mm(ob[ps_, 128:256], ones_b[:, 0:64], p_, kk == 0, kk == nk - 1, [R("ones_b"), RPT[i % 2]], [rob])
                if half == 0:
                    return
                rd, rrd = RDn[ip % 2], RRD[ip % 2]
                if kind == "A":
                    ts("dve", rd, ob[:, 128:256], esink[:, pr:pr + 1], ALU.add, [rob, R("esink")], [rrd])
                    S.op("dve", lambda e: e.reciprocal(out=rd, in_=rd), [rrd], [rrd])
                else:
                    S.op("dve", lambda e: e.reciprocal(out=rd, in_=ob[:, 128:256]), [rob], [rrd])
                tt("dve", OT[ip % 4], ob[:, 0:128], rd, ALU.mult, [rob, rrd], [ROT[ip % 4]])
                if pr == 0:
                    return
                pending.append((t, ip))

            pending = []

            def proj_tile(t, ip):
                j = 0 if t < NLAT else 1
                o0, o1 = OT[(ip - 1) % 4], OT[ip % 4]
                ro0, ro1 = ROT[(ip - 1) % 4], ROT[ip % 4]
                for hf in range(2):
                    bk, rb = banks[6 + hf], RB[6 + hf]
                    sl = slice(hf * 512, hf * 512 + 512)
                    mm(bk[:, :], o0, wo[:, 0, sl], True, False, [ro0, R("wo")], [rb])
                    mm(bk[:, :], o1, wo[:, 1, sl], False, True, [ro1, R("wo")], [rb])
                x_update(t, banks[6][:, :], banks[7][:, :], RB[6], RB[7], 0 + j, tmp, rtmp)

            n = len(work)
            scores(0)
            for i in range(n):
                expo(i)
                if i + 1 < n:
                    scores(i + 1)
                if len(pending) and work[i][1:] == (0, 1):
                    proj_tile(*pending.pop(0))
                pv(i)
            while pending:
                proj_tile(*pending.pop(0))

        def bc_mid(ap2d, n):
            return ap2d.unsqueeze(1).to_broadcast([128, n, ap2d.shape[1]])

        def bc_last(ap2d, n):
            return ap2d.unsqueeze(2).to_broadcast([128, ap2d.shape[1], n])

        def ssd_phase(l, need_ctx):
            S.barrier()
            rg = region()
            xs_tm = rg.take([128, NT, 512], BF16)
            B_tm = rg.take([128, NT, 128], BF16)
            BT = rg.take([128, TOK], BF16)
            CT = rg.take([128, TOK], BF16)
            dt = rg.take([128, NT, 16], F32)
            da = rg.take([128, NT, 16], F32)
            acs = rg.take([128, NT, 16], F32)
            scw = rg.take([128, NT, 16], F32)
            etg = rg.take([128, NT, 8], F32)
            convw = rg.take([128, 30], F32)
            convb = rg.take([128, 6], F32)
            dtb_b = rg.take([128, 16], F32)
            a_b = rg.take([128, 16], F32)
            Db = rg.take([128, 8], F32)
            normg = rg.take([128, 4], F32)
            mark = rg.off
            load("sp", convw, convw_d[l], [R("convw")])
            load("sp", convb, convb_d[l], [R("convb")])
            load("sp", dtb_b, dtb_d[l:l + 1, :].partition_broadcast(128), [R("dtb_b")])
            load("sp", a_b, alog_d[l:l + 1, :].partition_broadcast(128), [R("a_b")])
            load("sp", Db, ssdd_d[l:l + 1, :].partition_broadcast(128), [R("Db")])
            load("sp", normg, normgT_d[l], [R("normg")])
            act(a_b, a_b, AF.Exp, [R("a_b")], [R("a_b")])
            ts("dve", a_b, a_b, -1.0, ALU.mult, [R("a_b")], [R("a_b")])

            wx = [rg.take([128, 8, 128], BF16) for _ in range(2)]
            Rwx = [Res("wx%d" % i) for i in range(2)]
            pre2 = [rg.take([128, TOK], F32) for _ in range(2)]
            Rpre = [Res("pre%d" % i) for i in range(2)]
            acc = rg.take([128, TOK], F32)
            post = rg.take([128, TOK], BF16)
            SEGS = [(0, 2048), (2048, 2304)]
            def stageP(ci):
                i = ci % 2
                pre, rpre = pre2[i], Rpre[i]
                load("pool", wx[i], wview(w_in_d[l], 1024 + ci * 128, 1024 + ci * 128 + 128), [Rwx[i]], sem="wx%d" % i)
                for bi, (t0, ntile) in enumerate(BLOCKS):
                    n = ntile * 128
                    bk, rb = banks[bi % 4], RB[bi % 4]
                    for k in range(8):
                        mm(bk[:, 0:n], wx[i][:, k, :], HT[:, k, t0 * 128:t0 * 128 + n], k == 0, k == 7,
                           [Rwx[i]] + RH[t0:t0 + ntile], [rb])
                    act(pre[:, t0 * 128:t0 * 128 + n], bk[:, 0:n], AF.Copy, [rb], [rpre])

            def stageC(ci):
                i = ci % 2
                pre, rpre = pre2[i], Rpre[i]
                for (a, b) in SEGS:
                    ts("dve", acc[:, a:b], pre[:, a:b], convw[:, ci * 5 + 2:ci * 5 + 3], ALU.mult, [rpre, R("convw"), R("convb")], [R("acc")],
                       s2=convb[:, ci:ci + 1], op1=ALU.add)
                    for kk in (0, 1, 3, 4):
                        s = kk - 2
                        lo = max(a, a - s)
                        hi = min(b, b - s)
                        stt(acc[:, lo:hi], pre[:, lo + s:hi + s], convw[:, ci * 5 + kk:ci * 5 + kk + 1], acc[:, lo:hi], ALU.mult, ALU.add,
                            [rpre, R("convw"), R("acc")], [R("acc")])

            def stageF(ci):
                if ci < 4:
                    dst_fm, rdst = post, R("post")
                elif ci == 4:
                    dst_fm, rdst = BT, R("BT")
                else:
                    dst_fm, rdst = CT, R("CT")
                act(dst_fm, acc, AF.Silu, [R("acc")], [rdst])
                if ci <= 4:
                    for g0 in range(0, NT, 8):
                        ng = min(8, NT - g0)
                        bi_ = 4 + (g0 // 8) % 2 + 2 * (ci % 2)
                        bk, rb = banks[bi_], RB[bi_]
                        bv = bk.bitcast(BF16)
                        for q in range(ng):
                            t = g0 + q
                            tr(bv[:, q * 128:q * 128 + 128], dst_fm[:, t * 128:t * 128 + 128], ident_b[:], [rdst, R("ident_b")], [rb])
                        src = bv[:, 0:ng * 128].rearrange("p (q c) -> p q c", c=128)
                        if ci < 4:
                            cp("act_copy", xs_tm[:, g0:g0 + ng, ci * 128:ci * 128 + 128], src, [rb], [R("xs_tm")])
                        else:
                            cp("act_copy", B_tm[:, g0:g0 + ng, :], src, [rb], [R("B_tm")])

            stageP(0)
            for ci in range(6):
                if ci + 1 < 6:
                    stageP(ci + 1)
                stageC(ci)
                stageF(ci)

            if cfg.get("ssd_stop", 9) <= 1:
                return
            S.barrier()
            rg.off = mark
            zs = rg.take([128, NT, 512], BF16)
            mark2 = rg.off
            wz = rg.take([128, 8, 512], BF16)
            wdt = rg.take([128, 8, 16], BF16)
            tot = rg.take([128, NT, 16], F32)
            etot = rg.take([128, NT, 16], F32)
            load("pool", wz, wview(w_in_d[l], 512, 1024), [R("wz")], sem="wz")
            load("pool", wdt, wview(w_in_d[l], 1792, 1808), [R("wdt")], sem="wdt")
            for t in range(NT):
                for k in range(8):
                    mm(banks[0][:, t * 16:t * 16 + 16], HT[:, k, t * 128:t * 128 + 128], wdt[:, k, :], k == 0, k == 7,
                       [R("wdt"), RH[t]], [RB[0]])
            p0 = banks[0][:, 0:NT * 16].rearrange("p (t c) -> p t c", c=16)
            tt("dve", dt, p0, bc_mid(dtb_b, NT), ALU.add, [RB[0], R("dtb_b")], [R("dt")])
            act(dt, dt, AF.Exp, [R("dt")], [R("dt")])
            act(dt, dt, AF.Ln, [R("dt")], [R("dt")], bias=1.0, scale=1.0)
            tt("dve", da, dt, bc_mid(a_b, NT), ALU.mult, [R("dt"), R("a_b")], [R("da")])
            for t in range(NT):
                mm(banks[1][:, t * 16:t * 16 + 8], triU[:], da[:, t, 0:8], True, True, [R("triU"), R("da")], [RB[1]])
                mm(banks[1][:, t * 16 + 8:t * 16 + 16], triL[:], da[:, t, 8:16], True, True, [R("triL"), R("da")], [RB[1]])
                mm(banks[2][:, t * 16:t * 16 + 16], ones_f[:], da[:, t, :], True, True, [R("ones_f"), R("da")], [RB[2]])
            p1 = banks[1][:, 0:NT * 16].rearrange("p (t c) -> p t c", c=16)
            p2 = banks[2][:, 0:NT * 16].rearrange("p (t c) -> p t c", c=16)
            cp("dve", acs, p1, [RB[1]], [R("acs")])
            cp("dve", tot, p2, [RB[2]], [R("tot")])
            tt("dve", scw, tot, acs, ALU.subtract, [R("tot"), R("acs")], [R("scw")])
            act(scw, scw, AF.Exp, [R("scw")], [R("scw")])
            tt("dve", scw, scw, dt, ALU.mult, [R("scw"), R("dt")], [R("scw")])
            act(etot, tot, AF.Exp, [R("tot")], [R("etot")])
            for d_ in range(2):
                for g in range(2):
                    cp("dve", etg[64 * g:64 * g + 64, :, 4 * d_:4 * d_ + 4], etot[64 * g:64 * g + 64, :, 8 * d_ + 4 * g:8 * d_ + 4 * g + 4],
                       [R("etot")], [R("etg")])
            for t in range(NT):
                if t >= NLAT and not need_ctx:
                    continue
                bk, rb = banks[4 + t % 4], RB[4 + t % 4]
                for k in range(8):
                    mm(bk[:, :], HT[:, k, t * 128:t * 128 + 128], wz[:, k, :], k == 0, k == 7, [R("wz"), RH[t]], [rb])
                act(zs[:, t, :], bk[:, :], AF.Silu, [rb], [R("zs")])

            if cfg.get("ssd_stop", 9) <= 2:
                return
            S.barrier()
            rg.off = mark2
            prevB = rg.take([128, NT, 256], BF16)
            woS = rg.take([128, 4, D], BF16)
            load("pool", woS, w_outP_d[l][256:768, :].rearrange("(k p) n -> p k n", p=128), [R("woS")], sem="woS")
            hr = ht_region()
            xw = [hr.take([128, 512], BF16) for _ in range(2)]
            Rxw = [Res("xw%d" % i) for i in range(2)]
            xdt = [hr.take([128, 512], BF16) for _ in range(2)]
            Rxdt = [Res("xdt%d" % i) for i in range(2)]
            rhsU = [hr.take([128, 8, 128], F32) for _ in range(2)]
            RrhsU = [Res("rhsU%d" % i) for i in range(2)]
            E = [hr.take([128, 8, 128], BF16) for _ in range(2)]
            RE = [Res("E%d" % i) for i in range(2)]
            Eb = [hr.take([128, 8, 128], BF16) for _ in range(2)]
            REb = [Res("Eb%d" % i) for i in range(2)]
            Gm = [hr.take([128, 2, 128], BF16) for _ in range(2)]
            RGm = [Res("Gm%d" % i) for i in range(2)]
            nacs = hr.take([128, NT, 16], F32)
            Sf = hr.take([128, 256], F32)
            Sf_bf = hr.take([128, 256], BF16)
            Sb = hr.take([128, 256], F32)
            tmpD = hr.take([128, 512], F32)
            ytot = hr.take([128, 512], F32)
            yn = hr.take([128, 512], BF16)
            oT = hr.take([128, 4, 128], BF16)
            upd = hr.take([128, 512], F32)
            sjunk = hr.take([128, 512], BF16)
            ssq = hr.take([128, 64], F32)
            rupd = Res("updS")
            for t in range(NT):
                RH[t] = Res("H%d" % t)

            memset("dve", Sb, 0.0, [R("Sb")])
            memset("dve", Sf, 0.0, [R("Sf")])
            memset("dve", Sf_bf, 0.0, [R("Sf_bf")])
            ts("dve", nacs, acs, -1.0, ALU.mult, [R("acs")], [R("nacs")])

            def xs3(c):
                return xs_tm[:, c, :].rearrange("p (h q) -> p h q", q=64)

            STB0 = 5

            def states(c, d_, i, Sacc, rS, alt=False):
                STB = 7 if (alt and i % 2 == 1) else STB0
                tt("pool", xw[i][:].rearrange("p (h q) -> p h q", q=64), xs3(c), bc_last(scw[:, c, 8 * d_:8 * d_ + 8], 64), ALU.mult,
                   [R("xs_tm"), R("scw")], [Rxw[i]])
                for g in range(2):
                    mm(banks[STB][64 * g:64 * g + 64, 256:512], B_tm[:, c, 64 * g:64 * g + 64], xw[i][:, 256 * g:256 * g + 256], True, True,
                       [R("B_tm"), Rxw[i]], [RB[STB]])
                for hl in range(4):
                    sl = slice(64 * hl, 64 * hl + 64)
                    stt(Sacc[:, sl], Sacc[:, sl], etg[:, c, 4 * d_ + hl:4 * d_ + hl + 1], banks[STB][:, 256 + 64 * hl:256 + 64 * hl + 64], ALU.mult, ALU.add,
                        [rS, R("etg"), RB[STB]], [rS])

            for n_, c in enumerate([17, 16] + list(range(15, -1, -1))):
                cp("act_copy", prevB[:, c, :], Sb, [R("Sb")], [R("prevB")])
                if c != 0:
                    states(c, 1, n_ % 2, Sb, R("Sb"), alt=True)

            order2 = [16, 17] + list(range(NLAT))

            def front(n_, c):
                emit_out = need_ctx or c < NLAT
                csl = slice(c * 128, c * 128 + 128)
                if emit_out:
                    gbanks = ((banks[2][:, 0:128], RB[2]), (banks[3][:, 0:128], RB[3]))
                    for g in range(2):
                        ps_ = slice(64 * g, 64 * g + 64)
                        mm(gbanks[g][0], BT[ps_, csl], CT[ps_, csl], True, True, [R("BT"), R("CT")], [gbanks[g][1]])
                    for d_ in range(2):
                        tri = triU if d_ == 0 else triL
                        for g in range(2):
                            tt("dve", Gm[d_][:, g, :], gbanks[g][0], tri[:], ALU.mult, [gbanks[g][1], R("triU"), R("triL")], [RGm[d_]])
                    for d_ in range(2):
                        tri = triU if d_ == 0 else triL
                        tt("pool", rhsU[d_], bc_mid(tri[:], 8), bc_last(da[:, c, 8 * d_:8 * d_ + 8], 128), ALU.mult,
                           [R("triU"), R("triL"), R("da")], [RrhsU[d_]])
                    for d_ in range(2):
                        tt("dve", xdt[d_][:].rearrange("p (h q) -> p h q", q=64), xs3(c), bc_last(dt[:, c, 8 * d_:8 * d_ + 8], 64), ALU.mult,
                           [R("xs_tm"), R("dt")], [Rxdt[d_]])
                    for d_ in range(2):
                        for h in range(8):
                            bi_ = h // 4
                            mm(banks[bi_][:, (h % 4) * 128:(h % 4) * 128 + 128], ones_f[:], rhsU[d_][:, h, :], True, True,
                               [R("ones_f"), RrhsU[d_]], [RB[bi_]])
                        for q in range(2):
                            bi_ = q
                            for h in range(4 * q, 4 * q + 4):
                                act(E[d_][:, h, :], banks[bi_][:, (h % 4) * 128:(h % 4) * 128 + 128], AF.Exp, [RB[bi_], R("nacs")], [RE[d_]],
                                    bias=nacs[:, c, 8 * d_ + h:8 * d_ + h + 1], scale=1.0)
                            act(Eb[d_][:, 4 * q:4 * q + 4, :].rearrange("p h i -> p (h i)"), banks[bi_][:, :], AF.Exp, [RB[bi_]], [REb[d_]])
                    for d_ in range(2):
                        ts("dve", E[d_], E[d_], 1.0, ALU.min, [RE[d_]], [RE[d_]])
                        for g in range(2):
                            tt("dve", E[d_][:, 4 * g:4 * g + 4, :], E[d_][:, 4 * g:4 * g + 4, :], bc_mid(Gm[d_][:, g, :], 4), ALU.mult,
                               [RE[d_], RGm[d_]], [RE[d_]])
                        tt("pool", Eb[d_], Eb[d_], bc_mid(CT[:, csl], 8), ALU.mult, [REb[d_], R("CT")], [REb[d_]])

            def front_y(n_, c):
                emit_out = need_ctx or c < NLAT
                if emit_out:
                    for d_ in range(2):
                        for h in range(8):
                            g, hl = h // 4, h % 4
                            ysl = slice(64 * h, 64 * h + 64)
                            mm(banks[4][:, ysl], E[d_][:, h, :], xdt[d_][:, ysl], d_ == 0 and h == 0, False, [RE[d_], Rxdt[d_]], [RB[4]], sgc=True)
                            st_ = Sf_bf if d_ == 0 else prevB[:, c, :]
                            rst = R("Sf_bf") if d_ == 0 else R("prevB")
                            mm(banks[4][:, ysl], Eb[d_][64 * g:64 * g + 64, h, :], st_[64 * g:64 * g + 64, 64 * hl:64 * hl + 64], False, d_ == 1,
                               [REb[d_], rst], [RB[4]], sgc=True)

            def fstates(n_, c):
                if c != NLAT - 1:
                    states(c, 0, n_ % 2, Sf, R("Sf"))
                    cp("dve", Sf_bf, Sf, [R("Sf")], [R("Sf_bf")])

            def back1(n_, c):
                emit_out = need_ctx or c < NLAT
                if not emit_out:
                    return
                tt("dve", tmpD[:].rearrange("p (h q) -> p h q", q=64), xs3(c), bc_last(Db, 64), ALU.mult, [R("xs_tm"), R("Db")], [R("tmpD")])
                tt("dve", ytot, banks[4][:, :], tmpD, ALU.add, [RB[4], R("tmpD")], [R("ytot")])

            def back(n_, c):
                emit_out = need_ctx or c < NLAT
                if not emit_out:
                    return
                j = 0 if c < NLAT else 1
                tt("dve", ytot, ytot, zs[:, c, :], ALU.mult, [R("ytot"), R("zs")], [R("ytot")])
                act(sjunk, ytot, AF.Square, [R("ytot")], [R("sjunk"), R("ssq")], accum_out=ssq[:, c:c + 1])
                act(ssq[:, 32 + c:33 + c], ssq[:, c:c + 1], AF.Ln, [R("ssq")], [R("ssq")], bias=1e-6, scale=1.0 / 512)
                act(ssq[:, 32 + c:33 + c], ssq[:, 32 + c:33 + c], AF.Exp, [R("ssq")], [R("ssq")], scale=-0.5)
                ts("dve", yn, ytot, ssq[:, 32 + c:33 + c], ALU.mult, [R("ytot"), R("ssq")], [R("yn")])
                b5 = banks[5].bitcast(BF16)
                for kc in range(4):
                    tr(b5[:, kc * 128:kc * 128 + 128], yn[:, kc * 128:kc * 128 + 128], ident_b[:], [R("yn"), R("ident_b")], [RB[5]])
                for kc in range(4):
                    ts("dve", oT[:, kc, :], b5[:, kc * 128:kc * 128 + 128], normg[:, kc:kc + 1], ALU.mult, [RB[5], R("normg")], [R("oT")])
                for hf in range(2):
                    sl = slice(hf * 512, hf * 512 + 512)
                    for kc in range(4):
                        mm(banks[6 + hf][:, :], oT[:, kc, :], woS[:, kc, sl], kc == 0, kc == 3, [R("oT"), R("woS")], [RB[6 + hf]])
                for hf in range(2):
                    sl = slice(hf * 512, hf * 512 + 512)
                    tt("dve", upd, banks[6 + hf][:, :], GATE[:, j, sl], ALU.mult, [RB[6 + hf], R("GATE")], [rupd])
                    tt("dve", X[:, c, sl], X[:, c, sl], upd, ALU.add, [rupd, RX[c]], [RX[c]])

            front(0, order2[0])
            front_y(0, order2[0])
            fstates(0, order2[0])
            for n_, c in enumerate(order2):
                if n_ + 1 < len(order2):
                    front(n_ + 1, order2[n_ + 1])
                back1(n_, c)
                if n_ + 1 < len(order2):
                    front_y(n_ + 1, order2[n_ + 1])
                back(n_, c)
                if n_ + 1 < len(order2):
                    fstates(n_ + 1, order2[n_ + 1])


        def tree(op, dst, src, width, n1, tmpbuf):
            cur = src
            w = width
            while w > 1:
                h = w // 2
                out = dst.unsqueeze(2) if h == 1 else tmpbuf[:, :, 0:h]
                tt("dve", out, cur[:, :, 0:h], cur[:, :, h:w], op, [R("rt")], [R("rt")])
                cur = out
                w = h

        def moe_phase(l, need_ctx):
            tiles = list(range(NT)) if need_ctx else list(range(NLAT))
            ntl = len(tiles)
            S.barrier()
            rg = region()
            comb = rg.take([128, NT, 32], F32)
            lg = rg.take([128, NT, 36], F32)
            NS = 3
            WGU = [rg.take([128, 8, 512], BF16) for _ in range(2)]
            WD = [rg.take([128, 2, D], BF16) for _ in range(2)]
            WDx = [rg.take([128, 2, D], BF16) for _ in range(2)]
            WDc = [rg.take([128, 2, D], BF16) for _ in range(2)]
            Rgu = [Res("wgu%d" % i) for i in range(NS)]
            Rwd = [Res("wd%d" % i) for i in range(NS)]
            Rwdx = [Res("wdx%d" % i) for i in range(NS)]
            s_sb = [rg.take([128, 256], F32) for _ in range(2)]
            Rs = [Res("s%d" % i) for i in range(2)]
            hid = [rg.take([128, 256], BF16) for _ in range(2)]
            Rhid = [Res("hid%d" % i) for i in range(2)]
            hidT = [rg.take([128, 2, 128], BF16) for _ in range(2)]
            RhT = [Res("hidT%d" % i) for i in range(2)]
            mark = rg.off

            def load_dma(e, extra=()):
                sl = e % NS
                S.dma("pool", [lambda en: en.dma_start(out=WGU[sl][:, :, 0:256], in_=wg_d[l, e].rearrange("(k p) n -> p k n", p=128)),
                               lambda en: en.dma_start(out=WGU[sl][:, :, 256:512], in_=wu_d[l, e].rearrange("(k p) n -> p k n", p=128))],
                      "wgu%d" % sl, [], [Rgu[sl]] + list(extra))
                S.dma("pool", [lambda en: en.dma_start(out=WD[sl], in_=wd_d[l, e].rearrange("(k p) n -> p k n", p=128))],
                      "wd%d" % sl, [], [Rwd[sl]] + list(extra))

            def load_scale(e):
                sl = e % NS
                for fc in range(2):
                    tt("pool", WDx[sl][:, fc, :], WD[sl][:, fc, :], GATE[:, 2, :], ALU.mult, [Rwd[sl], R("GATE")], [Rwdx[sl]])
                    if need_ctx:
                        tt("pool", WDc[sl][:, fc, :], WD[sl][:, fc, :], GATE[:, 3, :], ALU.mult, [Rwd[sl], R("GATE")], [Rwdx[sl]])

            def load_expert(e, extra=()):
                load_dma(e, extra)
                load_scale(e)

            load_dma(0)
            load_dma(1)
            w_rt = rg.take([128, 8, 36], F32)
            brt = rg.take([128, 36], F32)
            load("sp", w_rt, w_rt_d[l].rearrange("(k p) n -> p k n", p=128), [R("w_rt")])
            load("sp", brt, b_rt_d[l:l + 1, :].partition_broadcast(128), [R("brt")])

            def router(t, hf, rhf):
                bi_ = 4 + t // 9
                c0 = (t % 9) * 36
                for k in range(8):
                    mm(banks[bi_][:, c0:c0 + 36], hf[:, k, :], w_rt[:, k, :], k == 0, k == 7, [rhf, R("w_rt")], [RB[bi_]])

            norm_phase(1, tiles, rg=rg, router=router)
            for q in range(2):
                t0, t1 = 9 * q, min(9 * q + 9, ntl)
                if t1 <= t0:
                    continue
                n_ = t1 - t0
                src = banks[4 + q][:, 0:n_ * 36].rearrange("p (t c) -> p t c", c=36)
                tt("dve", lg[:, t0:t1, :], src, bc_mid(brt, n_), ALU.add, [RB[4 + q], R("brt")], [R("rt")])
            S.barrier()
            rg.off = mark
            load_scale(0)
            load_scale(1)
            WGU.append(rg.take([128, 8, 512], BF16))
            WD.append(rg.take([128, 2, D], BF16))
            WDx.append(rg.take([128, 2, D], BF16))
            WDc.append(rg.take([128, 2, D], BF16))
            rg.off = mark
            gl = lg[:, 0:ntl, 0:4]
            el = lg[:, 0:ntl, 4:36]
            t4 = rg.take([128, NT, 4], F32)
            t32 = rg.take([128, NT, 32], F32)
            elm = rg.take([128, NT, 32], F32)
            m1b = rg.take([128, NT, 32], F32)
            m2b = rg.take([128, NT, 32], F32)
            gmax = rg.take([128, NT], F32)
            gw = rg.take([128, NT], F32)
            m1 = rg.take([128, NT], F32)
            m2 = rg.take([128, NT], F32)
            w1 = rg.take([128, NT], F32)
            w2 = rg.take([128, NT], F32)
            RT = [R("rt")]
            n = ntl
            tree(ALU.max, gmax[:, 0:n], gl, 4, n, t4[:, 0:n, :])
            tt("dve", t4[:, 0:n, :], gl, bc_last(gmax[:, 0:n], 4), ALU.subtract, RT, RT)
            act(t4[:, 0:n, :], t4[:, 0:n, :], AF.Exp, RT, RT)
            tree(ALU.add, gw[:, 0:n], t4[:, 0:n, :], 4, n, t32[:, 0:n, 0:4])
            S.op("dve", lambda e: e.reciprocal(out=gw[:, 0:n], in_=gw[:, 0:n]), RT, RT)
            tt("dve", t4[:, 0:n, :], gl, bc_last(gmax[:, 0:n], 4), ALU.is_equal, RT, RT)
            ts("dve", t4[:, 0:n, :], t4[:, 0:n, :], -1.0, ALU.add, RT, RT, s2=-NEG, op1=ALU.mult)
            for g in range(4):
                tt("dve", elm[:, 0:n, 8 * g:8 * g + 8], el[:, :, 8 * g:8 * g + 8], t4[:, 0:n, g:g + 1].to_broadcast([128, n, 8]), ALU.add, RT, RT)
            tree(ALU.max, m1[:, 0:n], elm[:, 0:n, :], 32, n, t32[:, 0:n, :])
            tt("dve", m1b[:, 0:n, :], elm[:, 0:n, :], bc_last(m1[:, 0:n], 32), ALU.is_equal, RT, RT)
            stt(elm[:, 0:n, :], m1b[:, 0:n, :], 2 * NEG, elm[:, 0:n, :], ALU.mult, ALU.add, RT, RT)
            tree(ALU.max, m2[:, 0:n], elm[:, 0:n, :], 32, n, t32[:, 0:n, :])
            tt("dve", m2b[:, 0:n, :], elm[:, 0:n, :], bc_last(m2[:, 0:n], 32), ALU.is_equal, RT, RT)
            tt("dve", w2[:, 0:n], m2[:, 0:n], m1[:, 0:n], ALU.subtract, RT, RT)
            act(w2[:, 0:n], w2[:, 0:n], AF.Exp, RT, RT)
            ts("dve", w1[:, 0:n], w2[:, 0:n], 1.0, ALU.add, RT, RT)
            S.op("dve", lambda e: e.reciprocal(out=w1[:, 0:n], in_=w1[:, 0:n]), RT, RT)
            tt("dve", w1[:, 0:n], w1[:, 0:n], gw[:, 0:n], ALU.mult, RT, RT)
            tt("dve", w2[:, 0:n], w2[:, 0:n], w1[:, 0:n], ALU.mult, RT, RT)
            tt("dve", m1b[:, 0:n, :], m1b[:, 0:n, :], bc_last(w1[:, 0:n], 32), ALU.mult, RT, RT)
            tt("dve", m2b[:, 0:n, :], m2b[:, 0:n, :], bc_last(w2[:, 0:n], 32), ALU.mult, RT, RT)
            tt("dve", comb[:, 0:n, :], m1b[:, 0:n, :], m2b[:, 0:n, :], ALU.add, RT, [R("comb")])
            items = [(e, t) for e in range(32) for t in tiles]
            nit = len(items)
            b2 = banks[2].bitcast(BF16)

            def stageG(i):
                e, t = items[i]
                sl = e % NS
                bk, rb = banks[i % 2], RB[i % 2]
                for k in range(8):
                    mm(bk[:, :], HT[:, k, t * 128:t * 128 + 128], WGU[sl][:, k, :], k == 0, k == 7, [RH[t], Rgu[sl]], [rb])
                act(s_sb[i % 2], bk[:, 0:256], AF.Silu, [rb], [Rs[i % 2]])
                stt(hid[i % 2], bk[:, 256:512], comb[:, t, e:e + 1], s_sb[i % 2], ALU.mult, ALU.mult, [rb, R("comb"), Rs[i % 2]], [Rhid[i % 2]])

            def stageT(i):
                for fc in range(2):
                    tr(b2[:, (i % 2) * 256 + fc * 128:(i % 2) * 256 + fc * 128 + 128], hid[i % 2][:, fc * 128:fc * 128 + 128], ident_b[:],
                       [Rhid[i % 2], R("ident_b")], [RB[2]])
                cp("act_copy", hidT[i % 2][:].rearrange("p a b -> p (a b)"), b2[:, (i % 2) * 256:(i % 2) * 256 + 256], [RB[2]], [RhT[i % 2]])

            def stageD(i):
                e, t = items[i]
                sl = e % NS
                wdd = WDx[sl] if t < NLAT else WDc[sl]
                for fc in range(2):
                    for hf in range(2):
                        bi_ = 4 + 2 * (i % 2) + hf
                        mm(banks[bi_][:, :], hidT[i % 2][:, fc, :], wdd[:, fc, hf * 512:hf * 512 + 512], fc == 0, fc == 1,
                           [RhT[i % 2], Rwdx[sl]], [RB[bi_]])
                for hf in range(2):
                    bi_ = 4 + 2 * (i % 2) + hf
                    sl_ = slice(hf * 512, hf * 512 + 512)
                    tt("dve", X[:, t, sl_], X[:, t, sl_], banks[bi_][:, :], ALU.add, [RX[t], RB[bi_]], [RX[t]])

            load_expert(2, extra=[R("rt")])
            for i in range(nit + 2):
                if i < nit:
                    stageG(i)
                if 0 <= i - 1 < nit:
                    stageT(i - 1)
                if 0 <= i - 2 < nit:
                    stageD(i - 2)
                    e, t = items[i - 2]
                    if t == tiles[-1] and e + NS < 32:
                        load_expert(e + NS)


        def final_phase():
            S.barrier()
            rg = region()
            gfb = rg.take([128, D], F32)
            junk = rg.take([128, D], BF16)
            load("sp", gfb, g_final_d.partition_broadcast(128), [R("gfb")])
            ot = [rg.take([128, D], F32) for _ in range(2)]
            rot = [Res("fo%d" % i) for i in range(2)]
            ov = out_d.rearrange("(t p) d -> p t d", p=128)
            for t in range(NLAT):
                b = t % 2
                if final_norm:
                    act(junk[:], X[:, t, :], AF.Square, [RX[t]], [R("junk"), R("ss%d" % t)], accum_out=small[:, t:t + 1])
                    act(small[:, 32 + t:33 + t], small[:, t:t + 1], AF.Ln, [R("ss%d" % t)], [R("rs%d" % t)], bias=1e-6, scale=1.0 / D)
                    act(small[:, 32 + t:33 + t], small[:, 32 + t:33 + t], AF.Exp, [R("rs%d" % t)], [R("rs%d" % t)], scale=-0.5)
                    stt(ot[b], X[:, t, :], small[:, 32 + t:33 + t], gfb, ALU.mult, ALU.mult, [RX[t], R("rs%d" % t), R("gfb")], [rot[b]])
                else:
                    cp("dve", ot[b], X[:, t, :], [RX[t]], [rot[b]])
                S.dma("sp", [lambda e, b=b, t=t: e.dma_start(out=ov[:, t, :], in_=ot[b])], "outst", [rot[b]], [])
            if dbg_d is not None:
                dv = dbg_d.rearrange("(t p) d -> p t d", p=128)
                for t in range(2):
                    S.dma("sp", [lambda e, t=t: e.dma_start(out=dv[:, t, :], in_=X[:, NLAT + t, :])], "outst", [RX[NLAT + t]], [])
            S.streams["sp"].append(("wait", ("dma", "outst"), S.dmacnt[("dma", "outst")]))

        for l in range(nlayers):
            need_ctx = l < DEPTH - 1
            mod_phase(l)
            S.barrier()
            norm_phase(0, list(range(NT)))
            if "A" in phases:
                attn_phase("A", l, need_ctx)
            if "N" in phases:
                attn_phase("N", l, need_ctx)
            if "S" in phases:
                ssd_phase(l, need_ctx)
            if "M" in phases:
                moe_phase(l, need_ctx)
        final_phase()
        S.emit()
    return nc


def _prep_shared(inp):
    f = np.float32
    sh = {}
    sh["w_mod"] = np.ascontiguousarray(inp["w_mod"], f)
    sh["b_mod"] = np.ascontiguousarray(inp["b_mod"], f)
    sh["b_modT"] = np.ascontiguousarray(inp["b_mod"].reshape(DEPTH, 48, 128).transpose(0, 2, 1), f)
    sh["g_mixT"] = np.ascontiguousarray(inp["g_mix"].reshape(DEPTH, 8, 128).transpose(0, 2, 1), f)
    sh["g_ffnT"] = np.ascontiguousarray(inp["g_ffn"].reshape(DEPTH, 8, 128).transpose(0, 2, 1), f)
    sh["g_final"] = np.ascontiguousarray(inp["g_final"].reshape(1, D), f)
    w_in = np.asarray(inp["w_in"], f)
    sw = _swap_idx()
    qcol = lambda h: np.arange(64 * h, 64 * h + 64)
    kcol = lambda g: 256 + np.arange(64 * g, 64 * g + 64)
    Q02 = np.concatenate([qcol(0), qcol(2)])
    Q13 = np.concatenate([qcol(1), qcol(3)])
    K01 = np.concatenate([kcol(0), kcol(1)])
    Q02s = np.concatenate([qcol(0)[sw], qcol(2)[sw]])
    Q13s = np.concatenate([qcol(1)[sw], qcol(3)[sw]])
    K01s = np.concatenate([kcol(0)[sw], kcol(1)[sw]])
    Vc = 384 + np.arange(128)
    colsA = np.concatenate([Q02, Q13, K01, Q02s, Q13s, K01s, Vc])
    sh["w_inA"] = np.ascontiguousarray(w_in[:, :, colsA])
    sh["w_in"] = np.ascontiguousarray(w_in)
    rows = np.concatenate([np.arange(0, 64), np.arange(128, 192), np.arange(64, 128), np.arange(192, 256), np.arange(256, 1024)])
    sh["w_outP"] = np.ascontiguousarray(np.asarray(inp["w_out"], f)[:, rows, :])
    sk = np.asarray(inp["attn_sink"], f)
    sinkE = np.zeros((DEPTH, 128, 2), f)
    sinkE[:, 0:64, 0] = sk[:, 0:1]; sinkE[:, 64:128, 0] = sk[:, 2:3]
    sinkE[:, 0:64, 1] = sk[:, 1:2]; sinkE[:, 64:128, 1] = sk[:, 3:4]
    sh["sinkE"] = sinkE
    cw = np.asarray(inp["ssd_conv_w"], f)
    sh["convw"] = np.ascontiguousarray(cw.reshape(DEPTH, 5, 6, 128).transpose(0, 3, 2, 1).reshape(DEPTH, 128, 30))
    sh["convb"] = np.ascontiguousarray(np.asarray(inp["ssd_conv_b"], f).reshape(DEPTH, 6, 128).transpose(0, 2, 1))
    sh["dtb"] = np.ascontiguousarray(np.asarray(inp["ssd_dt_bias"], f).reshape(DEPTH, 16))
    sh["alog"] = np.ascontiguousarray(np.asarray(inp["ssd_a_log"], f).reshape(DEPTH, 16))
    sh["ssdd"] = np.ascontiguousarray(np.asarray(inp["ssd_d"], f))
    sh["normgT"] = np.ascontiguousarray(np.asarray(inp["ssd_norm_g"], f).reshape(DEPTH, 4, 128).transpose(0, 2, 1))
    rpb = np.asarray(inp["na_rpb"], f)
    sh["naTab"] = np.stack([_na_table(rpb[l]).reshape(128, 6144) for l in range(DEPTH)], 0)
    sh["w_rt"] = np.ascontiguousarray(np.concatenate([np.asarray(inp["w_router_group"], f), np.asarray(inp["w_router_expert"], f)], -1))
    sh["b_rt"] = np.ascontiguousarray(np.concatenate([np.asarray(inp["b_router_group"], f), np.asarray(inp["b_router_expert"], f)], -1))
    sh["w_exp_gate"] = np.ascontiguousarray(np.asarray(inp["w_exp_gate"], f).reshape(DEPTH, 32, D, 256))
    sh["w_exp_up"] = np.ascontiguousarray(np.asarray(inp["w_exp_up"], f).reshape(DEPTH, 32, D, 256))
    sh["w_exp_down"] = np.ascontiguousarray(np.asarray(inp["w_exp_down"], f).reshape(DEPTH, 32, 256, D))
    ct = _const_tables()
    ct["maskA"] = ct["maskA"].reshape(128, 384)
    sh.update(ct)
    return sh


def _run(inp, cfg, cores=None):
    sh = _prep_shared(inp)
    x = np.asarray(inp["x"], np.float32)
    c = np.asarray(inp["c"], np.float32)
    ctx = np.asarray(inp["ctx"], np.float32)
    c_ctx = np.asarray(inp["c_ctx"], np.float32)
    cores = list(range(8)) if cores is None else cores
    in_maps = []
    for b in cores:
        m = dict(sh)
        m["x"] = np.ascontiguousarray(x[b])
        m["ctx"] = np.ascontiguousarray(ctx[b])
        scT = np.zeros((128, 8, 2), np.float32)
        scT[:, :, 0] = c[b].reshape(8, 128).T
        scT[:, :, 1] = c_ctx.reshape(8, 128).T
        m["scT"] = scT.reshape(128, 16)
        in_maps.append(m)
    nc = build_program(cfg)
    res = run_bass_kernel_spmd(nc, in_maps, core_ids=list(range(len(cores))))
    return res


def kernel(**inputs):
    res = _run(inputs, {})
    return np.stack([r["out"] for r in res.results], 0).astype(np.float32)
```
